# Optimizing a Trainium2 kernel written in Bass

```python
import jax
import jax.numpy as jnp
from jax import lax
import numpy as np

D_MODEL = 1024
BATCH = 4
SEQ = 4096
DEPTH = 2

N_BRANCH = 4
BRANCH_W = D_MODEL // 2
EPS = 1e-6
MAX_POS_OFFSET = 1024
S5_CH_PER_GROUP = 16
S5_GROUPS = BRANCH_W // S5_CH_PER_GROUP
S5_STATE = 64
S5_MAX_REAL = -1e-4
HG_HEADS = 4
HG_DK = BRANCH_W // HG_HEADS
HG_DV = BRANCH_W // HG_HEADS
HG_CHUNK = 64
RET_HEADS = 4
RET_DK = 64
RET_DV = BRANCH_W // RET_HEADS
RET_CHUNK = 64
ROPE_BASE = 10000.0
M2_HEADS = 8
M2_HEADDIM = BRANCH_W // M2_HEADS
M2_GROUPS = 2
M2_STATE = 128
M2_CONV = 4
M2_CHUNK = 64
M2_XBC = BRANCH_W + 2 * M2_GROUPS * M2_STATE
MOE_GROUPS = 4
MOE_EXPERTS_PER_GROUP = 8
MOE_TOPK = 2
MOE_FF = 256
IN_SIZES = (BRANCH_W,
            HG_HEADS * HG_DK, HG_HEADS * HG_DK, HG_HEADS * HG_DV, HG_HEADS * HG_DV,
            RET_HEADS * RET_DK, RET_HEADS * RET_DK, RET_HEADS * RET_DV, RET_HEADS * RET_DV,
            BRANCH_W, M2_XBC, M2_HEADS)
IN_W = BRANCH_W + 2 * HG_HEADS * (HG_DK + HG_DV) + 2 * RET_HEADS * (RET_DK + RET_DV) + BRANCH_W + M2_XBC + M2_HEADS

kernel_name = 'hybrid_gated_ssm_retention_moe_trunk'


def _rms(t):
    t32 = t.astype(jnp.float32)
    return (t32 * lax.rsqrt(jnp.mean(t32 * t32, axis=-1, keepdims=True) + EPS)).astype(t.dtype)


def _rope(t, positions):
    half = t.shape[-1] // 2
    inv_freq = ROPE_BASE ** (-jnp.arange(half, dtype=jnp.float32) / half)
    ang = positions.astype(jnp.float32)[:, :, None, None] * inv_freq
    cos, sin = jnp.cos(ang), jnp.sin(ang)
    t1 = t[..., :half].astype(jnp.float32)
    t2 = t[..., half:].astype(jnp.float32)
    return jnp.concatenate([t1 * cos - t2 * sin, t2 * cos + t1 * sin], axis=-1).astype(t.dtype)


def _chunk_gated_linear_attention(q, k, v, log_f, chunk):
    out_dtype = v.dtype
    f32 = jnp.float32
    bsz, nh, seq, dk = q.shape
    dv = v.shape[-1]
    n_chunks = seq // chunk

    def to_chunks(t):
        t = t.astype(f32).reshape(bsz, nh, n_chunks, chunk, t.shape[-1])
        return jnp.moveaxis(t, 2, 0)

    causal = jnp.tril(jnp.ones((chunk, chunk), dtype=bool))[:, :, None]

    def step(state, inp):
        qc, kc, vc, fc = inp
        b = jnp.cumsum(fc, axis=-2)
        diff = b[..., :, None, :] - b[..., None, :, :]
        decay = jnp.where(causal, jnp.exp(jnp.minimum(diff, 0.0)), 0.0)
        scores = jnp.sum(qc[..., :, None, :] * kc[..., None, :, :] * decay, axis=-1)
        o = (jnp.einsum('bhts,bhsv->bhtv', scores, vc)
             + jnp.einsum('bhtk,bhkv->bhtv', qc * jnp.exp(b), state))
        b_last = b[..., -1:, :]
        state = (jnp.exp(b_last[..., 0, :])[..., None] * state
                 + jnp.einsum('bhsk,bhsv->bhkv', kc * jnp.exp(b_last - b), vc))
        return state, o

    state0 = jnp.zeros((bsz, nh, dk, dv), f32)
    _, o = lax.scan(step, state0, tuple(map(to_chunks, (q, k, v, log_f))))
    o = jnp.moveaxis(o, 0, 2).reshape(bsz, nh, seq, dv)
    return o.astype(out_dtype)


def _ssd_chunked(x, dt, a, bm, cm, chunk):
    f32 = jnp.float32
    bsz, seq, nh, hp = x.shape
    ns = bm.shape[-1]
    n_chunks = seq // chunk
    xd = (x.astype(f32) * dt[..., None]).reshape(bsz, n_chunks, chunk, nh, hp)
    bc = bm.astype(f32).reshape(bsz, n_chunks, chunk, nh, ns)
    cc = cm.astype(f32).reshape(bsz, n_chunks, chunk, nh, ns)
    a_cs = jnp.cumsum((dt * a).reshape(bsz, n_chunks, chunk, nh).transpose(0, 3, 1, 2), axis=-1)
    causal = jnp.tril(jnp.ones((chunk, chunk), dtype=bool))
    seg = a_cs[..., :, None] - a_cs[..., None, :]
    l_mat = jnp.where(causal, jnp.exp(jnp.minimum(seg, 0.0)), 0.0)
    scores = jnp.einsum('bclhn,bcshn->bhcls', cc, bc) * l_mat
    y_diag = jnp.einsum('bhcls,bcshp->bclhp', scores, xd)
    decay_to_end = jnp.exp(a_cs[..., -1:] - a_cs)
    chunk_states = jnp.einsum('bclhn,bhcl,bclhp->bchpn', bc, decay_to_end, xd)
    chunk_decay = jnp.exp(a_cs[..., -1])

    def step(state, inp):
        dec, st = inp
        return dec[..., None, None] * state + st, state

    _, prev = lax.scan(step, jnp.zeros((bsz, nh, hp, ns), f32),
                       (jnp.moveaxis(chunk_decay, 2, 0), jnp.moveaxis(chunk_states, 1, 0)))
    prev = jnp.moveaxis(prev, 0, 1)
    y_off = jnp.einsum('bclhn,bchpn,bhcl->bclhp', cc, prev, jnp.exp(a_cs))
    return (y_diag + y_off).reshape(bsz, seq, nh, hp)


def _s5_mixer(u, lam_re, lam_im, b_re, b_im, c_re, c_im, d_skip, log_dt, w_glu):
    f32 = jnp.float32
    bsz, seq, _ = u.shape
    ug = u.astype(f32).reshape(bsz, seq, S5_GROUPS, S5_CH_PER_GROUP)
    lam = lax.complex(jnp.minimum(lam_re.astype(f32), S5_MAX_REAL), lam_im.astype(f32))
    step = jnp.exp(log_dt.astype(f32))[:, None]
    lam_bar = jnp.exp(lam * step)
    b_bar = ((lam_bar - 1.0) / lam)[..., None] * lax.complex(b_re.astype(f32), b_im.astype(f32))
    c_mat = lax.complex(c_re.astype(f32), c_im.astype(f32))
    bu = jnp.einsum('blgh,gph->blgp', ug.astype(jnp.complex64), b_bar)
    a = jnp.broadcast_to(lam_bar, (1, seq, S5_GROUPS, S5_STATE))

    def combine(e1, e2):
        a1, b1 = e1
        a2, b2 = e2
        return a2 * a1, a2 * b1 + b2

    _, states = lax.associative_scan(combine, (a, bu), axis=1)
    y = (jnp.einsum('blgp,ghp->blgh', states, c_mat).real
         + d_skip.astype(f32).reshape(S5_GROUPS, S5_CH_PER_GROUP) * ug)
    y = jax.nn.gelu(y.reshape(bsz, seq, BRANCH_W)).astype(u.dtype)
    return y * jax.nn.sigmoid(y @ w_glu)


def _hgrn2_mixer(q, f, i, g, lower_bound, norm_w):
    bsz, seq, _ = q.shape

    def heads(t, dh):
        return t.reshape(bsz, seq, HG_HEADS, dh).transpose(0, 2, 1, 3)

    q = jax.nn.silu(q)
    log_f = jnp.logaddexp(jnp.log(lower_bound), jnp.log1p(-lower_bound) + jax.nn.log_sigmoid(f.astype(jnp.float32)))
    k = -jnp.expm1(log_f)
    o = _chunk_gated_linear_attention(heads(q, HG_DK), heads(k, HG_DK), heads(i, HG_DV),
                                      heads(log_f, HG_DK), HG_CHUNK)
    o = _rms(o.transpose(0, 2, 1, 3)) * norm_w
    return o.reshape(bsz, seq, BRANCH_W) * jax.nn.silu(g)


def _retention_mixer(q, k, v, g, positions):
    bsz, seq, _ = q.shape
    qh = _rope(q.reshape(bsz, seq, RET_HEADS, RET_DK), positions)
    kh = _rope(k.reshape(bsz, seq, RET_HEADS, RET_DK), positions) * (RET_DK ** -0.5)
    vh = v.reshape(bsz, seq, RET_HEADS, RET_DV)
    log_gamma = jnp.log1p(-jnp.exp2(-5.0 - jnp.arange(RET_HEADS, dtype=jnp.float32)))
    log_f = jnp.broadcast_to(log_gamma[None, :, None, None], (bsz, RET_HEADS, seq, 1))
    o = _chunk_gated_linear_attention(qh.transpose(0, 2, 1, 3), kh.transpose(0, 2, 1, 3),
                                      vh.transpose(0, 2, 1, 3), log_f, RET_CHUNK)
    o = _rms(o.transpose(0, 2, 1, 3)).reshape(bsz, seq, BRANCH_W)
    return o * jax.nn.silu(g)


def _mamba2_mixer(z, xbc, dt_raw, conv_w, conv_b, dt_bias, a_log, d_skip, norm_w):
    bsz, seq, _ = xbc.shape
    xbc = lax.conv_general_dilated(xbc, conv_w[:, None, :], window_strides=(1,),
                                   padding=((M2_CONV - 1, 0),),
                                   dimension_numbers=('NWC', 'WIO', 'NWC'),
                                   feature_group_count=M2_XBC)
    xbc = jax.nn.silu(xbc + conv_b)
    xs, bm, cm = jnp.split(xbc, [BRANCH_W, BRANCH_W + M2_GROUPS * M2_STATE], axis=-1)
    heads_per_group = M2_HEADS // M2_GROUPS
    xs = xs.reshape(bsz, seq, M2_HEADS, M2_HEADDIM)
    bm = jnp.repeat(bm.reshape(bsz, seq, M2_GROUPS, M2_STATE), heads_per_group, axis=2)
    cm = jnp.repeat(cm.reshape(bsz, seq, M2_GROUPS, M2_STATE), heads_per_group, axis=2)
    dt = jax.nn.softplus(dt_raw.astype(jnp.float32) + dt_bias.astype(jnp.float32))
    a = -jnp.exp(a_log.astype(jnp.float32))
    y = _ssd_chunked(xs, dt, a, bm, cm, M2_CHUNK) + d_skip.astype(jnp.float32)[:, None] * xs.astype(jnp.float32)
    y = y.reshape(bsz, seq, BRANCH_W).astype(z.dtype)
    return _rms(y * jax.nn.silu(z)) * norm_w


def _hier_moe(h, w_group, b_group, w_expert, b_expert, w1, w3, w2):
    bsz, seq, d = h.shape
    t = h.reshape(bsz * seq, d)
    g_logits = (t @ w_group + b_group).astype(jnp.float32)
    g_prob = jax.nn.softmax(g_logits, axis=-1)
    g_sel = jnp.argmax(g_logits, axis=-1)
    g_w = jnp.take_along_axis(g_prob, g_sel[:, None], axis=1)
    e_logits = (t @ w_expert + b_expert).astype(jnp.float32).reshape(-1, MOE_GROUPS, MOE_EXPERTS_PER_GROUP)
    e_in_group = jnp.take_along_axis(e_logits, g_sel[:, None, None], axis=1)[:, 0]
    top_v, top_i = lax.top_k(e_in_group, MOE_TOPK)
    top_w = jax.nn.softmax(top_v, axis=-1) * g_w
    comb_e = jnp.sum(jax.nn.one_hot(top_i, MOE_EXPERTS_PER_GROUP, dtype=jnp.float32) * top_w[..., None], axis=1)
    comb = (jax.nn.one_hot(g_sel, MOE_GROUPS, dtype=jnp.float32)[:, :, None] * comb_e[:, None, :]).astype(t.dtype)
    out = jnp.zeros_like(t)
    for grp in range(MOE_GROUPS):
        act = jax.nn.silu(jnp.einsum('td,edf->tef', t, w1[grp])) * jnp.einsum('td,edf->tef', t, w3[grp])
        out = out + jnp.einsum('tef,efd->td', act * comb[:, grp, :, None], w2[grp])
    return out.reshape(bsz, seq, d)


def setup_inputs(seed: int = 0) -> dict:
    key = jax.random.key(seed)
    keys = jax.random.split(key, 48)
    counter = [0]
    f32 = jnp.float32

    def nk():
        counter[0] += 1
        return keys[counter[0] - 1]

    def nrm(shape, scale):
        return scale * jax.random.normal(nk(), shape, f32)

    def unif(shape, lo, hi):
        return jax.random.uniform(nk(), shape, f32, lo, hi)

    L = DEPTH
    D = D_MODEL
    x = nrm((BATCH, SEQ, D), 1.0)
    c = nrm((BATCH, D), 1.0)
    start = jax.random.randint(nk(), (BATCH, 1), 0, MAX_POS_OFFSET, dtype=jnp.int32)
    positions = start + jnp.arange(SEQ, dtype=jnp.int32)[None, :]
    ada_w = nrm((L, D, 6 * D), 0.5 * D ** -0.5)
    ada_b = nrm((L, 6 * D), 0.02)
    w_in = nrm((L, D, IN_W), D ** -0.5)
    s5_lam_re = -0.5 + nrm((L, S5_GROUPS, S5_STATE), 0.01)
    s5_lam_im = np.pi * jnp.arange(S5_STATE, dtype=f32)[None, None, :] + nrm((L, S5_GROUPS, S5_STATE), 0.01)
    s5_b_re = nrm((L, S5_GROUPS, S5_STATE, S5_CH_PER_GROUP), (2 * S5_CH_PER_GROUP) ** -0.5)
    s5_b_im = nrm((L, S5_GROUPS, S5_STATE, S5_CH_PER_GROUP), (2 * S5_CH_PER_GROUP) ** -0.5)
    s5_c_re = nrm((L, S5_GROUPS, S5_CH_PER_GROUP, S5_STATE), (2 * S5_STATE) ** -0.5)
    s5_c_im = nrm((L, S5_GROUPS, S5_CH_PER_GROUP, S5_STATE), (2 * S5_STATE) ** -0.5)
    s5_d = nrm((L, BRANCH_W), 1.0)
    s5_log_dt = unif((L, S5_GROUPS), float(np.log(1e-3)), float(np.log(1e-1)))
    s5_w_glu = nrm((L, BRANCH_W, BRANCH_W), BRANCH_W ** -0.5)
    hg_lb_logits = nrm((L, HG_HEADS * HG_DK), 0.5)
    hg_norm_w = 1.0 + nrm((L, HG_DV), 0.01)
    m2_conv_w = nrm((L, M2_CONV, M2_XBC), M2_CONV ** -0.5)
    m2_conv_b = nrm((L, M2_XBC), 0.01)
    dt0 = jnp.exp(unif((L, M2_HEADS), float(np.log(1e-3)), float(np.log(1e-1))))
    m2_dt_bias = dt0 + jnp.log(-jnp.expm1(-dt0))
    m2_a_log = jnp.log(unif((L, M2_HEADS), 1.0, 16.0))
    m2_d = 1.0 + nrm((L, M2_HEADS), 0.01)
    m2_norm_w = 1.0 + nrm((L, BRANCH_W), 0.01)
    w_branch = nrm((L, N_BRANCH, BRANCH_W, D), BRANCH_W ** -0.5)
    w_gate = nrm((L, D, N_BRANCH * D), D ** -0.5)
    b_gate = nrm((L, N_BRANCH * D), 0.01)
    w_out = nrm((L, D, D), D ** -0.5)
    moe_w_group = nrm((L, D, MOE_GROUPS), D ** -0.5)
    moe_b_group = nrm((L, MOE_GROUPS), 0.01)
    moe_w_expert = nrm((L, D, MOE_GROUPS * MOE_EXPERTS_PER_GROUP), D ** -0.5)
    moe_b_expert = nrm((L, MOE_GROUPS * MOE_EXPERTS_PER_GROUP), 0.01)
    moe_w1 = nrm((L, MOE_GROUPS, MOE_EXPERTS_PER_GROUP, D, MOE_FF), D ** -0.5)
    moe_w3 = nrm((L, MOE_GROUPS, MOE_EXPERTS_PER_GROUP, D, MOE_FF), D ** -0.5)
    moe_w2 = nrm((L, MOE_GROUPS, MOE_EXPERTS_PER_GROUP, MOE_FF, D), MOE_FF ** -0.5)
    final_norm_w = 1.0 + nrm((D,), 0.01)
    return {'x': x, 'c': c, 'positions': positions, 'ada_w': ada_w, 'ada_b': ada_b, 'w_in': w_in,
            's5_lam_re': s5_lam_re, 's5_lam_im': s5_lam_im, 's5_b_re': s5_b_re, 's5_b_im': s5_b_im,
            's5_c_re': s5_c_re, 's5_c_im': s5_c_im, 's5_d': s5_d, 's5_log_dt': s5_log_dt, 's5_w_glu': s5_w_glu,
            'hg_lb_logits': hg_lb_logits, 'hg_norm_w': hg_norm_w,
            'm2_conv_w': m2_conv_w, 'm2_conv_b': m2_conv_b, 'm2_dt_bias': m2_dt_bias, 'm2_a_log': m2_a_log,
            'm2_d': m2_d, 'm2_norm_w': m2_norm_w,
            'w_branch': w_branch, 'w_gate': w_gate, 'b_gate': b_gate, 'w_out': w_out,
            'moe_w_group': moe_w_group, 'moe_b_group': moe_b_group, 'moe_w_expert': moe_w_expert,
            'moe_b_expert': moe_b_expert, 'moe_w1': moe_w1, 'moe_w3': moe_w3, 'moe_w2': moe_w2,
            'final_norm_w': final_norm_w}


def reference(x, c, positions, ada_w, ada_b, w_in, s5_lam_re, s5_lam_im, s5_b_re, s5_b_im, s5_c_re, s5_c_im,
              s5_d, s5_log_dt, s5_w_glu, hg_lb_logits, hg_norm_w, m2_conv_w, m2_conv_b, m2_dt_bias, m2_a_log,
              m2_d, m2_norm_w, w_branch, w_gate, b_gate, w_out, moe_w_group, moe_b_group, moe_w_expert,
              moe_b_expert, moe_w1, moe_w3, moe_w2, final_norm_w):
    bsz, seq, d = x.shape
    split_at = []
    acc = 0
    for size in IN_SIZES[:-1]:
        acc += size
        split_at.append(acc)
    lb_cum = jnp.cumsum(jax.nn.softmax(hg_lb_logits.astype(jnp.float32), axis=0), axis=0)
    hg_lower_bounds = lb_cum - lb_cum[:1]
    cond = jax.nn.silu(c)
    for layer in range(DEPTH):
        mod = (cond @ ada_w[layer] + ada_b[layer]).reshape(bsz, 6, 1, d)
        shift_mix, scale_mix, gate_mix, shift_ffn, scale_ffn, gate_ffn = (mod[:, j] for j in range(6))
        h = _rms(x) * (1.0 + scale_mix) + shift_mix
        (s5_u, hg_q, hg_f, hg_i, hg_g, ret_q, ret_k, ret_v, ret_g,
         m2_z, m2_xbc, m2_dt) = jnp.split(h @ w_in[layer], split_at, axis=-1)
        y_s5 = _s5_mixer(s5_u, s5_lam_re[layer], s5_lam_im[layer], s5_b_re[layer], s5_b_im[layer],
                         s5_c_re[layer], s5_c_im[layer], s5_d[layer], s5_log_dt[layer], s5_w_glu[layer])
        y_hg = _hgrn2_mixer(hg_q, hg_f, hg_i, hg_g, hg_lower_bounds[layer], hg_norm_w[layer])
        y_ret = _retention_mixer(ret_q, ret_k, ret_v, ret_g, positions)
        y_m2 = _mamba2_mixer(m2_z, m2_xbc, m2_dt, m2_conv_w[layer], m2_conv_b[layer], m2_dt_bias[layer],
                             m2_a_log[layer], m2_d[layer], m2_norm_w[layer])
        branches = jnp.einsum('blnw,nwd->blnd', jnp.stack([y_s5, y_hg, y_ret, y_m2], axis=2), w_branch[layer])
        gates = jax.nn.sigmoid(h @ w_gate[layer] + b_gate[layer]).reshape(bsz, seq, N_BRANCH, d)
        x = x + gate_mix * (jnp.sum(gates * branches, axis=2) @ w_out[layer])
        h = _rms(x) * (1.0 + scale_ffn) + shift_ffn
        x = x + gate_ffn * _hier_moe(h, moe_w_group[layer], moe_b_group[layer], moe_w_expert[layer],
                                     moe_b_expert[layer], moe_w1[layer], moe_w3[layer], moe_w2[layer])
    return _rms(x) * final_norm_w
```

```python
import numpy as np
from contextlib import ExitStack
import concourse.bass as bass
import concourse.mybir as mybir
from concourse.bass_utils import run_bass_kernel_spmd

F32 = mybir.dt.float32
BF16 = mybir.dt.bfloat16
I32 = mybir.dt.int32
AF = mybir.ActivationFunctionType
ALU = mybir.AluOpType
AX = mybir.AxisListType

D = 1024
SEQ = 4096
DEPTH = 2
NT = SEQ // 512
EPS = 1e-6
TWO_PI = 6.283185307179586
SIN_SCALE = 6.28318


class Buf:
    __slots__ = ("name", "w", "r", "excl")

    def __init__(self, name=""):
        self.name = name
        self.w = None
        self.r = []
        self.excl = False


class T:
    __slots__ = ("t", "b")

    def __init__(self, t, name=""):
        self.t = t
        self.b = Buf(name)

    def __getitem__(self, k):
        return self.t[k]


class KB:
    NDMA = 48

    def __init__(self, nc, es):
        self.nc = nc
        self.es = es
        self.engs = {"pe": nc.tensor, "act": nc.scalar, "dve": nc.vector,
                     "pool": nc.gpsimd, "sp": nc.sync}
        self.done = {e: es.enter_context(nc.semaphore("done_" + e)) for e in self.engs}
        self.cnt = {e: 0 for e in self.engs}
        self.waited = {e: {} for e in self.engs}
        self.pending = {e: [] for e in self.engs}
        self.dma_sems = [es.enter_context(nc.semaphore("dma%d" % i)) for i in range(self.NDMA)]
        self.dma_cnt = [0] * self.NDMA
        self.next_dma = 0
        self.sw_sems = [es.enter_context(nc.semaphore("swd%d" % i)) for i in range(8)]
        self.sw_cnt = [0] * 8
        self.next_sw = 0
        self.bg_sems = [es.enter_context(nc.semaphore("bg%d" % i)) for i in range(8)]
        self.bg_cnt = [0] * 8
        self.next_bg = 0
        self.ninst = {e: 0 for e in self.engs}
        self.uid = 0

    def sb(self, name, shape, dtype, es=None):
        self.uid += 1
        es = es or self.es
        return T(es.enter_context(self.nc.sbuf_tensor("%s_%d" % (name, self.uid), list(shape), dtype)), name)

    def ps(self, name, shape, dtype, es=None):
        self.uid += 1
        es = es or self.es
        t = T(es.enter_context(self.nc.psum_tensor("%s_%d" % (name, self.uid), list(shape), dtype)), name)
        t.b.excl = True
        return t

    def _wait(self, eng, deps):
        w = self.waited[eng]
        best = {}
        for d in deps:
            if d is None:
                continue
            sem, val = d
            if eng == "pe" and sem is self.done["pe"]:
                continue
            k = id(sem)
            if w.get(k, 0) >= val:
                continue
            if k not in best or best[k][1] < val:
                best[k] = (sem, val)
        for k, (sem, val) in best.items():
            self.engs[eng].wait_ge(sem, val)
            self.ninst[eng] += 1
            w[k] = val

    @staticmethod
    def _bufs(xs):
        return [x.b if isinstance(x, T) else x for x in xs]

    def _deps(self, reads, writes):
        deps = []
        for b in reads:
            if b.w is not None:
                deps.append(b.w)
            if b.excl:
                deps.extend(b.r)
        for b in writes:
            if b.w is not None:
                deps.append(b.w)
            deps.extend(b.r)
        return deps

    @staticmethod
    def _prune(evs):
        best = {}
        for sem, val in evs:
            k = id(sem)
            if k not in best or best[k][1] < val:
                best[k] = (sem, val)
        return list(best.values())

    def _record(self, ev, reads, writes):
        for b in reads:
            b.r.append(ev)
            if len(b.r) > 16:
                b.r = self._prune(b.r)
        for b in writes:
            b.w = ev
            b.r = []

    def op(self, eng, fn, reads=(), writes=(), sig=True):
        reads = self._bufs(reads)
        writes = self._bufs(writes)
        self._wait(eng, self._deps(reads, writes))
        inst = fn(self.engs[eng])
        self.ninst[eng] += 1
        if not sig:
            self.pending[eng].append((reads, writes))
            return inst
        self.cnt[eng] += 1
        inst.then_inc(self.done[eng], 1)
        ev = (self.done[eng], self.cnt[eng])
        for rs, ws in self.pending[eng]:
            self._record(ev, rs, ws)
        self.pending[eng] = []
        self._record(ev, reads, writes)
        return inst

    def dma(self, q, out, in_, reads=(), writes=(), bg=False, **kw):
        reads = self._bufs(reads)
        writes = self._bufs(writes)
        self._wait(q, self._deps(reads, writes))
        if bg:
            i = self.next_bg
            self.next_bg = (i + 1) % len(self.bg_sems)
            s = self.bg_sems[i]
            cnts = self.bg_cnt
        elif q == "pool":
            i = self.next_sw
            self.next_sw = (i + 1) % len(self.sw_sems)
            s = self.sw_sems[i]
            cnts = self.sw_cnt
        else:
            i = self.next_dma
            self.next_dma = (i + 1) % self.NDMA
            s = self.dma_sems[i]
            cnts = self.dma_cnt
        if cnts[i] > 0:
            self._wait(q, [(s, 16 * cnts[i])])
        inst = self.engs[q].dma_start(out=out, in_=in_, **kw)
        self.ninst[q] += 1
        cnts[i] += 1
        inst.then_inc(s, 16)
        ev = (s, 16 * cnts[i])
        self._record(ev, reads, writes)
        return ev

    def all_events(self):
        evs = [(self.done[e], self.cnt[e]) for e in self.engs if self.cnt[e]]
        evs += [(self.dma_sems[i], 16 * self.dma_cnt[i]) for i in range(self.NDMA) if self.dma_cnt[i]]
        evs += [(self.sw_sems[i], 16 * self.sw_cnt[i]) for i in range(len(self.sw_sems)) if self.sw_cnt[i]]
        return evs

    def barrier(self):
        for e in self.engs:
            assert not self.pending[e], "pending unsignaled ops at barrier"
        evs = self.all_events()
        for e in self.engs:
            self._wait(e, evs)

    def act(self, out, in_, func, reads, writes, bias=None, scale=None, accum_out=None):
        kw = {}
        if bias is not None:
            kw["bias"] = bias
        if scale is not None:
            kw["scale"] = scale
        if accum_out is not None:
            kw["accum_out"] = accum_out
        return self.op("act", lambda e: e.activation(out, in_, func, **kw), reads, writes)

    def tt(self, eng, out, in0, in1, op, reads, writes):
        return self.op(eng, lambda e: e.tensor_tensor(out, in0, in1, op), reads, writes)

    def ts(self, eng, out, in0, s1, s2, op0, op1, reads, writes):
        if s2 is None:
            return self.op(eng, lambda e: e.tensor_scalar(out, in0, s1, None, op0), reads, writes)
        return self.op(eng, lambda e: e.tensor_scalar(out, in0, s1, s2, op0, op1), reads, writes)

    def stt(self, eng, out, in0, scalar, in1, op0, op1, reads, writes):
        return self.op(eng, lambda e: e.scalar_tensor_tensor(out, in0, scalar, in1, op0, op1), reads, writes)

    def copy(self, eng, out, in_, reads, writes):
        if eng == "act":
            return self.act(out, in_, AF.Copy, reads, writes)
        return self.op(eng, lambda e: e.tensor_copy(out, in_), reads, writes)

    def mm(self, out, lhsT, rhs, start, stop, reads, writes, sig=None):
        if sig is None:
            sig = stop
        return self.op("pe", lambda e: e.matmul(out, lhsT, rhs, start=start, stop=stop), reads, writes, sig=sig)

    def tr(self, out, in_, ident, reads, writes, sig=True):
        return self.op("pe", lambda e: e.transpose(out, in_, ident), reads, writes, sig=sig)

    def memset(self, eng, ap, val, writes):
        return self.op(eng, lambda e: e.memset(ap, val), (), writes)


O_U, O_HQ, O_HF, O_HI, O_HG = 0, 512, 1024, 1536, 2048
O_RQ, O_RK, O_RV, O_RG = 2560, 2816, 3072, 3584
O_MZ, O_XBC, O_DT = 4096, 4608, 5632

FM_KIND = (["copy"] * 4 + ["silu"] * 4 + ["f"] * 4 + ["silu"] * 4 +
           ["ropeA", "ropeA", "ropeB", "ropeB", "ropeA", "ropeA", "ropeB", "ropeB"] +
           ["silu"] * 4 + ["conv"] * 8)
NFM = len(FM_KIND)


def _fm_cols():
    cols = []
    cols += list(range(O_U, O_U + 512))
    cols += list(range(O_HQ, O_HQ + 512))
    cols += list(range(O_HF, O_HF + 512))
    cols += list(range(O_HG, O_HG + 512))

    def swapped(base):
        out = []
        for h in range(4):
            out += list(range(base + h * 64 + 32, base + h * 64 + 64))
            out += list(range(base + h * 64, base + h * 64 + 32))
        return out
    cols += list(range(O_RQ, O_RQ + 256)) + swapped(O_RQ)
    cols += list(range(O_RK, O_RK + 256)) + swapped(O_RK)
    cols += list(range(O_RG, O_RG + 512))
    cols += list(range(O_XBC, O_XBC + 1024))
    return np.array(cols, dtype=np.int64)


def _tm_cols():
    cols = list(range(O_HI, O_HI + 512)) + list(range(O_RV, O_RV + 512)) + list(range(O_MZ, O_MZ + 512))
    cols += list(range(O_DT, O_DT + 8))
    return np.array(cols, dtype=np.int64)


def _const_tables():
    c = {}
    c["ident"] = np.eye(128, dtype=np.float32)
    j = np.arange(128) % 64
    inv_freq = 10000.0 ** (-(j % 32).astype(np.float64) / 32.0)
    rope = np.zeros((128, 2), np.float32)
    rope[:, 0] = (inv_freq / TWO_PI).astype(np.float32)
    rope[:, 1] = np.where(j < 32, -1.0, 1.0)
    c["rope_c"] = rope
    t = np.arange(512)
    c["reset64"] = np.tile((t % 64 != 0).astype(np.float32)[None, :], (128, 1))
    sidx = np.arange(64)[:, None]
    tidx = (t % 64)[None, :]
    c["cmask64"] = (sidx <= tidx).astype(np.float32)
    c["hm64"] = np.tile(((t % 64) < 32).astype(np.float32)[None, :], (128, 1))
    gam = 1.0 - 2.0 ** (-5.0 - np.arange(4))
    r = (t % 64).astype(np.float64)
    c["ret_gq"] = np.stack([np.tile((g ** (r + 1))[None, :], (64, 1)) for g in gam]).astype(np.float32)
    dlt = (tidx - sidx).astype(np.float64)
    c["ret_mask"] = np.stack([np.where(dlt >= 0, g ** np.maximum(dlt, 0) * 0.125, 0.0) for g in gam]).astype(np.float32)
    c["iota1"] = np.tile((np.arange(512, dtype=np.float32) + 1.0)[None, :], (128, 1))
    cm = np.zeros((4, 128, 128), np.float32)
    for q in range(4):
        for g2 in range(2):
            g8 = 2 * q + g2
            cm[q, g2 * 64:(g2 + 1) * 64, g8 * 16:(g8 + 1) * 16] = 1.0
    c["s5_cmask"] = cm
    sel = np.zeros((32, 32, 128), np.float32)
    for e in range(32):
        sel[e, e, :] = 1.0
    c["moe_sel"] = sel
    r128 = (t % 128).astype(np.float64)
    c["ret_gq128"] = np.stack([np.tile((g ** (r128 + 1))[None, :], (64, 1)) for g in gam]).astype(np.float32)
    s1 = np.arange(128)[:, None]
    dl = ((t % 128)[None, :] - s1).astype(np.float64)
    c["ret_mask128"] = np.stack([np.where(dl >= 0, g ** np.maximum(dl, 0) * 0.125, 0.0) for g in gam]).astype(np.float32)
    c["ret_gk128"] = np.stack([g ** (127.0 - np.arange(128)) * 0.125 for g in gam], axis=1).astype(np.float32)
    c["negm64"] = ((c["cmask64"] - 1.0) * 30000.0).astype(np.float32)
    s128 = np.arange(128)[:, None]
    t128 = (np.arange(1024) % 128)[None, :]
    c["cmask128"] = (s128 <= t128).astype(np.float32)
    c["negm128"] = ((c["cmask128"] - 1.0) * 30000.0).astype(np.float32)
    c["ret_gk"] = np.stack([g ** (63.0 - np.arange(64)) * 0.125 for g in gam], axis=1).astype(np.float32)
    return c


class Prog:
    def __init__(self, debug=None, upto="all"):
        self.debug = debug or []
        self.upto = upto
        self.nc = bass.Bass("TRN2", target_bir_lowering=False)
        self.dram = {}
        self.dbuf = {}

    def din(self, name, shape, dtype=F32):
        self.dram[name] = self.nc.dram_tensor(name, list(shape), dtype, kind="ExternalInput").ap()
        self.dbuf[name] = Buf(name)
        return self.dram[name]

    def dscr(self, name, shape, dtype):
        kind = "ExternalOutput" if name in self.debug else "Internal"
        self.dram[name] = self.nc.dram_tensor(name, list(shape), dtype, kind=kind).ap()
        self.dbuf[name] = Buf(name)
        return self.dram[name]

    def dout(self, name, shape, dtype=F32):
        self.dram[name] = self.nc.dram_tensor(name, list(shape), dtype, kind="ExternalOutput").ap()
        self.dbuf[name] = Buf(name)
        return self.dram[name]

    def declare(self):
        L = DEPTH
        self.din("xT", [D, SEQ])
        self.din("c_pk", [128, 8])
        self.din("pos", [1, SEQ], I32)
        self.din("ada_w", [L, D, 6 * D])
        self.din("ada_b_pk", [L, 128, 48])
        self.din("w_fm", [L, D, NFM * 128])
        self.din("w_tm", [L, D, 1544])
        self.din("hg_lb_pk", [128, L, 4])
        self.din("conv_w_pk", [L, 128, 8, 4])
        self.din("conv_b_pk", [L, 128, 8])
        self.din("dt_bias", [L, 1, 8])
        self.din("ident", [128, 128])
        self.din("rope_c", [128, 2])
        self.din("reset64", [128, 512])
        self.din("cmask64", [64, 512])
        self.din("hm64", [128, 512])
        self.din("hg_norm_pk", [128, DEPTH])
        self.din("ret_gq", [4, 64, 512])
        self.din("ret_mask", [4, 64, 512])
        self.din("ret_gk", [64, 4])
        self.din("ret_gq128", [4, 64, 512])
        self.din("ret_mask128", [4, 128, 512])
        self.din("ret_gk128", [128, 4])
        self.din("negm64", [64, 512])
        self.din("cmask128", [128, 1024])
        self.din("negm128", [128, 1024])
        self.din("iota1", [128, 512])
        self.din("s5_cmask", [4, 128, 128])
        self.din("s5_lr_pk", [DEPTH, 128, 16])
        self.din("s5_li_pk", [DEPTH, 128, 16])
        self.din("s5_ldt_pk", [DEPTH, 128, 16])
        self.din("s5_bre_pk", [DEPTH, 128, 16, 16])
        self.din("s5_bim_pk", [DEPTH, 128, 16, 16])
        self.din("s5_cre_pk", [DEPTH, 4, 128, 64])
        self.din("s5_cim_pk", [DEPTH, 4, 128, 64])
        self.din("s5_d_pk", [DEPTH, 128, 4])
        self.din("s5_wglu", [DEPTH, 512, 512])
        self.din("w_gate", [DEPTH, D, 4 * D])
        self.din("b_gate_pk", [DEPTH, 128, 32])
        self.din("w_branch", [DEPTH, 4 * 512, D])
        self.din("w_out", [DEPTH, D, D])
        self.din("moe_wr", [DEPTH, D, 36])
        self.din("moe_br", [DEPTH, 1, 36])
        self.din("moe_w13_pk", [DEPTH, 32, 128, 8 * 512])
        self.din("moe_w2_pk", [DEPTH, 4, 128, 16 * D])
        self.dscr("MW13", [DEPTH, 32, 128, 8 * 512], BF16)
        self.dscr("MW2", [DEPTH, 4, 128, 16 * D], BF16)
        self.dscr("CMB", [2, 32, 1024], F32)
        self.din("moe_sel", [32, 32, 128])
        self.din("fnorm_pk", [128, 8])
        self.din("m2_alog", [DEPTH, 1, 8])
        self.din("m2_d", [DEPTH, 1, 8])
        self.din("m2_normw", [DEPTH, 1, 512])
        self.dout("outT", [D, SEQ])
        self.dscr("UT", [512, SEQ], BF16)
        self.dscr("HGQ", [512, SEQ], BF16)
        self.dscr("HGLF", [512, SEQ], F32)
        self.dscr("HGK", [512, SEQ], BF16)
        self.dscr("HGG", [512, SEQ], BF16)
        self.dscr("RQ", [256, SEQ], BF16)
        self.dscr("RK", [256, SEQ], BF16)
        self.dscr("RG", [512, SEQ], BF16)
        self.dscr("XBC", [1024, SEQ], BF16)
        self.dscr("HGI", [SEQ, 512], BF16)
        self.dscr("RV", [SEQ, 512], BF16)
        self.dscr("MZ", [SEQ, 512], BF16)
        self.dscr("DT", [SEQ, 8], F32)
        self.dscr("MOD", [128, 2 * 48], F32)
        self.dscr("YB", [4 * 512, SEQ], BF16)
        self.dscr("XS", [D, SEQ], F32)

    def build(self):
        nc = self.nc
        self.declare()
        with ExitStack() as es:
            kb = self.kb = KB(nc, es)
            self.es = es
            self.consts()
            self.prologue()
            for layer in range(DEPTH):
                def ph(name, fn):
                    with self.nc.named_scope("%s_L%d" % (name, layer)):
                        fn(layer)
                ph("A", self.phase_a)
                if self.upto == "S5":
                    ph("S5", self.phase_s5)
                    break
                if self.upto == "SSD":
                    ph("SSD", self.phase_ssd)
                    break
                if self.upto == "RET":
                    ph("RET", self.phase_ret)
                    break
                if self.upto == "HG":
                    ph("HG", self.phase_hg)
                    break
                ph("HG", self.phase_hg)
                ph("RET", self.phase_ret)
                ph("SSD", self.phase_ssd)
                ph("S5", self.phase_s5)
                ph("C", self.phase_c)
                ph("MOE", self.phase_moe)
            if self.upto == "all":
                with self.nc.named_scope("FINAL"):
                    self.phase_final()
            self.finish()
        return nc

    def finish(self):
        kb = self.kb
        kb.barrier()
        kb._wait("sp", [(kb.bg_sems[i], 16 * kb.bg_cnt[i]) for i in range(len(kb.bg_sems)) if kb.bg_cnt[i]])
        print("[kernel] instruction counts", kb.ninst, flush=True)

    def consts(self):
        kb = self.kb
        dr, db = self.dram, self.dbuf
        self.identf = kb.sb("identf", [128, 128], F32)
        self.identb = kb.sb("identb", [128, 128], BF16)
        kb.dma("sp", self.identf[:], dr["ident"], reads=[db["ident"]], writes=[self.identf])
        kb.copy("dve", self.identb[:], self.identf[:], [self.identf], [self.identb])
        self.ones_d = kb.sb("ones_d", [128, 128], BF16)
        kb.memset("dve", self.ones_d[:], 1.0 / 1024.0, [self.ones_d])
        self.ones_df = kb.sb("ones_df", [128, 128], F32)
        kb.memset("dve", self.ones_df[:], 1.0 / 1024.0, [self.ones_df])
        self.ones_h = kb.sb("ones_h", [128, 128], BF16)
        kb.memset("dve", self.ones_h[:], 1.0 / 128.0, [self.ones_h])
        self.ropec = kb.sb("ropec", [128, 2], F32)
        kb.dma("sp", self.ropec[:], dr["rope_c"], reads=[db["rope_c"]], writes=[self.ropec])
        self.reset64 = kb.sb("reset64", [128, 512], F32)
        kb.dma("sp", self.reset64[:], dr["reset64"], reads=[db["reset64"]], writes=[self.reset64])
        self.hm64 = kb.sb("hm64", [128, 512], F32)
        kb.dma("sp", self.hm64[:], dr["hm64"], reads=[db["hm64"]], writes=[self.hm64])
        self.cmask64 = kb.sb("cmask64", [64, 512], F32)
        kb.dma("sp", self.cmask64[:], dr["cmask64"], reads=[db["cmask64"]], writes=[self.cmask64])
        self.hgnorm = kb.sb("hgnorm", [128, DEPTH], F32)
        kb.dma("sp", self.hgnorm[:], dr["hg_norm_pk"], reads=[db["hg_norm_pk"]], writes=[self.hgnorm])
        self.mod = kb.sb("mod", [128, 2, 48], F32)
        self.ops = kb.sb("ops", [128, 2, 16], F32)
        self.psum = [kb.ps("ps%d" % i, [128, 512], F32) for i in range(8)]
        self.ps_i = 0

    def bank(self):
        p = self.psum[self.ps_i]
        self.ps_i = (self.ps_i + 1) % 8
        return p

    def prologue(self):
        kb = self.kb
        dr, db = self.dram, self.dbuf
        with ExitStack() as es:
            c_f = kb.sb("c_f", [128, 8], F32, es)
            cond = kb.sb("cond", [128, 8], BF16, es)
            adab = kb.sb("adab", [128, 2, 48], F32, es)
            wbuf = [kb.sb("adaw%d" % i, [128, 8, 512], BF16, es) for i in range(2)]
            kb.dma("sp", c_f[:], dr["c_pk"], reads=[db["c_pk"]], writes=[c_f])
            kb.dma("sp", adab[:], dr["ada_b_pk"].rearrange("l p j -> p l j"), reads=[db["ada_b_pk"]], writes=[adab])
            kb.act(cond[:], c_f[:], AF.Silu, [c_f], [cond])
            for l in range(DEPTH):
                ps = self.bank()
                for g in range(12):
                    w = wbuf[g % 2]
                    kb.dma("pool", w[:], dr["ada_w"][l, :, g * 512:(g + 1) * 512].rearrange("(k p) n -> p k n", p=128),
                           reads=[db["ada_w"]], writes=[w])
                    for bi in range(4):
                        col = g * 4 + bi
                        for k in range(8):
                            kb.mm(ps[:, col:col + 1], w[:, k, bi * 128:(bi + 1) * 128], cond[:, k:k + 1],
                                  start=(k == 0), stop=(k == 7), reads=[w, cond], writes=[ps],
                                  sig=(k == 7 and bi == 3))
                kb.tt("dve", self.mod[:, l, :], ps[:, 0:48], adab[:, l, :], ALU.add, [ps, adab], [self.mod])
                kb.ts("dve", self.ops[:, l, 0:8], self.mod[:, l, 8:16], 1.0, None, ALU.add, None, [self.mod], [self.ops])
                kb.ts("dve", self.ops[:, l, 8:16], self.mod[:, l, 32:40], 1.0, None, ALU.add, None, [self.mod], [self.ops])
            if "MOD" in self.debug:
                kb.dma("sp", dr["MOD"], self.mod[:].rearrange("p l j -> p (l j)"), reads=[self.mod], writes=[db["MOD"]])
            kb.barrier()

    def h_stats(self, x_t, sq, rstd, W=512, fp32_sq=False):
        kb = self.kb
        ps = self.bank()
        kb.act(sq[:], x_t[:], AF.Square, [x_t], [sq])
        ones = self.ones_df if fp32_sq else self.ones_d
        for k in range(8):
            kb.mm(ps[:, 0:W], ones[:], sq[:, k, :], start=(k == 0), stop=(k == 7),
                  reads=[ones, sq], writes=[ps])
        kb.act(rstd[:], ps[:, 0:W], AF.Ln, [ps], [rstd], bias=EPS)
        kb.act(rstd[:], rstd[:], AF.Exp, [rstd], [rstd], scale=-0.5)

    def h_apply(self, x_t, l, which, rstd, tmp):
        kb = self.kb
        so = 0 if which == 0 else 8
        for k in range(8):
            kb.stt("dve", tmp[:, k, :], x_t[:, k, :], self.ops[:, l, so + k:so + k + 1], rstd[:],
                   ALU.mult, ALU.mult, [x_t, self.ops, rstd], [tmp])

    def h_tile(self, x_t, h_out_ap_fn, l, which, sq, rstd, tmp, W=512, fp32_sq=False):
        self.h_stats(x_t, sq, rstd, W=W, fp32_sq=fp32_sq)
        self.h_apply(x_t, l, which, rstd, tmp)

    def phase_a(self, l):
        kb = self.kb
        dr, db = self.dram, self.dbuf
        xsrc = "xT" if l == 0 else "XS"
        with ExitStack() as es:
            hT = kb.sb("hT", [128, 8, SEQ], BF16, es)
            hTt = []
            for t in range(NT):
                v = T(hT.t[:, :, t * 512:(t + 1) * 512], "hT%d" % t)
                hTt.append(v)
            WA = 256
            xt = [kb.sb("xt%d" % i, [128, 8, WA], F32, es) for i in range(2)]
            sqs = [kb.sb("sq%d" % i, [128, 8, WA], BF16, es) for i in range(2)]
            o32 = kb.sb("o32", [128, SEQ + 4], F32, es)
            tmps = []
            for i in range(2):
                v = T(o32.t[:, 4 + i * 2048:4 + (i + 1) * 2048].rearrange("p (k n) -> p k n", n=WA), "tmpv%d" % i)
                tmps.append(v)
            rstds = [kb.sb("rstd%d" % i, [128, WA], F32, es) for i in range(2)]
            NA = SEQ // WA

            def a_stats(t):
                kb.dma("sp", xt[t % 2][:], dr[xsrc][:, t * WA:(t + 1) * WA].rearrange("(k p) n -> p k n", p=128),
                       reads=[db[xsrc]], writes=[xt[t % 2]])
                self.h_stats(xt[t % 2], sqs[t % 2], rstds[t % 2], W=WA)
            a_stats(0)
            for t in range(NA):
                if t + 1 < NA:
                    a_stats(t + 1)
                x_t = xt[t % 2]
                tmp = tmps[t % 2]
                self.h_apply(x_t, l, 0, rstds[t % 2], tmp)
                hv = hTt[t // 2]
                for k in range(8):
                    kb.act(hT[:, k, t * WA:(t + 1) * WA], tmp[:, k, :], AF.Identity, [tmp, self.mod], [hv],
                           bias=self.mod[:, l, k:k + 1])
            for v in tmps:
                o32.b.r.extend(v.b.r)
                if v.b.w is not None:
                    o32.b.r.append(v.b.w)
            cosT = kb.sb("cosT", [128, SEQ], F32, es)
            sinT = kb.sb("sinT", [128, SEQ], F32, es)
            lbc = kb.sb("lbc", [128, 4], F32, es)
            oml = kb.sb("oml", [128, 4], F32, es)
            convw = kb.sb("convw", [128, 8, 4], F32, es)
            convb = kb.sb("convb", [128, 8], F32, es)
            dtb = kb.sb("dtb", [128, 8], F32, es)
            CH = 512
            posi = kb.sb("posi", [128, CH], I32, es)
            ang = kb.sb("ang", [128, CH], F32, es)
            ki = kb.sb("ki", [128, CH], I32, es)
            kf = kb.sb("kf", [128, CH], F32, es)
            lbl = kb.sb("lbl", [128, 2, 4], F32, es)
            for c4 in range(SEQ // CH):
                csl = slice(c4 * CH, (c4 + 1) * CH)
                kb.dma("sp", posi[:], dr["pos"][:, csl].partition_broadcast(128), reads=[db["pos"]], writes=[posi])
                kb.copy("dve", kf[:], posi[:], [posi], [kf])
                kb.ts("dve", ang[:], kf[:], self.ropec[:, 0:1], None, ALU.mult, None, [kf, self.ropec], [ang])
                kb.copy("dve", ki[:], ang[:], [ang], [ki])
                kb.copy("dve", kf[:], ki[:], [ki], [kf])
                kb.tt("dve", kf[:], ang[:], kf[:], ALU.subtract, [ang, kf], [kf])
                kb.act(sinT[:, csl], kf[:], AF.Sin, [kf], [sinT], scale=SIN_SCALE)
                kb.ts("dve", sinT[:, csl], sinT[:, csl], self.ropec[:, 1:2], None, ALU.mult, None, [sinT, self.ropec], [sinT])
                kb.ts("dve", ang[:], ang[:], 0.25, None, ALU.add, None, [ang], [ang])
                kb.copy("dve", ki[:], ang[:], [ang], [ki])
                kb.copy("dve", kf[:], ki[:], [ki], [kf])
                kb.tt("dve", kf[:], ang[:], kf[:], ALU.subtract, [ang, kf], [kf])
                kb.act(cosT[:, csl], kf[:], AF.Sin, [kf], [cosT], scale=SIN_SCALE)
            kb.dma("sp", lbl[:], dr["hg_lb_pk"], reads=[db["hg_lb_pk"]], writes=[lbl])
            if l == 0:
                kb.memset("dve", lbc[:], 0.0, [lbc])
            else:
                kb.tt("dve", lbc[:], lbl[:, 1, :], lbl[:, 0, :], ALU.subtract, [lbl], [lbc])
                kb.act(lbc[:], lbc[:], AF.Sigmoid, [lbc], [lbc])
            kb.ts("dve", oml[:], lbc[:], -1.0, 1.0, ALU.mult, ALU.add, [lbc], [oml])
            kb.dma("sp", convw[:], dr["conv_w_pk"][l], reads=[db["conv_w_pk"]], writes=[convw])
            kb.dma("sp", convb[:], dr["conv_b_pk"][l], reads=[db["conv_b_pk"]], writes=[convb])
            kb.dma("sp", dtb[:], dr["dt_bias"][l].partition_broadcast(128), reads=[db["dt_bias"]], writes=[dtb])
            wb = [kb.sb("wfm%d" % i, [128, 8, 512], BF16, es) for i in range(2)]
            o16 = [kb.sb("o16_%d" % i, [128, SEQ], BF16, es) for i in range(2)]
            k16 = kb.sb("k16", [128, SEQ], BF16, es)
            t1 = [kb.sb("t1_%d" % i, [128, 512], F32, es) for i in range(2)]
            t2 = [kb.sb("t2_%d" % i, [128, 512], F32, es) for i in range(2)]
            kb.memset("dve", o32[:, 0:4], 0.0, [o32])
            dests = (["UT"] * 4 + ["HGQ"] * 4 + ["HGLF"] * 4 + ["HGG"] * 4 +
                     ["RQ", "RQ", None, None, "RK", "RK", None, None] + ["RG"] * 4 + ["XBC"] * 8)
            drow = ([0, 1, 2, 3] * 4 + [0, 1, 0, 0, 0, 1, 0, 0] + [0, 1, 2, 3] + list(range(8)))
            oi = 0
            ti = 0
            for g in range(NFM // 4):
                w = wb[g % 2]
                kb.dma("pool", w[:], dr["w_fm"][l, :, g * 512:(g + 1) * 512].rearrange("(k p) n -> p k n", p=128),
                       reads=[db["w_fm"]], writes=[w])

                def mm_block(bi, t):
                    ps = self.bank()
                    for k in range(8):
                        kb.mm(ps[:], w[:, k, bi * 128:(bi + 1) * 128], hT[:, k, t * 512:(t + 1) * 512],
                              start=(k == 0), stop=(k == 7), reads=[w, hTt[t]], writes=[ps])
                    return ps
                kinds = FM_KIND[g * 4:(g + 1) * 4]
                if kinds[0] == "ropeA":
                    for bi in range(2):
                        blk = g * 4 + bi
                        o = o16[oi % 2]
                        oi += 1
                        for t in range(NT):
                            sl = slice(t * 512, (t + 1) * 512)
                            pa = mm_block(bi, t)
                            pb = mm_block(bi + 2, t)
                            a1 = t1[ti % 2]
                            a2 = t2[ti % 2]
                            ti += 1
                            kb.tt("dve", a1[:], pa[:], cosT[:, sl], ALU.mult, [pa, cosT], [a1])
                            kb.tt("dve", a2[:], pb[:], sinT[:, sl], ALU.mult, [pb, sinT], [a2])
                            kb.tt("pool", o[:, sl], a1[:], a2[:], ALU.add, [a1, a2], [o])
                        dn = dests[blk]
                        kb.dma("sp", dr[dn][drow[blk] * 128:(drow[blk] + 1) * 128, :], o[:], reads=[o], writes=[db[dn]])
                    continue
                for bi in range(4):
                    blk = g * 4 + bi
                    kind = FM_KIND[blk]
                    dn = dests[blk]
                    rows = slice(drow[blk] * 128, (drow[blk] + 1) * 128)
                    o = o16[oi % 2]
                    oi += 1
                    for t in range(NT):
                        sl = slice(t * 512, (t + 1) * 512)
                        ps = mm_block(bi, t)
                        if kind == "copy":
                            kb.act(o[:, sl], ps[:], AF.Copy, [ps], [o])
                        elif kind == "silu":
                            kb.act(o[:, sl], ps[:], AF.Silu, [ps], [o])
                        elif kind == "f":
                            a1 = t1[ti % 2]
                            ti += 1
                            kb.act(a1[:], ps[:], AF.Sigmoid, [ps], [a1])
                            kb.ts("dve", a1[:], a1[:], oml[:, bi:bi + 1], lbc[:, bi:bi + 1], ALU.mult, ALU.add,
                                  [a1, oml, lbc], [a1])
                            kb.act(o32[:, 4 + t * 512:4 + (t + 1) * 512], a1[:], AF.Ln, [a1], [o32])
                            kb.ts("dve", k16[:, sl], a1[:], -1.0, 1.0, ALU.mult, ALU.add, [a1], [k16])
                        elif kind == "conv":
                            kb.act(o32[:, 4 + t * 512:4 + (t + 1) * 512], ps[:], AF.Copy, [ps], [o32])
                    if kind == "f":
                        kb.dma("sp", dr["HGLF"][rows, :], o32[:, 4:4 + SEQ], reads=[o32], writes=[db["HGLF"]])
                        kb.dma("sp", dr["HGK"][rows, :], k16[:], reads=[k16], writes=[db["HGK"]])
                    elif kind == "conv":
                        cb = blk - 28
                        acc = hTacc = None
                        for t in range(NT):
                            sl = slice(t * 512, (t + 1) * 512)
                            a1 = t1[ti % 2]
                            ti += 1
                            base = 1 + t * 512
                            kb.ts("dve", a1[:], o32[:, base:base + 512], convw[:, cb, 0:1], None, ALU.mult, None,
                                  [o32, convw], [a1])
                            for i in range(1, 4):
                                kb.stt("dve", a1[:], o32[:, base + i:base + i + 512], convw[:, cb, i:i + 1], a1[:],
                                       ALU.mult, ALU.add, [o32, convw, a1], [a1])
                            kb.act(o[:, sl], a1[:], AF.Silu, [a1, convb], [o], bias=convb[:, cb:cb + 1])
                        kb.dma("sp", dr[dn][rows, :], o[:], reads=[o], writes=[db[dn]])
                    else:
                        kb.dma("sp", dr[dn][rows, :], o[:], reads=[o], writes=[db[dn]])
            tm_dest = ["HGI", "RV", "MZ"]
            st = []
            for i in range(2):
                v = T(o16[i].t[:, 0:2048].rearrange("p (j n) -> p j n", n=512), "stv%d" % i)
                v.b = o16[i].b
                st.append(v)
            si = 0
            for g in range(3):
                w = wb[(NFM // 4 + g) % 2]
                kb.dma("pool", w[:], dr["w_tm"][l, :, g * 512:(g + 1) * 512].rearrange("(k p) n -> p k n", p=128),
                       reads=[db["w_tm"]], writes=[w])
                for q4 in range(8):
                    s = st[si % 2]
                    si += 1
                    for j in range(4):
                        tt_ = q4 * 4 + j
                        ps = self.bank()
                        for k in range(8):
                            kb.mm(ps[:], hT[:, k, tt_ * 128:(tt_ + 1) * 128], w[:, k, :], start=(k == 0), stop=(k == 7),
                                  reads=[hTt[tt_ // 4], w], writes=[ps])
                        if g == 2:
                            kb.act(s[:, j, :], ps[:], AF.Silu, [ps], [s])
                        else:
                            kb.copy("dve", s[:, j, :], ps[:], [ps], [s])
                    kb.dma("sp", dr[tm_dest[g]][q4 * 512:(q4 + 1) * 512, :].rearrange("(j p) n -> p j n", p=128), s[:],
                           reads=[s], writes=[db[tm_dest[g]]])
            wdt = kb.sb("wdt", [128, 8, 8], BF16, es)
            dts = kb.sb("dts", [128, 32, 8], F32, es)
            kb.dma("pool", wdt[:], dr["w_tm"][l, :, 1536:1544].rearrange("(k p) n -> p k n", p=128),
                   reads=[db["w_tm"]], writes=[wdt])
            ps = self.bank()
            for tt_ in range(32):
                for k in range(8):
                    kb.mm(ps[:, tt_ * 8:(tt_ + 1) * 8], hT[:, k, tt_ * 128:(tt_ + 1) * 128], wdt[:, k, :],
                          start=(k == 0), stop=(k == 7), reads=[hTt[tt_ // 4], wdt], writes=[ps], sig=(k == 7 and tt_ == 31))
            kb.tt("dve", dts[:], ps[:, 0:256].rearrange("p (t h) -> p t h", h=8),
                  dtb[:].unsqueeze(1).to_broadcast([128, 32, 8]), ALU.add, [ps, dtb], [dts])
            kb.act(dts[:], dts[:], AF.Exp, [dts], [dts])
            kb.act(dts[:], dts[:], AF.Ln, [dts], [dts], bias=1.0)
            kb.dma("sp", dr["DT"].rearrange("(t p) h -> p t h", p=128), dts[:], reads=[dts], writes=[db["DT"]])
            for e8 in range(4):
                kb.dma("pool", dr["MW13"][l, e8 * 8:(e8 + 1) * 8].rearrange("e p f -> p e f"),
                       dr["moe_w13_pk"][l, e8 * 8:(e8 + 1) * 8].rearrange("e p f -> p e f"), reads=[db["moe_w13_pk"]], writes=[db["MW13"]], bg=True)
            kb.dma("pool", dr["MW2"][l].rearrange("g p f -> p g f"), dr["moe_w2_pk"][l].rearrange("g p f -> p g f"),
                   reads=[db["moe_w2_pk"]], writes=[db["MW2"]], bg=True)
            kb.barrier()


    def head_rstd(self, src, osq, rs, ones, psn):
        kb = self.kb
        kb.act(osq[:], src[:], AF.Square, [src], [osq])
        kb.mm(psn[:], ones[:], osq[:], start=True, stop=True, reads=[ones, osq], writes=[psn])
        kb.act(rs[:], psn[:], AF.Ln, [psn], [rs], bias=EPS)
        kb.act(rs[:], rs[:], AF.Exp, [rs], [rs], scale=-0.5)

    def phase_hg(self, l):
        kb = self.kb
        dr, db = self.dram, self.dbuf
        NH = 4
        with ExitStack() as es:
            P = self.psum
            psO = [P[h] for h in range(NH)]
            psKV = []
            for h in range(NH):
                bk = P[4 + h // 2]
                v = T(bk.t[:, (h % 2) * 128:(h % 2 + 1) * 128], "kv%d" % h)
                v.b = bk.b
                psKV.append(v)
            rot = [P[6], P[7]]
            ri = [0]

            def rbank():
                b = rot[ri[0] % 2]
                ri[0] += 1
                return b
            S = [kb.sb("S%d" % h, [128, 128], F32, es) for h in range(NH)]
            for h in range(NH):
                kb.memset("dve", S[h][:], 0.0, [S[h]])

            def mk(name, shape, dt, n=NH):
                return [kb.sb("%s%d" % (name, i), shape, dt, es) for i in range(n)]
            qt = mk("qt", [128, 512], BF16, 2 * NH)
            kt = mk("kt", [128, 512], BF16, 2 * NH)
            gt = mk("gt", [128, 512], BF16, 2 * NH)
            lf = mk("lf", [128, 512], F32, 2 * NH)
            vt = mk("vt", [64, 8, 128], BF16, 2 * NH)
            bb = mk("bb", [128, 512], F32)
            bm = mk("bm", [128, 512], F32)
            eq = mk("eq", [128, 512], F32)
            ek = mk("ek", [128, 512], F32)
            qs = mk("qs", [128, 512], BF16)
            ks = mk("ks", [128, 512], BF16)
            ksz = mk("ksz", [128, 512], BF16)
            kT = mk("kT", [64, 1024], BF16)
            sc = mk("sc", [64, 512], BF16)
            for h in range(NH):
                kb.memset("dve", sc[h][:], 0.0, [sc[h]])
            em = mk("em", [128, 8], F32)
            cd = mk("cd", [128, 8], F32)
            Sb = mk("Sb", [128, 128], BF16, 2 * NH)
            tS = mk("tS", [128, 128], F32)
            osq = mk("osq", [128, 512], BF16)
            rs = mk("rs", [128, 512], F32)
            y1 = mk("y1", [128, 512], F32)
            yo = mk("yo", [128, 512], BF16, 2 * NH)
            import os
            stop = os.environ.get("HG_STOP", "")
            for st in range(NT if not stop else 1):
                tsl = slice(st * 512, (st + 1) * 512)
                par = (st % 2) * NH
                for h in range(NH):
                    rows = slice(h * 128, (h + 1) * 128)
                    i = par + h
                    kb.dma("sp", qt[i][:], dr["HGQ"][rows, tsl], reads=[db["HGQ"]], writes=[qt[i]])
                    kb.dma("sp", kt[i][:], dr["HGK"][rows, tsl], reads=[db["HGK"]], writes=[kt[i]])
                    kb.dma("sp", lf[i][:], dr["HGLF"][rows, tsl], reads=[db["HGLF"]], writes=[lf[i]])
                    kb.dma("sp", gt[i][:], dr["HGG"][rows, tsl], reads=[db["HGG"]], writes=[gt[i]])
                    kb.dma("sp", vt[i][:], dr["HGI"][tsl, rows].rearrange("(c p) d -> p c d", p=64),
                           reads=[db["HGI"]], writes=[vt[i]])
                for h in range(NH):
                    i = par + h
                    kb.op("dve", lambda e: e.tensor_tensor_scan(bb[h][:], self.reset64[:], lf[i][:], 0.0, ALU.mult, ALU.add),
                          [self.reset64, lf[i]], [bb[h]])
                    b3 = bb[h][:].rearrange("p (c t) -> p c t", t=64)
                    bm3 = bm[h][:].rearrange("p (c t) -> p c t", t=64)
                    kb.tt("dve", bm3, b3, b3[:, :, 31:32].to_broadcast([128, 8, 64]), ALU.subtract, [bb[h]], [bm[h]])
                    kb.act(eq[h][:], bm[h][:], AF.Exp, [bm[h]], [eq[h]])
                    kb.act(ek[h][:], bm[h][:], AF.Exp, [bm[h]], [ek[h]], scale=-1.0)
                    kb.act(em[h][:], b3[:, :, 31], AF.Exp, [bb[h]], [em[h]])
                    kb.act(cd[h][:], bm3[:, :, 63], AF.Exp, [bm[h]], [cd[h]])
                    kb.tt("dve", qs[h][:], qt[i][:], eq[h][:], ALU.mult, [qt[i], eq[h]], [qs[h]])
                    kb.tt("dve", ks[h][:], kt[i][:], ek[h][:], ALU.mult, [kt[i], ek[h]], [ks[h]])
                    pT = rbank()
                    pTb = pT.t[:].bitcast(BF16)
                    for c in range(8):
                        kb.tr(pTb[0:64, c * 128:(c + 1) * 128], ks[h][:, c * 64:(c + 1) * 64], self.identb[:],
                              [ks[h], self.identb], [pT], sig=(c == 7))
                    kb.copy("act", kT[h][:], pTb[0:64, :], [pT], [kT[h]])
                    kb.tt("dve", ksz[h][:], ks[h][:], self.hm64[:], ALU.mult, [ks[h], self.hm64], [ksz[h]])
                    pS = rbank()
                    for c in range(8):
                        cs = slice(c * 64, (c + 1) * 64)
                        c1 = slice(c * 64, c * 64 + 32)
                        c2 = slice(c * 64 + 32, (c + 1) * 64)
                        kb.mm(pS[0:64, c1], ksz[h][:, cs], qs[h][:, c1], start=True, stop=True,
                              reads=[ksz[h], qs[h]], writes=[pS], sig=False)
                        kb.mm(pS[0:64, c2], ks[h][:, cs], qs[h][:, c2], start=True, stop=True,
                              reads=[ks[h], qs[h]], writes=[pS], sig=(c == 7))
                    kb.op("dve", lambda e: e.copy_predicated(sc[h][:], self.cmask64[:].bitcast(mybir.dt.uint32), pS[0:64, :]),
                          [pS, self.cmask64], [sc[h]])
                if stop == "prep":
                    break
                for c in range(8):
                    cs = slice(c * 64, (c + 1) * 64)
                    for h in range(NH):
                        sb_ = Sb[(c % 2) * NH + h]
                        kb.act(sb_[:], S[h][:], AF.Identity, [S[h], em[h]], [sb_], scale=em[h][:, c:c + 1])
                    for h in range(NH):
                        i = par + h
                        sb_ = Sb[(c % 2) * NH + h]
                        kb.mm(psO[h][:, cs], vt[i][:, c, :], sc[h][:, cs], start=True, stop=False,
                              reads=[vt[i], sc[h]], writes=[psO[h]], sig=False)
                        kb.mm(psO[h][:, cs], sb_[:], qs[h][:, cs], start=False, stop=True,
                              reads=[sb_, qs[h]], writes=[psO[h]], sig=True)
                        kb.mm(psKV[h][:], kT[h][:, c * 128:(c + 1) * 128], vt[i][:, c, :], start=True, stop=True,
                              reads=[kT[h], vt[i]], writes=[psKV[h]])
                    for h in range(NH):
                        kb.stt("dve", tS[h][:], S[h][:], em[h][:, c:c + 1], psKV[h][:], ALU.mult, ALU.add,
                               [S[h], em[h], psKV[h]], [tS[h]])
                        kb.ts("dve", S[h][:], tS[h][:], cd[h][:, c:c + 1], None, ALU.mult, None, [tS[h], cd[h]], [S[h]])
                if stop == "chunk":
                    break
                for h in range(NH):
                    i = par + h
                    psn = rbank()
                    self.head_rstd(psO[h], osq[h], rs[h], self.ones_h, psn)
                    kb.stt("dve", y1[h][:], psO[h][:], self.hgnorm[:, l:l + 1], rs[h][:], ALU.mult, ALU.mult,
                           [psO[h], self.hgnorm, rs[h]], [y1[h]])
                    kb.tt("dve", yo[i][:], y1[h][:], gt[i][:], ALU.mult, [y1[h], gt[i]], [yo[i]])
                    kb.dma("sp", dr["YB"][512 + h * 128:512 + (h + 1) * 128, tsl], yo[i][:], reads=[yo[i]], writes=[db["YB"]])
            kb.barrier()

    def phase_ret(self, l):
        kb = self.kb
        dr, db = self.dram, self.dbuf
        NH = 4
        GAM = [1.0 - 2.0 ** (-5.0 - h) for h in range(4)]
        with ExitStack() as es:
            P = self.psum
            psO = [P[h] for h in range(NH)]
            psKV = []
            for h in range(NH):
                bk = P[4 + h // 2]
                v = T(bk.t[0:64, (h % 2) * 128:(h % 2 + 1) * 128], "rkv%d" % h)
                v.b = bk.b
                psKV.append(v)
            rot = [P[6], P[7]]
            ri = [0]

            def rbank():
                b = rot[ri[0] % 2]
                ri[0] += 1
                return b

            def mk(name, shape, dt, n=NH):
                return [kb.sb("%s%d" % (name, i), shape, dt, es) for i in range(n)]
            gq = mk("gq", [64, 512], F32)
            mret = mk("mret", [128, 512], F32)
            gk = kb.sb("gk", [128, 4], F32, es)
            kb.dma("sp", gk[:], dr["ret_gk128"], reads=[db["ret_gk128"]], writes=[gk])
            for h in range(NH):
                kb.dma("sp", gq[h][:], dr["ret_gq128"][h], reads=[db["ret_gq128"]], writes=[gq[h]])
                kb.dma("sp", mret[h][:], dr["ret_mask128"][h], reads=[db["ret_mask128"]], writes=[mret[h]])
            S = mk("rS", [64, 128], F32)
            for h in range(NH):
                kb.memset("dve", S[h][:], 0.0, [S[h]])
            qt = mk("rqt", [64, 512], BF16, 2 * NH)
            kt = mk("rkt", [64, 512], BF16, 2 * NH)
            gt = mk("rgt", [128, 512], BF16, 2 * NH)
            vt = mk("rvt", [128, 4, 128], BF16, 2 * NH)
            qd = mk("rqd", [64, 512], BF16)
            kT = mk("rkT", [128, 256], BF16)
            sc = mk("rsc", [128, 512], BF16)
            Sb = mk("rSb", [64, 128], BF16, 2 * NH)
            osq = mk("rosq", [128, 512], BF16)
            rs = mk("rrs", [128, 512], F32)
            y1 = mk("ry1", [128, 512], F32)
            yo = mk("ryo", [128, 512], BF16, 2 * NH)
            for st in range(NT):
                tsl = slice(st * 512, (st + 1) * 512)
                par = (st % 2) * NH
                for h in range(NH):
                    i = par + h
                    kb.dma("sp", qt[i][:], dr["RQ"][h * 64:(h + 1) * 64, tsl], reads=[db["RQ"]], writes=[qt[i]])
                    kb.dma("sp", kt[i][:], dr["RK"][h * 64:(h + 1) * 64, tsl], reads=[db["RK"]], writes=[kt[i]])
                    kb.dma("sp", gt[i][:], dr["RG"][h * 128:(h + 1) * 128, tsl], reads=[db["RG"]], writes=[gt[i]])
                    kb.dma("sp", vt[i][:], dr["RV"][tsl, h * 128:(h + 1) * 128].rearrange("(c p) d -> p c d", p=128),
                           reads=[db["RV"]], writes=[vt[i]])
                for h in range(NH):
                    i = par + h
                    kb.tt("dve", qd[h][:], qt[i][:], gq[h][:], ALU.mult, [qt[i], gq[h]], [qd[h]])
                    pT = rbank()
                    pTb = pT.t[:].bitcast(BF16)
                    for c in range(4):
                        kb.tr(pTb[:, c * 64:(c + 1) * 64], kt[i][:, c * 128:(c + 1) * 128], self.identb[0:64, 0:64],
                              [kt[i], self.identb], [pT], sig=(c == 3))
                    kb.act(kT[h][:], pTb[:, 0:256], AF.Identity, [pT, gk], [kT[h]], scale=gk[:, h:h + 1])
                    pS = rbank()
                    for c in range(4):
                        cs = slice(c * 128, (c + 1) * 128)
                        kb.mm(pS[:, cs], kt[i][:, cs], qt[i][:, cs], start=True, stop=True,
                              reads=[kt[i], qt[i]], writes=[pS], sig=(c == 3))
                    kb.tt("dve", sc[h][:], pS[:, :], mret[h][:], ALU.mult, [pS, mret[h]], [sc[h]])
                for c in range(4):
                    cs = slice(c * 128, (c + 1) * 128)
                    for h in range(NH):
                        sb_ = Sb[(c % 2) * NH + h]
                        kb.copy("act", sb_[:], S[h][:], [S[h]], [sb_])
                    for h in range(NH):
                        i = par + h
                        sb_ = Sb[(c % 2) * NH + h]
                        kb.mm(psO[h][:, cs], vt[i][:, c, :], sc[h][:, cs], start=True, stop=False,
                              reads=[vt[i], sc[h]], writes=[psO[h]], sig=False)
                        kb.mm(psO[h][:, cs], sb_[:], qd[h][:, cs], start=False, stop=True,
                              reads=[sb_, qd[h]], writes=[psO[h]], sig=True)
                        kb.mm(psKV[h][:], kT[h][:, c * 64:(c + 1) * 64], vt[i][:, c, :], start=True, stop=True,
                              reads=[kT[h], vt[i]], writes=[psKV[h]])
                    for h in range(NH):
                        kb.stt("dve", S[h][:], S[h][:], float(GAM[h] ** 128), psKV[h][:], ALU.mult, ALU.add,
                               [S[h], psKV[h]], [S[h]])
                for h in range(NH):
                    i = par + h
                    psn = rbank()
                    self.head_rstd(psO[h], osq[h], rs[h], self.ones_h, psn)
                    kb.tt("dve", y1[h][:], psO[h][:], rs[h][:], ALU.mult, [psO[h], rs[h]], [y1[h]])
                    kb.tt("dve", yo[i][:], y1[h][:], gt[i][:], ALU.mult, [y1[h], gt[i]], [yo[i]])
                    kb.dma("sp", dr["YB"][1024 + h * 128:1024 + (h + 1) * 128, tsl], yo[i][:], reads=[yo[i]], writes=[db["YB"]])
            kb.barrier()

    def phase_ssd(self, l):
        kb = self.kb
        dr, db = self.dram, self.dbuf
        with ExitStack() as es:
            P = self.psum
            pD = [P[0], P[1]]
            BG, Bt, pY1, pY2, pKV = P[2], P[3], P[4], P[5], P[6]

            def sbt(name, shape, dt):
                return kb.sb(name, shape, dt, es)
            cmk = sbt("cmk128", [128, 1024], F32)
            negm = sbt("negm128", [128, 1024], F32)
            kb.dma("sp", cmk[:], dr["cmask128"], reads=[db["cmask128"]], writes=[cmk])
            kb.dma("sp", negm[:], dr["negm128"], reads=[db["negm128"]], writes=[negm])
            ones = sbt("ones128", [128, 1024], F32)
            kb.memset("dve", ones[:], 1.0, [ones])
            arow = sbt("arow", [128, 8], F32)
            drow = sbt("drow", [128, 8], F32)
            nrow = sbt("nrow", [128, 512], F32)
            kb.dma("sp", arow[:], dr["m2_alog"][l].partition_broadcast(128), reads=[db["m2_alog"]], writes=[arow])
            kb.dma("sp", drow[:], dr["m2_d"][l].partition_broadcast(128), reads=[db["m2_d"]], writes=[drow])
            kb.dma("sp", nrow[:], dr["m2_normw"][l].partition_broadcast(128), reads=[db["m2_normw"]], writes=[nrow])
            kb.act(arow[:], arow[:], AF.Exp, [arow], [arow])
            S = sbt("mS", [128, 512], F32)
            Sb = sbt("mSb", [128, 512], BF16)
            kb.memset("dve", S[:], 0.0, [S])
            kb.memset("dve", Sb[:], 0.0, [Sb])
            xsT = [sbt("xsT%d" % i, [128, 4, 512], BF16) for i in range(2)]
            BT = [sbt("BT%d" % i, [128, 2, 512], BF16) for i in range(2)]
            CT = [sbt("CT%d" % i, [128, 2, 512], BF16) for i in range(2)]
            zt = [sbt("zt%d" % i, [128, 4, 512], BF16) for i in range(2)]
            dtt = [sbt("dtt%d" % i, [128, 4, 8], F32) for i in range(2)]
            yo = [sbt("myo%d" % i, [128, 4, 512], BF16) for i in range(2)]

            def two(name, shape, dt):
                return [sbt("%s%d" % (name, i), shape, dt) for i in range(2)]
            dA, dAn = two("dA", [128, 8], F32), two("dAn", [128, 8], F32)
            X, Y = two("X", [128, 1024], F32), two("Y", [128, 1024], F32)
            Lm = two("Lm", [128, 1024], F32)
            scL = two("scL", [128, 1024], BF16)
            xB = two("xB", [128, 768], BF16)
            vv, vh = two("vv", [128, 512], BF16), two("vh", [128, 512], BF16)
            bsb, eb, dec, eBT = two("bsb", [128, 8], F32), two("eb", [128, 8], F32), two("dec", [128, 8], F32), two("eBT", [128, 8], F32)
            ta, tb, tc_, yz, ysq = (two("ta", [128, 512], F32), two("tb", [128, 512], F32), two("tc", [128, 512], F32),
                                    two("yz", [128, 512], F32), two("ysq", [128, 512], F32))
            ssq, rst = two("ssq", [128, 1], F32), two("rst", [128, 1], F32)
            yn = two("yn", [128, 512], BF16)
            tS = sbt("tSm", [128, 512], F32)
            cm3 = cmk[:].rearrange("p (h t) -> p h t", t=128)
            on3 = ones[:].rearrange("p (h t) -> p h t", t=128)
            ci = 0
            for st in range(NT):
                tsl = slice(st * 512, (st + 1) * 512)
                i = st % 2
                kb.dma("sp", xsT[i][:], dr["XBC"][0:512, tsl].rearrange("(k p) n -> p k n", p=128), reads=[db["XBC"]], writes=[xsT[i]])
                kb.dma("sp", BT[i][:], dr["XBC"][512:768, tsl].rearrange("(k p) n -> p k n", p=128), reads=[db["XBC"]], writes=[BT[i]])
                kb.dma("sp", CT[i][:], dr["XBC"][768:1024, tsl].rearrange("(k p) n -> p k n", p=128), reads=[db["XBC"]], writes=[CT[i]])
                kb.dma("sp", zt[i][:], dr["MZ"][tsl, :].rearrange("(c p) d -> p c d", p=128), reads=[db["MZ"]], writes=[zt[i]])
                kb.dma("sp", dtt[i][:], dr["DT"][tsl, :].rearrange("(c p) d -> p c d", p=128), reads=[db["DT"]], writes=[dtt[i]])
                for c in range(4):
                    cs = slice(c * 128, (c + 1) * 128)
                    j = ci % 2
                    ci += 1
                    BGb = BG.t[:].bitcast(BF16)
                    Btb = Bt.t[:].bitcast(BF16)
                    kb.tt("dve", dAn[j][:], dtt[i][:, c, :], arow[:], ALU.mult, [dtt[i], arow], [dAn[j]])
                    kb.ts("dve", dA[j][:], dAn[j][:], -1.0, None, ALU.mult, None, [dAn[j]], [dA[j]])
                    kb.tt("dve", X[j][:].rearrange("p (h t) -> p h t", t=128), cm3,
                          dA[j][:].unsqueeze(2).to_broadcast([128, 8, 128]), ALU.mult, [cmk, dA[j]], [X[j]])
                    kb.tt("dve", Y[j][:].rearrange("p (h t) -> p h t", t=128), on3,
                          dAn[j][:].unsqueeze(2).to_broadcast([128, 8, 128]), ALU.mult, [ones, dAn[j]], [Y[j]])
                    for hb in range(2):
                        hsl = slice(hb * 512, (hb + 1) * 512)
                        kb.mm(pD[hb][:], self.identf[:], negm[:, hsl], start=True, stop=False, reads=[self.identf, negm], writes=[pD[hb]], sig=False)
                        kb.mm(pD[hb][:], cmk[:, 0:128], Y[j][:, hsl], start=False, stop=False, reads=[cmk, Y[j]], writes=[pD[hb]], sig=False)
                        kb.mm(pD[hb][:], ones[:, 0:128], X[j][:, hsl], start=False, stop=True, reads=[ones, X[j]], writes=[pD[hb]], sig=True)
                        kb.act(Lm[j][:, hsl], pD[hb][:], AF.Exp, [pD[hb]], [Lm[j]])
                    for g in range(2):
                        kb.mm(BG[:, g * 128:(g + 1) * 128], BT[i][:, g, cs], CT[i][:, g, cs], start=True, stop=True,
                              reads=[BT[i], CT[i]], writes=[BG], sig=(g == 1))
                    kb.mm(Bt[:, 384:392], cmk[:, 0:128], dA[j][:], start=True, stop=True, reads=[cmk, dA[j]], writes=[Bt], sig=False)
                    kb.mm(Bt[:, 392:400], ones[:, 0:128], dA[j][:], start=True, stop=True, reads=[ones, dA[j]], writes=[Bt], sig=False)
                    for k in range(4):
                        kb.tr(Btb[:, k * 128:(k + 1) * 128], xsT[i][:, k, cs], self.identb[:], [xsT[i], self.identb], [Bt], sig=False)
                    for g in range(2):
                        kb.tr(Btb[:, 512 + g * 128:512 + (g + 1) * 128], BT[i][:, g, cs], self.identb[:],
                              [BT[i], self.identb], [Bt], sig=(g == 1))
                    for g in range(2):
                        gs = slice(g * 512, (g + 1) * 512)
                        kb.tt("dve", scL[j][:, gs].rearrange("p (h t) -> p h t", t=128), Lm[j][:, gs].rearrange("p (h t) -> p h t", t=128),
                              BG[:, g * 128:(g + 1) * 128].unsqueeze(1).to_broadcast([128, 4, 128]), ALU.mult, [Lm[j], BG], [scL[j]])
                    kb.copy("act", xB[j][:], Btb[:, 0:768], [Bt], [xB[j]])
                    kb.copy("act", bsb[j][:], Bt[:, 384:392], [Bt], [bsb[j]])
                    kb.act(eb[j][:], Bt[:, 384:392], AF.Exp, [Bt], [eb[j]])
                    kb.act(eBT[j][:], Bt[:, 392:400], AF.Exp, [Bt], [eBT[j]])
                    kb.tt("dve", dec[j][:], Bt[:, 392:400], bsb[j][:], ALU.subtract, [Bt, bsb[j]], [dec[j]])
                    kb.act(dec[j][:], dec[j][:], AF.Exp, [dec[j]], [dec[j]])
                    kb.tt("dve", vv[j][:].rearrange("p (h t) -> p h t", t=64), xB[j][:, 0:512].rearrange("p (h t) -> p h t", t=64),
                          dtt[i][:, c, :].unsqueeze(2).to_broadcast([128, 8, 64]), ALU.mult, [xB[j], dtt[i]], [vv[j]])
                    for h in range(8):
                        kb.mm(pY1[:, h * 64:(h + 1) * 64], scL[j][:, h * 128:(h + 1) * 128], vv[j][:, h * 64:(h + 1) * 64],
                              start=True, stop=True, reads=[scL[j], vv[j]], writes=[pY1], sig=(h == 7))
                    for g in range(2):
                        gs = slice(g * 256, (g + 1) * 256)
                        kb.mm(pY2[:, gs], CT[i][:, g, cs], Sb[:, gs], start=True, stop=True, reads=[CT[i], Sb], writes=[pY2], sig=(g == 1))
                    kb.tt("dve", tb[j][:].rearrange("p (h t) -> p h t", t=64), xB[j][:, 0:512].rearrange("p (h t) -> p h t", t=64),
                          drow[:].unsqueeze(2).to_broadcast([128, 8, 64]), ALU.mult, [xB[j], drow], [tb[j]])
                    kb.tt("dve", vh[j][:].rearrange("p (h t) -> p h t", t=64), vv[j][:].rearrange("p (h t) -> p h t", t=64),
                          dec[j][:].unsqueeze(2).to_broadcast([128, 8, 64]), ALU.mult, [vv[j], dec[j]], [vh[j]])
                    for g in range(2):
                        gs = slice(g * 256, (g + 1) * 256)
                        kb.mm(pKV[:, gs], xB[j][:, 512 + g * 128:512 + (g + 1) * 128], vh[j][:, gs], start=True, stop=True,
                              reads=[xB[j], vh[j]], writes=[pKV], sig=(g == 1))
                    kb.tt("dve", ta[j][:].rearrange("p (h t) -> p h t", t=64), pY2[:].rearrange("p (h t) -> p h t", t=64),
                          eb[j][:].unsqueeze(2).to_broadcast([128, 8, 64]), ALU.mult, [pY2, eb[j]], [ta[j]])
                    kb.tt("dve", tS[:].rearrange("p (h t) -> p h t", t=64), S[:].rearrange("p (h t) -> p h t", t=64),
                          eBT[j][:].unsqueeze(2).to_broadcast([128, 8, 64]), ALU.mult, [S, eBT[j]], [tS])
                    kb.tt("dve", S[:], tS[:], pKV[:], ALU.add, [tS, pKV], [S])
                    kb.copy("act", Sb[:], S[:], [S], [Sb])
                    kb.tt("dve", tc_[j][:], pY1[:], ta[j][:], ALU.add, [pY1, ta[j]], [tc_[j]])
                    kb.tt("dve", tc_[j][:], tc_[j][:], tb[j][:], ALU.add, [tc_[j], tb[j]], [tc_[j]])
                    kb.tt("dve", yz[j][:], tc_[j][:], zt[i][:, c, :], ALU.mult, [tc_[j], zt[i]], [yz[j]])
                    kb.memset("dve", ssq[j][:], 0.0, [ssq[j]])
                    kb.act(ysq[j][:], yz[j][:], AF.Square, [yz[j]], [ysq[j], ssq[j]], accum_out=ssq[j][:])
                    kb.act(rst[j][:], ssq[j][:], AF.Ln, [ssq[j]], [rst[j]], bias=EPS, scale=1.0 / 512.0)
                    kb.act(rst[j][:], rst[j][:], AF.Exp, [rst[j]], [rst[j]], scale=-0.5)
                    kb.stt("dve", yn[j][:], yz[j][:], rst[j][:, 0:1], nrow[:], ALU.mult, ALU.mult, [yz[j], rst[j], nrow], [yn[j]])
                    for k in range(4):
                        kb.tr(BGb[:, 512 + k * 128:512 + (k + 1) * 128], yn[j][:, k * 128:(k + 1) * 128],
                              self.identb[:], [yn[j], self.identb], [BG], sig=(k == 3))
                    kb.copy("act", yo[i][:, :, cs], BGb[:, 512:1024].rearrange("p (k t) -> p k t", t=128), [BG], [yo[i]])
                kb.dma("sp", dr["YB"][1536:2048, tsl].rearrange("(k p) n -> p k n", p=128), yo[i][:], reads=[yo[i]], writes=[db["YB"]])
            kb.barrier()


    def phase_ssd64(self, l):
        kb = self.kb
        dr, db = self.dram, self.dbuf
        with ExitStack() as es:
            P = self.psum
            pD, pGB, pT, pY1, pY2, pKV = P[0], P[1], P[2], P[3], P[4], P[5]
            pYT = [P[6], P[7]]

            def sbt(name, shape, dt):
                return kb.sb(name, shape, dt, es)
            negm = sbt("negm", [64, 512], F32)
            kb.dma("sp", negm[:], dr["negm64"], reads=[db["negm64"]], writes=[negm])
            ones = sbt("ones64", [64, 512], F32)
            kb.memset("dve", ones[:], 1.0, [ones])
            arow = sbt("arow", [64, 8], F32)
            drow = sbt("drow", [64, 8], F32)
            nrow = sbt("nrow", [64, 512], F32)
            kb.dma("sp", arow[:], dr["m2_alog"][l].partition_broadcast(64), reads=[db["m2_alog"]], writes=[arow])
            kb.dma("sp", drow[:], dr["m2_d"][l].partition_broadcast(64), reads=[db["m2_d"]], writes=[drow])
            kb.dma("sp", nrow[:], dr["m2_normw"][l].partition_broadcast(64), reads=[db["m2_normw"]], writes=[nrow])
            kb.act(arow[:], arow[:], AF.Exp, [arow], [arow])
            S = sbt("mS", [128, 512], F32)
            Sb = sbt("mSb", [128, 512], BF16)
            kb.memset("dve", S[:], 0.0, [S])
            kb.memset("dve", Sb[:], 0.0, [Sb])
            xsT = [sbt("xsT%d" % i, [128, 4, 512], BF16) for i in range(2)]
            BT = [sbt("BT%d" % i, [128, 2, 512], BF16) for i in range(2)]
            CT = [sbt("CT%d" % i, [128, 2, 512], BF16) for i in range(2)]
            zt = [sbt("zt%d" % i, [64, 8, 512], BF16) for i in range(2)]
            dtt = [sbt("dtt%d" % i, [64, 8, 8], F32) for i in range(2)]
            dA = sbt("dA", [64, 8], F32)
            dAn = sbt("dAn", [64, 8], F32)
            X = sbt("X", [64, 512], F32)
            Y = sbt("Y", [64, 512], F32)
            Lm = sbt("Lm", [64, 512], F32)
            scL = sbt("scL", [64, 512], BF16)
            xB = sbt("xB", [64, 768], BF16)
            vv = sbt("vv", [64, 512], BF16)
            vh = sbt("vh", [64, 512], BF16)
            bsb = sbt("bsb", [64, 8], F32)
            eb = sbt("eb", [64, 8], F32)
            dec = sbt("dec", [64, 8], F32)
            eBT = sbt("eBT", [128, 8], F32)
            ta = sbt("ta", [64, 512], F32)
            tb = sbt("tb", [64, 512], F32)
            tc_ = sbt("tc", [64, 512], F32)
            yz = sbt("yz", [64, 512], F32)
            ysq = sbt("ysq", [64, 512], F32)
            ssq = sbt("ssq", [64, 1], F32)
            rst = sbt("rst", [64, 1], F32)
            yn = sbt("yn", [64, 512], BF16)
            tS = sbt("tSm", [128, 512], F32)
            yo = [sbt("myo%d" % i, [128, 4, 512], BF16) for i in range(2)]
            cm3 = self.cmask64[:].rearrange("p (h t) -> p h t", t=64)
            for st in range(NT):
                tsl = slice(st * 512, (st + 1) * 512)
                i = st % 2
                kb.dma("sp", xsT[i][:], dr["XBC"][0:512, tsl].rearrange("(k p) n -> p k n", p=128), reads=[db["XBC"]], writes=[xsT[i]])
                kb.dma("sp", BT[i][:], dr["XBC"][512:768, tsl].rearrange("(k p) n -> p k n", p=128), reads=[db["XBC"]], writes=[BT[i]])
                kb.dma("sp", CT[i][:], dr["XBC"][768:1024, tsl].rearrange("(k p) n -> p k n", p=128), reads=[db["XBC"]], writes=[CT[i]])
                kb.dma("sp", zt[i][:], dr["MZ"][tsl, :].rearrange("(c p) d -> p c d", p=64), reads=[db["MZ"]], writes=[zt[i]])
                kb.dma("sp", dtt[i][:], dr["DT"][tsl, :].rearrange("(c p) d -> p c d", p=64), reads=[db["DT"]], writes=[dtt[i]])
                pYTb = [b.t[:].bitcast(BF16) for b in pYT]
                for c in range(8):
                    cs = slice(c * 64, (c + 1) * 64)
                    kb.tt("dve", dAn[:], dtt[i][:, c, :], arow[:], ALU.mult, [dtt[i], arow], [dAn])
                    kb.ts("dve", dA[:], dAn[:], -1.0, None, ALU.mult, None, [dAn], [dA])
                    kb.tt("dve", X[:].rearrange("p (h t) -> p h t", t=64), cm3,
                          dA[:].unsqueeze(2).to_broadcast([64, 8, 64]), ALU.mult, [self.cmask64, dA], [X])
                    kb.tt("dve", Y[:].rearrange("p (h t) -> p h t", t=64), ones[:].rearrange("p (h t) -> p h t", t=64),
                          dAn[:].unsqueeze(2).to_broadcast([64, 8, 64]), ALU.mult, [ones, dAn], [Y])
                    kb.mm(pD[0:64, :], self.identf[0:64, 0:64], negm[:], start=True, stop=False,
                          reads=[self.identf, negm], writes=[pD], sig=False)
                    kb.mm(pD[0:64, :], self.cmask64[:, 0:64], Y[:], start=False, stop=False,
                          reads=[self.cmask64, Y], writes=[pD], sig=False)
                    kb.mm(pD[0:64, :], ones[:, 0:64], X[:], start=False, stop=True,
                          reads=[ones, X], writes=[pD], sig=True)
                    kb.act(Lm[:], pD[0:64, :], AF.Exp, [pD], [Lm])
                    for g in range(2):
                        kb.mm(pGB[0:64, g * 64:(g + 1) * 64], BT[i][:, g, cs], CT[i][:, g, cs], start=True, stop=True,
                              reads=[BT[i], CT[i]], writes=[pGB], sig=False)
                    kb.mm(pGB[0:64, 128:136], self.cmask64[:, 0:64], dA[:], start=True, stop=True,
                          reads=[self.cmask64, dA], writes=[pGB], sig=False)
                    kb.mm(pGB[0:64, 136:144], ones[:, 0:64], dA[:], start=True, stop=True,
                          reads=[ones, dA], writes=[pGB], sig=False)
                    kb.mm(pGB[:, 144:152], ones[:, 0:128], dA[:], start=True, stop=True,
                          reads=[ones, dA], writes=[pGB], sig=True)
                    for g in range(2):
                        kb.tt("dve", scL[:, g * 256:(g + 1) * 256].rearrange("p (h t) -> p h t", t=64),
                              Lm[:, g * 256:(g + 1) * 256].rearrange("p (h t) -> p h t", t=64),
                              pGB[0:64, g * 64:(g + 1) * 64].unsqueeze(1).to_broadcast([64, 4, 64]), ALU.mult,
                              [Lm, pGB], [scL])
                    kb.copy("act", bsb[:], pGB[0:64, 128:136], [pGB], [bsb])
                    kb.act(eb[:], pGB[0:64, 128:136], AF.Exp, [pGB], [eb])
                    kb.tt("dve", dec[:], pGB[0:64, 136:144], bsb[:], ALU.subtract, [pGB, bsb], [dec])
                    kb.act(dec[:], dec[:], AF.Exp, [dec], [dec])
                    kb.act(eBT[:], pGB[:, 144:152], AF.Exp, [pGB], [eBT])
                    pTb = pT.t[:].bitcast(BF16)
                    for k in range(4):
                        kb.tr(pTb[0:64, k * 128:(k + 1) * 128], xsT[i][:, k, cs], self.identb[:], [xsT[i], self.identb], [pT], sig=False)
                    for g in range(2):
                        kb.tr(pTb[0:64, 512 + g * 128:512 + (g + 1) * 128], BT[i][:, g, cs], self.identb[:],
                              [BT[i], self.identb], [pT], sig=(g == 1))
                    kb.copy("act", xB[:], pTb[0:64, 0:768], [pT], [xB])
                    kb.tt("dve", vv[:].rearrange("p (h t) -> p h t", t=64), xB[:, 0:512].rearrange("p (h t) -> p h t", t=64),
                          dtt[i][:, c, :].unsqueeze(2).to_broadcast([64, 8, 64]), ALU.mult, [xB, dtt[i]], [vv])
                    for h in range(8):
                        hs = slice(h * 64, (h + 1) * 64)
                        kb.mm(pY1[0:64, hs], scL[:, hs], vv[:, hs], start=True, stop=True, reads=[scL, vv], writes=[pY1], sig=(h == 7))
                    for g in range(2):
                        gs = slice(g * 256, (g + 1) * 256)
                        kb.mm(pY2[0:64, gs], CT[i][:, g, cs], Sb[:, gs], start=True, stop=True, reads=[CT[i], Sb], writes=[pY2], sig=(g == 1))
                    kb.tt("dve", ta[:].rearrange("p (h t) -> p h t", t=64), pY2[0:64, :].rearrange("p (h t) -> p h t", t=64),
                          eb[:].unsqueeze(2).to_broadcast([64, 8, 64]), ALU.mult, [pY2, eb], [ta])
                    kb.tt("dve", tb[:].rearrange("p (h t) -> p h t", t=64), xB[:, 0:512].rearrange("p (h t) -> p h t", t=64),
                          drow[:].unsqueeze(2).to_broadcast([64, 8, 64]), ALU.mult, [xB, drow], [tb])
                    kb.tt("dve", tc_[:], pY1[0:64, :], ta[:], ALU.add, [pY1, ta], [tc_])
                    kb.tt("dve", tc_[:], tc_[:], tb[:], ALU.add, [tc_, tb], [tc_])
                    kb.tt("dve", yz[:], tc_[:], zt[i][:, c, :], ALU.mult, [tc_, zt[i]], [yz])
                    kb.memset("dve", ssq[:], 0.0, [ssq])
                    kb.act(ysq[:], yz[:], AF.Square, [yz], [ysq, ssq], accum_out=ssq[:])
                    kb.act(rst[:], ssq[:], AF.Ln, [ssq], [rst], bias=EPS, scale=1.0 / 512.0)
                    kb.act(rst[:], rst[:], AF.Exp, [rst], [rst], scale=-0.5)
                    kb.stt("dve", yn[:], yz[:], rst[:, 0:1], nrow[:], ALU.mult, ALU.mult, [yz, rst, nrow], [yn])
                    for k in range(4):
                        bk = pYT[k // 2]
                        kb.tr(pYTb[k // 2][:, (k % 2) * 512 + c * 64:(k % 2) * 512 + (c + 1) * 64], yn[:, k * 128:(k + 1) * 128],
                              self.identb[0:64, 0:64], [yn, self.identb], [bk], sig=(k % 2 == 1))
                    kb.tt("dve", vh[:].rearrange("p (h t) -> p h t", t=64), vv[:].rearrange("p (h t) -> p h t", t=64),
                          dec[:].unsqueeze(2).to_broadcast([64, 8, 64]), ALU.mult, [vv, dec], [vh])
                    for g in range(2):
                        gs = slice(g * 256, (g + 1) * 256)
                        kb.mm(pKV[:, gs], xB[:, 512 + g * 128:512 + (g + 1) * 128], vh[:, gs], start=True, stop=True,
                              reads=[xB, vh], writes=[pKV], sig=(g == 1))
                    kb.tt("dve", tS[:].rearrange("p (h t) -> p h t", t=64), S[:].rearrange("p (h t) -> p h t", t=64),
                          eBT[:].unsqueeze(2).to_broadcast([128, 8, 64]), ALU.mult, [S, eBT], [tS])
                    kb.tt("dve", S[:], tS[:], pKV[:], ALU.add, [tS, pKV], [S])
                    kb.copy("act", Sb[:], S[:], [S], [Sb])
                for k in range(4):
                    kb.copy("act" if k % 2 else "dve", yo[i][:, k, :], pYTb[k // 2][:, (k % 2) * 512:(k % 2 + 1) * 512],
                            [pYT[k // 2]], [yo[i]])
                kb.dma("sp", dr["YB"][1536:2048, tsl].rearrange("(k p) n -> p k n", p=128), yo[i][:], reads=[yo[i]], writes=[db["YB"]])
            kb.barrier()

    def frac(self, dst, src, ti, tf, bufs_r, dst_t, ti_t, tf_t):
        kb = self.kb
        kb.copy("dve", ti, src, bufs_r, [ti_t])
        kb.copy("dve", tf, ti, [ti_t], [tf_t])
        kb.tt("dve", dst, src, tf, ALU.subtract, bufs_r + [tf_t], [dst_t])

    def phase_s5(self, l):
        kb = self.kb
        dr, db = self.dram, self.dbuf
        J = 4
        NBK = 512 // J
        with ExitStack() as es:
            P = self.psum

            def sbt(name, shape, dt, e=None):
                return kb.sb(name, shape, dt, e or es)
            rho4 = sbt("rho4", [128, 16], F32)
            BTre = [sbt("BTre%d" % j, [128, 16, 128], BF16) for j in range(J)]
            BTim = [sbt("BTim%d" % j, [128, 16, 128], BF16) for j in range(J)]
            W1 = [sbt("W1_%d" % r, [128, 16, 128], BF16) for r in range(J)]
            W2 = [sbt("W2_%d" % r, [128, 16, 128], BF16) for r in range(J)]
            Kt = sbt("Kt", [128, 4, J, 128], BF16)
            cosT = sbt("s5cos", [128, 16, NBK], F32)
            sinT = sbt("s5sin", [128, 16, NBK], F32)
            dsk = sbt("dsk", [128, 4], F32)
            wglu = sbt("wglu", [128, 4, 512], BF16)
            kb.dma("sp", dsk[:], dr["s5_d_pk"][l], reads=[db["s5_d_pk"]], writes=[dsk])
            kb.dma("pool", wglu[:], dr["s5_wglu"][l].rearrange("(k p) n -> p k n", p=128), reads=[db["s5_wglu"]], writes=[wglu])
            with ExitStack() as e1:
                def s1(name, shape, dt=F32):
                    return sbt(name, shape, dt, e1)

                def v16(name):
                    return s1(name, [128, 16])
                lr, li, dt_, tfv, t_f, fr, fr2, fr4 = [v16(n) for n in ("lr", "li", "dtv", "tfv", "t_f", "fr", "fr2", "fr4")]
                t_i = s1("t_i", [128, 16], I32)
                sn, cs_, den, cr, ci, ta, tb, xr, rho = [v16(n) for n in ("sn", "cs", "den", "cr", "ci", "ta", "tb", "xr", "rho")]
                Lr = [v16("Lr%d" % j) for j in range(J + 1)]
                Li = [v16("Li%d" % j) for j in range(J + 1)]
                kb.dma("sp", lr[:], dr["s5_lr_pk"][l], reads=[db["s5_lr_pk"]], writes=[lr])
                kb.dma("sp", li[:], dr["s5_li_pk"][l], reads=[db["s5_li_pk"]], writes=[li])
                kb.dma("sp", dt_[:], dr["s5_ldt_pk"][l], reads=[db["s5_ldt_pk"]], writes=[dt_])
                kb.ts("dve", lr[:], lr[:], -1e-4, None, ALU.min, None, [lr], [lr])
                kb.act(dt_[:], dt_[:], AF.Exp, [dt_], [dt_])
                kb.tt("dve", rho[:], lr[:], dt_[:], ALU.mult, [lr, dt_], [rho])
                kb.act(rho[:], rho[:], AF.Exp, [rho], [rho])
                kb.tt("dve", tfv[:], li[:], dt_[:], ALU.mult, [li, dt_], [tfv])
                kb.ts("dve", tfv[:], tfv[:], 1.0 / TWO_PI, None, ALU.mult, None, [tfv], [tfv])
                self.frac(fr[:], tfv[:], t_i[:], t_f[:], [tfv], fr, t_i, t_f)
                kb.act(sn[:], fr[:], AF.Sin, [fr], [sn], scale=SIN_SCALE)
                kb.ts("dve", tfv[:], fr[:], 0.25, None, ALU.add, None, [fr], [tfv])
                self.frac(fr2[:], tfv[:], t_i[:], t_f[:], [tfv], fr2, t_i, t_f)
                kb.act(cs_[:], fr2[:], AF.Sin, [fr2], [cs_], scale=SIN_SCALE)
                kb.tt("dve", Lr[1][:], rho[:], cs_[:], ALU.mult, [rho, cs_], [Lr[1]])
                kb.tt("dve", Li[1][:], rho[:], sn[:], ALU.mult, [rho, sn], [Li[1]])

                def cmul(orr, oi, ar, ai, br_, bi_):
                    kb.tt("dve", ta[:], ar[:], br_[:], ALU.mult, [ar, br_], [ta])
                    kb.tt("dve", tb[:], ai[:], bi_[:], ALU.mult, [ai, bi_], [tb])
                    kb.tt("dve", orr[:], ta[:], tb[:], ALU.subtract, [ta, tb], [orr])
                    kb.tt("dve", ta[:], ar[:], bi_[:], ALU.mult, [ar, bi_], [ta])
                    kb.tt("dve", tb[:], ai[:], br_[:], ALU.mult, [ai, br_], [tb])
                    kb.tt("dve", oi[:], ta[:], tb[:], ALU.add, [ta, tb], [oi])
                cmul(Lr[2], Li[2], Lr[1], Li[1], Lr[1], Li[1])
                cmul(Lr[3], Li[3], Lr[2], Li[2], Lr[1], Li[1])
                cmul(Lr[4], Li[4], Lr[2], Li[2], Lr[2], Li[2])
                kb.tt("dve", rho4[:], rho[:], rho[:], ALU.mult, [rho], [rho4])
                kb.tt("dve", rho4[:], rho4[:], rho4[:], ALU.mult, [rho4], [rho4])
                kb.ts("dve", tfv[:], fr[:], float(J), None, ALU.mult, None, [fr], [tfv])
                self.frac(fr4[:], tfv[:], t_i[:], t_f[:], [tfv], fr4, t_i, t_f)
                kb.ts("dve", xr[:], Lr[1][:], -1.0, None, ALU.add, None, [Lr[1]], [xr])
                kb.tt("dve", den[:], lr[:], lr[:], ALU.mult, [lr], [den])
                kb.tt("dve", ta[:], li[:], li[:], ALU.mult, [li], [ta])
                kb.tt("dve", den[:], den[:], ta[:], ALU.add, [den, ta], [den])
                kb.op("dve", lambda e: e.reciprocal(den[:], den[:]), [den], [den])
                kb.tt("dve", ta[:], xr[:], lr[:], ALU.mult, [xr, lr], [ta])
                kb.tt("dve", tb[:], Li[1][:], li[:], ALU.mult, [Li[1], li], [tb])
                kb.tt("dve", ta[:], ta[:], tb[:], ALU.add, [ta, tb], [ta])
                kb.tt("dve", cr[:], ta[:], den[:], ALU.mult, [ta, den], [cr])
                kb.tt("dve", ta[:], Li[1][:], lr[:], ALU.mult, [Li[1], lr], [ta])
                kb.tt("dve", tb[:], xr[:], li[:], ALU.mult, [xr, li], [tb])
                kb.tt("dve", ta[:], ta[:], tb[:], ALU.subtract, [ta, tb], [ta])
                kb.tt("dve", ci[:], ta[:], den[:], ALU.mult, [ta, den], [ci])
                bre = s1("bre", [128, 16, 16])
                bim = s1("bim", [128, 16, 16])
                bbr = s1("bbr", [128, 16, 16])
                bbi = s1("bbi", [128, 16, 16])
                tq = s1("tq", [128, 16, 16])
                Zre = s1("Zre", [128, 16, 128])
                Zim = s1("Zim", [128, 16, 128])
                Zjr = s1("Zjr", [128, 16, 128])
                Zji = s1("Zji", [128, 16, 128])
                Zt = s1("Zt", [128, 16, 128])
                kb.dma("sp", bre[:], dr["s5_bre_pk"][l], reads=[db["s5_bre_pk"]], writes=[bre])
                kb.dma("sp", bim[:], dr["s5_bim_pk"][l], reads=[db["s5_bim_pk"]], writes=[bim])
                crb = cr[:].unsqueeze(2).to_broadcast([128, 16, 16])
                cib = ci[:].unsqueeze(2).to_broadcast([128, 16, 16])
                kb.tt("dve", bbr[:], bre[:], crb, ALU.mult, [bre, cr], [bbr])
                kb.tt("dve", tq[:], bim[:], cib, ALU.mult, [bim, ci], [tq])
                kb.tt("dve", bbr[:], bbr[:], tq[:], ALU.subtract, [bbr, tq], [bbr])
                kb.tt("dve", bbi[:], bim[:], crb, ALU.mult, [bim, cr], [bbi])
                kb.tt("dve", tq[:], bre[:], cib, ALU.mult, [bre, ci], [tq])
                kb.tt("dve", bbi[:], bbi[:], tq[:], ALU.add, [bbi, tq], [bbi])
                kb.memset("dve", Zre[:], 0.0, [Zre])
                kb.memset("dve", Zim[:], 0.0, [Zim])
                for g2 in range(2):
                    rows = slice(g2 * 64, (g2 + 1) * 64)
                    for q in range(4):
                        col = (2 * q + g2) * 16
                        kb.copy("dve", Zre[rows, q::4, col:col + 16], bbr[rows, q::4, :], [bbr], [Zre])
                        kb.copy("dve", Zim[rows, q::4, col:col + 16], bbi[rows, q::4, :], [bbi], [Zim])
                Cre_f = s1("Cre_f", [128, 16, 128])
                Cimn_f = s1("Cimn_f", [128, 16, 128])
                cmk = s1("cmk", [128, 4, 128])
                kb.dma("sp", cmk[:], dr["s5_cmask"].rearrange("q p m -> p q m"), reads=[db["s5_cmask"]], writes=[cmk])
                c2 = [s1("c2_%d" % i, [128, 128]) for i in range(2)]
                ctr = [s1("ctr_%d" % i, [128, 128]) for i in range(2)]
                bi_ = 0
                for (nm, Cd, sgn) in (("s5_cre_pk", Cre_f, 1.0), ("s5_cim_pk", Cimn_f, -1.0)):
                    for ct in range(4):
                        cc = c2[ct % 2]
                        ctt = ctr[ct % 2]
                        kb.dma("sp", cc[:, 0:64], dr[nm][l, ct], reads=[db[nm]], writes=[cc])
                        kb.dma("sp", cc[:, 64:128], dr[nm][l, ct], reads=[db[nm]], writes=[cc])
                        ps = P[bi_ % 8]
                        bi_ += 1
                        kb.tr(ps[:, 0:128], cc[:], self.identf[:], [cc, self.identf], [ps])
                        kb.ts("dve", ctt[:], ps[:, 0:128], sgn, None, ALU.mult, None, [ps], [ctt])
                        for q in range(4):
                            kb.tt("dve", Cd[:, ct * 4 + q, :], ctt[:], cmk[:, q, :], ALU.mult, [ctt, cmk], [Cd])
                for j in range(J):
                    if j == 0:
                        zr_, zi_ = Zre, Zim
                    else:
                        ab = Lr[j][:].unsqueeze(2).to_broadcast([128, 16, 128])
                        bb_ = Li[j][:].unsqueeze(2).to_broadcast([128, 16, 128])
                        kb.tt("dve", Zjr[:], Zre[:], ab, ALU.mult, [Zre, Lr[j]], [Zjr])
                        kb.tt("dve", Zt[:], Zim[:], bb_, ALU.mult, [Zim, Li[j]], [Zt])
                        kb.tt("dve", Zjr[:], Zjr[:], Zt[:], ALU.subtract, [Zjr, Zt], [Zjr])
                        kb.tt("dve", Zji[:], Zim[:], ab, ALU.mult, [Zim, Lr[j]], [Zji])
                        kb.tt("dve", Zt[:], Zre[:], bb_, ALU.mult, [Zre, Li[j]], [Zt])
                        kb.tt("dve", Zji[:], Zji[:], Zt[:], ALU.add, [Zji, Zt], [Zji])
                        zr_, zi_ = Zjr, Zji
                    for (Z, BTd) in ((zr_, BTre[j]), (zi_, BTim[j])):
                        for g4 in range(4):
                            ps = P[bi_ % 8]
                            bi_ += 1
                            for jj in range(4):
                                gp = g4 * 4 + jj
                                kb.tr(ps[:, jj * 128:(jj + 1) * 128], Z[:, gp, :], self.identf[:], [Z, self.identf], [ps], sig=(jj == 3))
                            kb.copy("act", BTd[:, g4 * 4:(g4 + 1) * 4, :], ps[:].rearrange("p (j m) -> p j m", m=128), [ps], [BTd])
                    ps = P[bi_ % 8]
                    bi_ += 1
                    for ct in range(4):
                        for q in range(4):
                            gp = ct * 4 + q
                            kb.mm(ps[:, ct * 128:(ct + 1) * 128], zr_[:, gp, :], Cre_f[:, gp, :], start=(q == 0), stop=False,
                                  reads=[zr_, Cre_f], writes=[ps], sig=False)
                            kb.mm(ps[:, ct * 128:(ct + 1) * 128], zi_[:, gp, :], Cimn_f[:, gp, :], start=False, stop=(q == 3),
                                  reads=[zi_, Cimn_f], writes=[ps], sig=(q == 3))
                    kb.copy("act", Kt[:, :, j, :], ps[:].rearrange("p (c m) -> p c m", m=128), [ps], [Kt])
                for r in range(J):
                    ab = Lr[r + 1][:].unsqueeze(2).to_broadcast([128, 16, 128])
                    bb_ = Li[r + 1][:].unsqueeze(2).to_broadcast([128, 16, 128])
                    kb.tt("dve", Zjr[:], Cre_f[:], ab, ALU.mult, [Cre_f, Lr[r + 1]], [Zjr])
                    kb.tt("dve", Zt[:], Cimn_f[:], bb_, ALU.mult, [Cimn_f, Li[r + 1]], [Zt])
                    kb.tt("dve", W1[r][:], Zjr[:], Zt[:], ALU.add, [Zjr, Zt], [W1[r]])
                    kb.tt("dve", Zji[:], Cimn_f[:], ab, ALU.mult, [Cimn_f, Lr[r + 1]], [Zji])
                    kb.tt("dve", Zt[:], Cre_f[:], bb_, ALU.mult, [Cre_f, Li[r + 1]], [Zt])
                    kb.tt("dve", W2[r][:], Zji[:], Zt[:], ALU.subtract, [Zji, Zt], [W2[r]])
                io = s1("iota", [128, 512])
                kb.dma("sp", io[:], dr["iota1"], reads=[db["iota1"]], writes=[io])
                an = s1("an", [128, NBK])
                a_i = s1("a_i", [128, NBK], I32)
                a_f = s1("a_f", [128, NBK])
                a_r = s1("a_r", [128, NBK])
                for gp in range(16):
                    kb.ts("dve", an[:], io[:, 0:NBK], fr4[:, gp:gp + 1], None, ALU.mult, None, [io, fr4], [an])
                    self.frac(a_r[:], an[:], a_i[:], a_f[:], [an], a_r, a_i, a_f)
                    kb.act(sinT[:, gp, :], a_r[:], AF.Sin, [a_r], [sinT], scale=SIN_SCALE)
                    kb.ts("dve", an[:], a_r[:], 0.25, None, ALU.add, None, [a_r], [an])
                    self.frac(a_r[:], an[:], a_i[:], a_f[:], [an], a_r, a_i, a_f)
                    kb.act(cosT[:, gp, :], a_r[:], AF.Sin, [a_r], [cosT], scale=SIN_SCALE)
                kb.barrier()
            import os
            if os.environ.get("S5_STOP") == "setup":
                return
            sre_p = sbt("sre_p", [128, 16], F32)
            sim_p = sbt("sim_p", [128, 16], F32)
            kb.memset("dve", sre_p[:], 0.0, [sre_p])
            kb.memset("dve", sim_p[:], 0.0, [sim_p])
            SPr = [sbt("SPr%d" % i, [128, 16, NBK + 4], BF16) for i in range(2)]
            SPi = [sbt("SPi%d" % i, [128, 16, NBK + 4], BF16) for i in range(2)]
            for i in range(2):
                kb.memset("dve", SPr[i][:], 0.0, [SPr[i]])
                kb.memset("dve", SPi[i][:], 0.0, [SPi[i]])
            uT = [sbt("s5u%d" % i, [128, 4, 512], BF16) for i in range(2)]
            u4 = [sbt("s5u4_%d" % i, [128, 4, J, NBK], BF16) for i in range(2)]
            NB = 2

            def mk(name, dt=F32, n=NB, w=NBK):
                return [sbt("%s%d" % (name, i), [128, w], dt) for i in range(n)]
            t1, t2, t3, t4 = mk("s5t1"), mk("s5t2"), mk("s5t3"), mk("s5t4")
            zir, zii, zr, zi = mk("zir"), mk("zii"), mk("zr"), mk("zi")
            u1, u2, u3, u4_ = mk("u1"), mk("u2"), mk("u3"), mk("u4")
            yg = [sbt("yg%d" % i, [128, 4, 512], BF16) for i in range(2)]
            gx = mk("gx", w=512)
            gsq = mk("gsq", w=512)
            gin = mk("gin", w=512)
            gsg = mk("gsg", w=512)
            yo = [sbt("s5yo%d" % i, [128, 4, 512], BF16) for i in range(2)]
            psA = [P[0], P[1]]
            psB = [P[2], P[3]]
            psY = [P[4], P[5]]
            psG = [P[6], P[7]]
            for st in range(NT):
                tsl = slice(st * 512, (st + 1) * 512)
                ui = uT[st % 2]
                ud = u4[st % 2]
                spr, spi = SPr[st % 2], SPi[st % 2]
                spr_o, spi_o = SPr[(st + 1) % 2], SPi[(st + 1) % 2]
                kb.dma("sp", ui[:], dr["UT"][:, tsl].rearrange("(k p) n -> p k n", p=128), reads=[db["UT"]], writes=[ui])
                kb.copy("act", ud[:], ui[:].rearrange("p k (c r) -> p k r c", r=J), [ui], [ud])
                if st > 0:
                    kb.copy("dve", spr[:, :, 0:1], spr_o[:, :, NBK:NBK + 1], [spr_o], [spr])
                    kb.copy("dve", spi[:, :, 0:1], spi_o[:, :, NBK:NBK + 1], [spi_o], [spi])
                ygt = yg[st % 2]
                for ct in range(4):
                    pY = psY[ct % 2]

                    def body(q, j):
                        gp = ct * 4 + q
                        pa, pb = psA[j], psB[j]
                        cg = cosT[:, gp, :]
                        sg = sinT[:, gp, :]
                        for tp in range(J):
                            kb.mm(pa[:, 0:NBK], BTre[tp][:, gp, :], ud[:, ct, J - 1 - tp, :], start=(tp == 0), stop=(tp == J - 1),
                                  reads=[BTre[tp], ud], writes=[pa])
                        for tp in range(J):
                            kb.mm(pb[:, 0:NBK], BTim[tp][:, gp, :], ud[:, ct, J - 1 - tp, :], start=(tp == 0), stop=(tp == J - 1),
                                  reads=[BTim[tp], ud], writes=[pb])
                        yield
                        kb.tt("dve", t1[j][:], pa[:, 0:NBK], cg, ALU.mult, [pa, cosT], [t1[j]])
                        yield
                        kb.tt("dve", t2[j][:], pb[:, 0:NBK], sg, ALU.mult, [pb, sinT], [t2[j]])
                        yield
                        kb.tt("dve", t3[j][:], pb[:, 0:NBK], cg, ALU.mult, [pb, cosT], [t3[j]])
                        yield
                        kb.tt("dve", t4[j][:], pa[:, 0:NBK], sg, ALU.mult, [pa, sinT], [t4[j]])
                        yield
                        kb.tt("dve", zir[j][:], t1[j][:], t2[j][:], ALU.add, [t1[j], t2[j]], [zir[j]])
                        yield
                        kb.tt("dve", zii[j][:], t3[j][:], t4[j][:], ALU.subtract, [t3[j], t4[j]], [zii[j]])
                        yield
                        rb = rho4[:, gp:gp + 1].to_broadcast([128, NBK])
                        kb.op("dve", lambda e: e.tensor_tensor_scan(zr[j][:], rb, zir[j][:], sre_p[:, gp:gp + 1], ALU.mult, ALU.add),
                              [rho4, zir[j], sre_p], [zr[j]])
                        yield
                        kb.op("dve", lambda e: e.tensor_tensor_scan(zi[j][:], rb, zii[j][:], sim_p[:, gp:gp + 1], ALU.mult, ALU.add),
                              [rho4, zii[j], sim_p], [zi[j]])
                        yield
                        kb.tt("dve", u1[j][:], zr[j][:], cg, ALU.mult, [zr[j], cosT], [u1[j]])
                        yield
                        kb.tt("dve", u2[j][:], zi[j][:], sg, ALU.mult, [zi[j], sinT], [u2[j]])
                        yield
                        kb.tt("dve", u3[j][:], zr[j][:], sg, ALU.mult, [zr[j], sinT], [u3[j]])
                        yield
                        kb.tt("dve", u4_[j][:], zi[j][:], cg, ALU.mult, [zi[j], cosT], [u4_[j]])
                        yield
                        kb.tt("dve", spr[:, gp, 1:NBK + 1], u1[j][:], u2[j][:], ALU.subtract, [u1[j], u2[j]], [spr])
                        yield
                        kb.tt("dve", spi[:, gp, 1:NBK + 1], u3[j][:], u4_[j][:], ALU.add, [u3[j], u4_[j]], [spi])
                        yield
                        kb.tt("dve", sre_p[:, gp:gp + 1], u1[j][:, NBK - 1:NBK], u2[j][:, NBK - 1:NBK], ALU.subtract, [u1[j], u2[j]], [sre_p])
                        kb.tt("dve", sim_p[:, gp:gp + 1], u3[j][:, NBK - 1:NBK], u4_[j][:, NBK - 1:NBK], ALU.add, [u3[j], u4_[j]], [sim_p])
                        yield

                    for qq in range(0, 4, 2):
                        gens = [body(qq, 0), body(qq + 1, 1)]
                        while gens:
                            for g_ in list(gens):
                                try:
                                    next(g_)
                                except StopIteration:
                                    gens.remove(g_)
                    if os.environ.get("S5_STOP") == "up":
                        continue
                    for r in range(J):
                        osl = slice(r * NBK, (r + 1) * NBK)
                        n_mm = 8 + r + 1
                        i_mm = 0
                        for q in range(4):
                            gp = ct * 4 + q
                            kb.mm(pY[:, osl], W1[r][:, gp, :], spr[:, gp, 0:NBK], start=(i_mm == 0), stop=False,
                                  reads=[W1[r], spr], writes=[pY], sig=False)
                            i_mm += 1
                            kb.mm(pY[:, osl], W2[r][:, gp, :], spi[:, gp, 0:NBK], start=False, stop=False,
                                  reads=[W2[r], spi], writes=[pY], sig=False)
                            i_mm += 1
                        for tp in range(r + 1):
                            i_mm += 1
                            kb.mm(pY[:, osl], Kt[:, ct, tp, :], ud[:, ct, r - tp, :], start=False, stop=(i_mm == n_mm),
                                  reads=[Kt, ud], writes=[pY], sig=(i_mm == n_mm))
                    jj = ct % 2
                    kb.stt("dve", gx[jj][:].rearrange("p (c r) -> p c r", r=J), ui[:, ct, :].rearrange("p (c r) -> p c r", r=J),
                           dsk[:, ct:ct + 1], pY[:].rearrange("p (r c) -> p c r", r=J), ALU.mult, ALU.add, [ui, dsk, pY], [gx[jj]])
                    kb.act(gsq[jj][:], gx[jj][:], AF.Square, [gx[jj]], [gsq[jj]])
                    kb.ts("dve", gsq[jj][:], gsq[jj][:], 0.044715, 1.0, ALU.mult, ALU.add, [gsq[jj]], [gsq[jj]])
                    kb.tt("dve", gin[jj][:], gsq[jj][:], gx[jj][:], ALU.mult, [gsq[jj], gx[jj]], [gin[jj]])
                    kb.act(gsg[jj][:], gin[jj][:], AF.Sigmoid, [gin[jj]], [gsg[jj]], scale=1.5957691216057308)
                    kb.tt("dve", ygt[:, ct, :], gx[jj][:], gsg[jj][:], ALU.mult, [gx[jj], gsg[jj]], [ygt])
                if os.environ.get("S5_STOP") == "up":
                    continue
                yot = yo[st % 2]
                for ob in range(4):
                    pG = psG[ob % 2]
                    for k in range(4):
                        kb.mm(pG[:], wglu[:, k, ob * 128:(ob + 1) * 128], ygt[:, k, :], start=(k == 0), stop=(k == 3),
                              reads=[wglu, ygt], writes=[pG])
                    jj = ob % 2
                    kb.act(gsg[jj][:], pG[:], AF.Sigmoid, [pG], [gsg[jj]])
                    kb.tt("dve", yot[:, ob, :], ygt[:, ob, :], gsg[jj][:], ALU.mult, [ygt, gsg[jj]], [yot])
                kb.dma("sp", dr["YB"][0:512, tsl].rearrange("(k p) n -> p k n", p=128), yot[:], reads=[yot], writes=[db["YB"]])
            kb.barrier()

    def phase_c(self, l):
        kb = self.kb
        dr, db = self.dram, self.dbuf
        xsrc = "xT" if l == 0 else "XS"
        W = 512
        with ExitStack() as es:
            def sbt(name, shape, dt):
                return kb.sb(name, shape, dt, es)
            wg = sbt("wg", [128, 8, 4 * D], BF16)
            wbr = sbt("wbr", [128, 16, D], BF16)
            wo = sbt("wo", [128, 8, D], BF16)
            bg = sbt("bg", [128, 32], F32)
            kb.dma("sp", bg[:], dr["b_gate_pk"][l], reads=[db["b_gate_pk"]], writes=[bg])
            for j in range(8):
                kb.dma("pool", wg[:, :, j * 512:(j + 1) * 512], dr["w_gate"][l, :, j * 512:(j + 1) * 512].rearrange("(k p) n -> p k n", p=128),
                       reads=[db["w_gate"]], writes=[wg])
            for j in range(2):
                kb.dma("pool", wbr[:, :, j * 512:(j + 1) * 512], dr["w_branch"][l, :, j * 512:(j + 1) * 512].rearrange("(k p) n -> p k n", p=128),
                       reads=[db["w_branch"]], writes=[wbr])
                kb.dma("pool", wo[:, :, j * 512:(j + 1) * 512], dr["w_out"][l, :, j * 512:(j + 1) * 512].rearrange("(k p) n -> p k n", p=128),
                       reads=[db["w_out"]], writes=[wo])
            xt = [sbt("cx%d" % i, [128, 8, W], F32) for i in range(1)] * 2
            yt = [sbt("cy%d" % i, [128, 16, W], BF16) for i in range(1)] * 2
            sq = sbt("csq", [128, 8, W], BF16)
            tmp = sbt("ctmp", [128, 8, W], F32)
            rstd = sbt("crstd", [128, W], F32)
            hb = sbt("chb", [128, 8, W], BF16)
            mt = sbt("cm", [128, 8, W], BF16)
            gs = [sbt("cg%d" % i, [128, W], F32) for i in range(2)]
            pr = [sbt("cp%d" % i, [128, W], F32) for i in range(2)]
            macc = [sbt("cma%d" % i, [128, W], F32) for i in range(2)]
            gi = 0
            hbs = [hb, hb]
            NTC = SEQ // W

            def prep(t):
                tsl_ = slice(t * W, (t + 1) * W)
                x_t_ = xt[t % 2]
                y_t_ = yt[t % 2]
                kb.dma("sp", x_t_[:], dr[xsrc][:, tsl_].rearrange("(k p) n -> p k n", p=128), reads=[db[xsrc]], writes=[x_t_])
                kb.dma("sp", y_t_[:], dr["YB"][:, tsl_].rearrange("(k p) n -> p k n", p=128), reads=[db["YB"]], writes=[y_t_])
                self.h_tile(x_t_, None, l, 0, sq, rstd, tmp, W=W)
                for k in range(8):
                    kb.act(hbs[t % 2][:, k, :], tmp[:, k, :], AF.Identity, [tmp, self.mod], [hbs[t % 2]], bias=self.mod[:, l, k:k + 1])
            prep(0)
            for t in range(NTC):
                tsl = slice(t * W, (t + 1) * W)
                x_t = xt[t % 2]
                y_t = yt[t % 2]
                hb = hbs[t % 2]
                if t > 0:
                    prep(t)
                for d in range(8):
                    ma = macc[d % 2]
                    for n in range(4):
                        pg = self.bank()
                        for k in range(8):
                            kb.mm(pg[:, 0:W], wg[:, k, n * D + d * 128:n * D + (d + 1) * 128], hb[:, k, :], start=(k == 0), stop=(k == 7),
                                  reads=[wg, hb], writes=[pg])
                        pb = self.bank()
                        for k in range(4):
                            kb.mm(pb[:, 0:W], wbr[:, n * 4 + k, d * 128:(d + 1) * 128], y_t[:, n * 4 + k, :], start=(k == 0), stop=(k == 3),
                                  reads=[wbr, y_t], writes=[pb])
                        g = gs[gi % 2]
                        p_ = pr[gi % 2]
                        gi += 1
                        kb.act(g[:], pg[:, 0:W], AF.Sigmoid, [pg, bg], [g], bias=bg[:, n * 8 + d:n * 8 + d + 1])
                        if n == 0:
                            kb.tt("dve", ma[:], g[:], pb[:, 0:W], ALU.mult, [g, pb], [ma])
                        else:
                            kb.tt("dve", p_[:], g[:], pb[:, 0:W], ALU.mult, [g, pb], [p_])
                            if n < 3:
                                kb.tt("dve", ma[:], ma[:], p_[:], ALU.add, [ma, p_], [ma])
                            else:
                                kb.tt("dve", mt[:, d, :], ma[:], p_[:], ALU.add, [ma, p_], [mt])
                for ob in range(8):
                    po = self.bank()
                    for d in range(8):
                        kb.mm(po[:, 0:W], wo[:, d, ob * 128:(ob + 1) * 128], mt[:, d, :], start=(d == 0), stop=(d == 7),
                              reads=[wo, mt], writes=[po])
                    kb.stt("dve", x_t[:, ob, :], po[:, 0:W], self.mod[:, l, 16 + ob:16 + ob + 1], x_t[:, ob, :], ALU.mult, ALU.add,
                           [po, self.mod, x_t], [x_t])
                kb.dma("sp", dr["XS"][:, tsl].rearrange("(k p) n -> p k n", p=128), x_t[:], reads=[x_t], writes=[db["XS"]])
            kb.barrier()

    def phase_moe(self, l):
        kb = self.kb
        dr, db = self.dram, self.dbuf
        MT = 1024
        with ExitStack() as es:
            def sbt(name, shape, dt):
                return kb.sb(name, shape, dt, es)
            acc = sbt("acc", [128, 8, MT], F32)
            h2 = sbt("h2", [128, 8, MT], BF16)
            actb = sbt("actb", [128, 16, MT], BF16)
            w2g = sbt("w2g", [128, 16, D], BF16)
            w13 = [sbt("w13_%d" % i, [128, 8, 512], BF16) for i in range(2)]
            combT = sbt("combT", [32, MT], F32)
            cbs = [sbt("cbs%d" % i, [128, 512], F32) for i in range(4)]
            cbi = 0
            wr = sbt("wr", [128, 8, 36], F32)
            br = sbt("br", [128, 36], F32)
            kb.dma("sp", wr[:], dr["moe_wr"][l].rearrange("(k p) n -> p k n", p=128), reads=[db["moe_wr"]], writes=[wr])
            kb.dma("sp", br[:], dr["moe_br"][l].partition_broadcast(128), reads=[db["moe_br"]], writes=[br])
            sq = sbt("msq", [128, 8, 512], F32)
            tmp = sbt("mtmp", [128, 8, 512], F32)
            rstd = sbt("mrstd", [128, 512], F32)
            sl_ = [sbt("msl%d" % i, [128, 512], F32) for i in range(2)]
            tl_ = [sbt("mtl%d" % i, [128, 512], F32) for i in range(2)]
            lg = sbt("lg", [128, 36], F32)
            gmax = sbt("gmax", [128, 1], F32)
            ngm = sbt("ngm", [128, 1], F32)
            goh = sbt("goh", [128, 4], F32)
            gex = sbt("gex", [128, 4], F32)
            gsum = sbt("gsum", [128, 1], F32)
            gw = sbt("gw", [128, 1], F32)
            e3 = sbt("e3", [128, 4, 8], F32)
            esel = sbt("esel", [128, 8], F32)
            m1 = sbt("m1", [128, 1], F32)
            m2 = sbt("m2", [128, 1], F32)
            oh1 = sbt("oh1", [128, 8], F32)
            oh2 = sbt("oh2", [128, 8], F32)
            e2 = sbt("e2", [128, 8], F32)
            dd = sbt("dd", [128, 1], F32)
            ed = sbt("ed", [128, 1], F32)
            den = sbt("rden", [128, 1], F32)
            w1_ = sbt("rw1", [128, 1], F32)
            w2_ = sbt("rw2", [128, 1], F32)
            c1 = sbt("rc1", [128, 8], F32)
            ce = sbt("rce", [128, 8], F32)
            comb = sbt("comb", [128, 32], F32)
            si = 0
            wi = 0
            for mt_ in range(SEQ // MT):
                msl = slice(mt_ * MT, (mt_ + 1) * MT)
                kb.dma("sp", acc[:], dr["XS"][:, msl].rearrange("(k p) n -> p k n", p=128), reads=[db["XS"]], writes=[acc])
                for hf in range(MT // 512):
                    hs = slice(hf * 512, (hf + 1) * 512)
                    xv = T(acc.t[:, :, hs], "accv")
                    xv.b = acc.b
                    self.h_tile(xv, None, l, 1, sq, rstd, tmp, W=512, fp32_sq=True)
                    for k in range(8):
                        kb.ts("dve", tmp[:, k, :], tmp[:, k, :], self.mod[:, l, 24 + k:24 + k + 1], None, ALU.add, None,
                              [tmp, self.mod], [tmp])
                        kb.copy("act", h2[:, k, hs], tmp[:, k, :], [tmp], [h2])
                    for sub in range(4):
                        ss = slice(sub * 128, (sub + 1) * 128)
                        ps = self.bank()
                        for k in range(8):
                            kb.mm(ps[:, 0:36], tmp[:, k, ss], wr[:, k, :], start=(k == 0), stop=(k == 7), reads=[tmp, wr], writes=[ps])
                        kb.tt("dve", lg[:], ps[:, 0:36], br[:], ALU.add, [ps, br], [lg])
                        kb.op("dve", lambda e: e.tensor_reduce(gmax[:], lg[:, 0:4], AX.X, ALU.max), [lg], [gmax])
                        kb.ts("dve", goh[:], lg[:, 0:4], gmax[:, 0:1], None, ALU.is_equal, None, [lg, gmax], [goh])
                        kb.ts("dve", ngm[:], gmax[:], -1.0, None, ALU.mult, None, [gmax], [ngm])
                        kb.memset("dve", gsum[:], 0.0, [gsum])
                        kb.act(gex[:], lg[:, 0:4], AF.Exp, [lg, ngm], [gex, gsum], bias=ngm[:, 0:1], accum_out=gsum[:])
                        kb.op("dve", lambda e: e.reciprocal(gw[:], gsum[:]), [gsum], [gw])
                        kb.tt("dve", e3[:], lg[:, 4:36].rearrange("p (g e) -> p g e", e=8), goh[:].unsqueeze(2).to_broadcast([128, 4, 8]),
                              ALU.mult, [lg, goh], [e3])
                        kb.op("dve", lambda e: e.tensor_reduce(esel[:], e3[:].rearrange("p g e -> p e g"), AX.X, ALU.add), [e3], [esel])
                        kb.op("dve", lambda e: e.tensor_reduce(m1[:], esel[:], AX.X, ALU.max), [esel], [m1])
                        kb.ts("dve", oh1[:], esel[:], m1[:, 0:1], None, ALU.is_equal, None, [esel, m1], [oh1])
                        kb.stt("dve", e2[:], oh1[:], -1e30, esel[:], ALU.mult, ALU.add, [oh1, esel], [e2])
                        kb.op("dve", lambda e: e.tensor_reduce(m2[:], e2[:], AX.X, ALU.max), [e2], [m2])
                        kb.ts("dve", oh2[:], e2[:], m2[:, 0:1], None, ALU.is_equal, None, [e2, m2], [oh2])
                        kb.tt("dve", dd[:], m2[:], m1[:], ALU.subtract, [m2, m1], [dd])
                        kb.act(ed[:], dd[:], AF.Exp, [dd], [ed])
                        kb.ts("dve", den[:], ed[:], 1.0, None, ALU.add, None, [ed], [den])
                        kb.op("dve", lambda e: e.reciprocal(den[:], den[:]), [den], [den])
                        kb.tt("dve", w1_[:], den[:], gw[:], ALU.mult, [den, gw], [w1_])
                        kb.tt("dve", w2_[:], w1_[:], ed[:], ALU.mult, [w1_, ed], [w2_])
                        kb.ts("dve", c1[:], oh1[:], w1_[:, 0:1], None, ALU.mult, None, [oh1, w1_], [c1])
                        kb.stt("dve", ce[:], oh2[:], w2_[:, 0:1], c1[:], ALU.mult, ALU.add, [oh2, w2_, c1], [ce])
                        kb.tt("dve", comb[:].rearrange("p (g e) -> p g e", e=8), goh[:].unsqueeze(2).to_broadcast([128, 4, 8]),
                              ce[:].unsqueeze(1).to_broadcast([128, 4, 8]), ALU.mult, [goh, ce], [comb])
                        pt = self.bank()
                        kb.tr(pt[0:32, 0:128], comb[:], self.identf[:], [comb, self.identf], [pt])
                        kb.copy("act", combT[:, hf * 512 + sub * 128:hf * 512 + (sub + 1) * 128], pt[0:32, 0:128], [pt], [combT])
                cmb_d = dr["CMB"][mt_ % 2]
                kb.dma("sp", cmb_d, combT[:], reads=[combT], writes=[db["CMB"]])
                for grp in range(4):
                    kb.dma("sp", w2g[:].rearrange("p k n -> p (k n)"), dr["MW2"][l, grp], reads=[db["MW2"]], writes=[w2g])
                    for e in range(8):
                        ge = grp * 8 + e
                        w = w13[wi % 2]
                        wi += 1
                        kb.dma("sp", w[:].rearrange("p k n -> p (k n)"), dr["MW13"][l, ge], reads=[db["MW13"]], writes=[w])
                        for ts_ in range(MT // 512):
                            tsl = slice(ts_ * 512, (ts_ + 1) * 512)
                            pcb = cbs[cbi % 4]
                            cbi += 1
                            kb.dma("sp", pcb[:], cmb_d[ge:ge + 1, tsl].partition_broadcast(128), reads=[db["CMB"]], writes=[pcb])
                            for fb in range(2):
                                p1 = self.bank()
                                for k in range(8):
                                    kb.mm(p1[:], w[:, k, fb * 128:(fb + 1) * 128], h2[:, k, tsl], start=(k == 0), stop=(k == 7), reads=[w, h2], writes=[p1])
                                p3 = self.bank()
                                for k in range(8):
                                    kb.mm(p3[:], w[:, k, 256 + fb * 128:256 + (fb + 1) * 128], h2[:, k, tsl], start=(k == 0), stop=(k == 7), reads=[w, h2], writes=[p3])
                                sl = sl_[si % 2]
                                tl = tl_[si % 2]
                                si += 1
                                kb.act(sl[:], p1[:], AF.Silu, [p1], [sl])
                                kb.tt("dve", tl[:], sl[:], p3[:], ALU.mult, [sl, p3], [tl])
                                kb.tt("dve", actb[:, e * 2 + fb, tsl], tl[:], pcb[:], ALU.mult, [tl, pcb], [actb])
                    for ob in range(8):
                        for ts_ in range(MT // 512):
                            tsl = slice(ts_ * 512, (ts_ + 1) * 512)
                            po = self.bank()
                            for kk in range(16):
                                kb.mm(po[:], w2g[:, kk, ob * 128:(ob + 1) * 128], actb[:, kk, tsl], start=(kk == 0), stop=(kk == 15),
                                      reads=[w2g, actb], writes=[po])
                            kb.stt("dve", acc[:, ob, tsl], po[:], self.mod[:, l, 40 + ob:40 + ob + 1], acc[:, ob, tsl], ALU.mult, ALU.add,
                                   [po, self.mod, acc], [acc])
                kb.dma("sp", dr["XS"][:, msl].rearrange("(k p) n -> p k n", p=128), acc[:], reads=[acc], writes=[db["XS"]])
            kb.barrier()

    def phase_final(self):
        kb = self.kb
        dr, db = self.dram, self.dbuf
        with ExitStack() as es:
            fw = kb.sb("fw", [128, 8], F32, es)
            kb.dma("sp", fw[:], dr["fnorm_pk"], reads=[db["fnorm_pk"]], writes=[fw])
            xt = [kb.sb("fx%d" % i, [128, 8, 512], F32, es) for i in range(2)]
            ot = [kb.sb("fo%d" % i, [128, 8, 512], F32, es) for i in range(2)]
            sq = kb.sb("fsq", [128, 8, 512], F32, es)
            rstd = kb.sb("frs", [128, 512], F32, es)
            for t in range(NT):
                tsl = slice(t * 512, (t + 1) * 512)
                x_t = xt[t % 2]
                o_t = ot[t % 2]
                kb.dma("sp", x_t[:], dr["XS"][:, tsl].rearrange("(k p) n -> p k n", p=128), reads=[db["XS"]], writes=[x_t])
                ps = self.bank()
                kb.act(sq[:], x_t[:], AF.Square, [x_t], [sq])
                for k in range(8):
                    kb.mm(ps[:], self.ones_df[:], sq[:, k, :], start=(k == 0), stop=(k == 7), reads=[self.ones_df, sq], writes=[ps])
                kb.act(rstd[:], ps[:], AF.Ln, [ps], [rstd], bias=EPS)
                kb.act(rstd[:], rstd[:], AF.Exp, [rstd], [rstd], scale=-0.5)
                for k in range(8):
                    kb.stt("dve", o_t[:, k, :], x_t[:, k, :], fw[:, k:k + 1], rstd[:], ALU.mult, ALU.mult, [x_t, fw, rstd], [o_t])
                kb.dma("sp", dr["outT"][:, tsl].rearrange("(k p) n -> p k n", p=128), o_t[:], reads=[o_t], writes=[db["outT"]])
            kb.barrier()


def _prep_inputs(inputs, b):
    f = lambda a: np.ascontiguousarray(a, dtype=np.float32)
    m = {}
    m["xT"] = f(inputs["x"][b].T)
    m["c_pk"] = f(inputs["c"][b].reshape(8, 128).T)
    m["pos"] = np.ascontiguousarray(inputs["positions"][b][None, :].astype(np.int32))
    m["ada_w"] = f(inputs["ada_w"])
    m["ada_b_pk"] = f(inputs["ada_b"].reshape(DEPTH, 6, 8, 128).transpose(0, 3, 1, 2).reshape(DEPTH, 128, 48))
    m["w_fm"] = f(inputs["w_in"][:, :, _fm_cols()])
    m["w_tm"] = f(inputs["w_in"][:, :, _tm_cols()])
    m["hg_lb_pk"] = f(inputs["hg_lb_logits"].reshape(DEPTH, 4, 128).transpose(2, 0, 1))
    m["conv_w_pk"] = f(inputs["m2_conv_w"].reshape(DEPTH, 4, 8, 128).transpose(0, 3, 2, 1))
    m["conv_b_pk"] = f(inputs["m2_conv_b"].reshape(DEPTH, 8, 128).transpose(0, 2, 1))
    m["dt_bias"] = f(inputs["m2_dt_bias"].reshape(DEPTH, 1, 8))
    m["hg_norm_pk"] = f(inputs["hg_norm_w"].T)
    pk = lambda a: a.reshape(DEPTH, 16, 2, 64).transpose(0, 2, 3, 1).reshape(DEPTH, 128, 16)
    m["s5_lr_pk"] = f(pk(inputs["s5_lam_re"]))
    m["s5_li_pk"] = f(pk(inputs["s5_lam_im"]))
    m["s5_ldt_pk"] = f(np.broadcast_to(inputs["s5_log_dt"].reshape(DEPTH, 16, 2).transpose(0, 2, 1)[:, :, None, :],
                                       (DEPTH, 2, 64, 16)).reshape(DEPTH, 128, 16))
    pkb = lambda a: a.reshape(DEPTH, 16, 2, 64, 16).transpose(0, 2, 3, 1, 4).reshape(DEPTH, 128, 16, 16)
    m["s5_bre_pk"] = f(pkb(inputs["s5_b_re"]))
    m["s5_bim_pk"] = f(pkb(inputs["s5_b_im"]))
    m["s5_cre_pk"] = f(inputs["s5_c_re"].reshape(DEPTH, 4, 128, 64))
    m["s5_cim_pk"] = f(inputs["s5_c_im"].reshape(DEPTH, 4, 128, 64))
    m["s5_d_pk"] = f(inputs["s5_d"].reshape(DEPTH, 4, 128).transpose(0, 2, 1))
    m["s5_wglu"] = f(inputs["s5_w_glu"])
    m["w_gate"] = f(inputs["w_gate"])
    m["b_gate_pk"] = f(inputs["b_gate"].reshape(DEPTH, 32, 128).transpose(0, 2, 1))
    m["w_branch"] = f(inputs["w_branch"].reshape(DEPTH, 4 * 512, D))
    m["w_out"] = f(inputs["w_out"])
    m["moe_wr"] = f(np.concatenate([inputs["moe_w_group"], inputs["moe_w_expert"]], axis=2))
    m["moe_br"] = f(np.concatenate([inputs["moe_b_group"], inputs["moe_b_expert"]], axis=1).reshape(DEPTH, 1, 36))
    w13 = np.concatenate([inputs["moe_w1"].reshape(DEPTH, 32, 8, 128, 256), inputs["moe_w3"].reshape(DEPTH, 32, 8, 128, 256)], axis=4)
    m["moe_w13_pk"] = f(w13.transpose(0, 1, 3, 2, 4).reshape(DEPTH, 32, 128, 8 * 512))
    m["moe_w2_pk"] = f(inputs["moe_w2"].reshape(DEPTH, 4, 16, 128, D).transpose(0, 1, 3, 2, 4).reshape(DEPTH, 4, 128, 16 * D))
    m["fnorm_pk"] = f(inputs["final_norm_w"].reshape(8, 128).T)
    m["m2_alog"] = f(inputs["m2_a_log"].reshape(DEPTH, 1, 8))
    m["m2_d"] = f(inputs["m2_d"].reshape(DEPTH, 1, 8))
    m["m2_normw"] = f(inputs["m2_norm_w"].reshape(DEPTH, 1, 512))
    m.update(_const_tables())
    return m


def run(inputs, batches=(0, 1, 2, 3), core_ids=None, debug=None, upto="all", trace=False):
    prog = Prog(debug=debug, upto=upto)
    nc = prog.build()
    in_maps = [_prep_inputs(inputs, b) for b in batches]
    if core_ids is None:
        core_ids = list(range(len(batches)))
    if trace:
        return run_bass_kernel_spmd(nc, in_maps, core_ids=core_ids, trace=True)
    res = run_bass_kernel_spmd(nc, in_maps, core_ids=core_ids)
    return res.results


def kernel(**inputs):
    inputs = {k: np.asarray(v) for k, v in inputs.items()}
    results = run(inputs, batches=(0, 1, 2, 3), core_ids=[0, 2, 4, 6])
    out = np.stack([np.ascontiguousarray(r["outT"].T) for r in results], axis=0)
    return out.astype(np.float32)
```

```python
import numpy as np
from contextlib import ExitStack
import concourse.bass as bass
import concourse.mybir as mybir
from concourse.bass_utils import run_bass_kernel_spmd

F32 = mybir.dt.float32
BF16 = mybir.dt.bfloat16
I32 = mybir.dt.int32
AF = mybir.ActivationFunctionType
ALU = mybir.AluOpType
AX = mybir.AxisListType

D = 1024
SEQ = 4096
DEPTH = 2
NT = SEQ // 512
EPS = 1e-6
TWO_PI = 6.283185307179586
SIN_SCALE = 6.28318


class Buf:
    __slots__ = ("name", "w", "r", "excl")

    def __init__(self, name=""):
        self.name = name
        self.w = None
        self.r = []
        self.excl = False


class T:
    __slots__ = ("t", "b")

    def __init__(self, t, name=""):
        self.t = t
        self.b = Buf(name)

    def __getitem__(self, k):
        return self.t[k]


class KB:
    NDMA = 48

    def __init__(self, nc, es):
        self.nc = nc
        self.es = es
        self.engs = {"pe": nc.tensor, "act": nc.scalar, "dve": nc.vector,
                     "pool": nc.gpsimd, "sp": nc.sync}
        self.done = {e: es.enter_context(nc.semaphore("done_" + e)) for e in self.engs}
        self.cnt = {e: 0 for e in self.engs}
        self.waited = {e: {} for e in self.engs}
        self.pending = {e: [] for e in self.engs}
        self.dma_sems = [es.enter_context(nc.semaphore("dma%d" % i)) for i in range(self.NDMA)]
        self.dma_cnt = [0] * self.NDMA
        self.next_dma = 0
        self.sw_sems = [es.enter_context(nc.semaphore("swd%d" % i)) for i in range(8)]
        self.sw_cnt = [0] * 8
        self.next_sw = 0
        self.bg_sems = [es.enter_context(nc.semaphore("bg%d" % i)) for i in range(8)]
        self.bg_cnt = [0] * 8
        self.next_bg = 0
        self.ninst = {e: 0 for e in self.engs}
        self.uid = 0

    def sb(self, name, shape, dtype, es=None):
        self.uid += 1
        es = es or self.es
        return T(es.enter_context(self.nc.sbuf_tensor("%s_%d" % (name, self.uid), list(shape), dtype)), name)

    def ps(self, name, shape, dtype, es=None):
        self.uid += 1
        es = es or self.es
        t = T(es.enter_context(self.nc.psum_tensor("%s_%d" % (name, self.uid), list(shape), dtype)), name)
        t.b.excl = True
        return t

    def _wait(self, eng, deps):
        w = self.waited[eng]
        best = {}
        for d in deps:
            if d is None:
                continue
            sem, val = d
            if eng == "pe" and sem is self.done["pe"]:
                continue
            k = id(sem)
            if w.get(k, 0) >= val:
                continue
            if k not in best or best[k][1] < val:
                best[k] = (sem, val)
        for k, (sem, val) in best.items():
            self.engs[eng].wait_ge(sem, val)
            self.ninst[eng] += 1
            w[k] = val

    @staticmethod
    def _bufs(xs):
        return [x.b if isinstance(x, T) else x for x in xs]

    def _deps(self, reads, writes):
        deps = []
        for b in reads:
            if b.w is not None:
                deps.append(b.w)
            if b.excl:
                deps.extend(b.r)
        for b in writes:
            if b.w is not None:
                deps.append(b.w)
            deps.extend(b.r)
        return deps

    @staticmethod
    def _prune(evs):
        best = {}
        for sem, val in evs:
            k = id(sem)
            if k not in best or best[k][1] < val:
                best[k] = (sem, val)
        return list(best.values())

    def _record(self, ev, reads, writes):
        for b in reads:
            b.r.append(ev)
            if len(b.r) > 16:
                b.r = self._prune(b.r)
        for b in writes:
            b.w = ev
            b.r = []

    def op(self, eng, fn, reads=(), writes=(), sig=True):
        reads = self._bufs(reads)
        writes = self._bufs(writes)
        self._wait(eng, self._deps(reads, writes))
        inst = fn(self.engs[eng])
        self.ninst[eng] += 1
        if not sig:
            self.pending[eng].append((reads, writes))
            return inst
        self.cnt[eng] += 1
        inst.then_inc(self.done[eng], 1)
        ev = (self.done[eng], self.cnt[eng])
        for rs, ws in self.pending[eng]:
            self._record(ev, rs, ws)
        self.pending[eng] = []
        self._record(ev, reads, writes)
        return inst

    def dma(self, q, out, in_, reads=(), writes=(), bg=False, **kw):
        reads = self._bufs(reads)
        writes = self._bufs(writes)
        self._wait(q, self._deps(reads, writes))
        if bg:
            i = self.next_bg
            self.next_bg = (i + 1) % len(self.bg_sems)
            s = self.bg_sems[i]
            cnts = self.bg_cnt
        elif q == "pool":
            i = self.next_sw
            self.next_sw = (i + 1) % len(self.sw_sems)
            s = self.sw_sems[i]
            cnts = self.sw_cnt
        else:
            i = self.next_dma
            self.next_dma = (i + 1) % self.NDMA
            s = self.dma_sems[i]
            cnts = self.dma_cnt
        if cnts[i] > 0:
            self._wait(q, [(s, 16 * cnts[i])])
        inst = self.engs[q].dma_start(out=out, in_=in_, **kw)
        self.ninst[q] += 1
        cnts[i] += 1
        inst.then_inc(s, 16)
        ev = (s, 16 * cnts[i])
        self._record(ev, reads, writes)
        return ev

    def all_events(self):
        evs = [(self.done[e], self.cnt[e]) for e in self.engs if self.cnt[e]]
        evs += [(self.dma_sems[i], 16 * self.dma_cnt[i]) for i in range(self.NDMA) if self.dma_cnt[i]]
        evs += [(self.sw_sems[i], 16 * self.sw_cnt[i]) for i in range(len(self.sw_sems)) if self.sw_cnt[i]]
        return evs

    def barrier(self):
        for e in self.engs:
            assert not self.pending[e], "pending unsignaled ops at barrier"
        evs = self.all_events()
        for e in self.engs:
            self._wait(e, evs)

    def act(self, out, in_, func, reads, writes, bias=None, scale=None, accum_out=None):
        kw = {}
        if bias is not None:
            kw["bias"] = bias
        if scale is not None:
            kw["scale"] = scale
        if accum_out is not None:
            kw["accum_out"] = accum_out
        return self.op("act", lambda e: e.activation(out, in_, func, **kw), reads, writes)

    def tt(self, eng, out, in0, in1, op, reads, writes):
        return self.op(eng, lambda e: e.tensor_tensor(out, in0, in1, op), reads, writes)

    def ts(self, eng, out, in0, s1, s2, op0, op1, reads, writes):
        if s2 is None:
            return self.op(eng, lambda e: e.tensor_scalar(out, in0, s1, None, op0), reads, writes)
        return self.op(eng, lambda e: e.tensor_scalar(out, in0, s1, s2, op0, op1), reads, writes)

    def stt(self, eng, out, in0, scalar, in1, op0, op1, reads, writes):
        return self.op(eng, lambda e: e.scalar_tensor_tensor(out, in0, scalar, in1, op0, op1), reads, writes)

    def copy(self, eng, out, in_, reads, writes):
        if eng == "act":
            return self.act(out, in_, AF.Copy, reads, writes)
        return self.op(eng, lambda e: e.tensor_copy(out, in_), reads, writes)

    def mm(self, out, lhsT, rhs, start, stop, reads, writes, sig=None):
        if sig is None:
            sig = stop
        return self.op("pe", lambda e: e.matmul(out, lhsT, rhs, start=start, stop=stop), reads, writes, sig=sig)

    def tr(self, out, in_, ident, reads, writes, sig=True):
        return self.op("pe", lambda e: e.transpose(out, in_, ident), reads, writes, sig=sig)

    def memset(self, eng, ap, val, writes):
        return self.op(eng, lambda e: e.memset(ap, val), (), writes)


O_U, O_HQ, O_HF, O_HI, O_HG = 0, 512, 1024, 1536, 2048
O_RQ, O_RK, O_RV, O_RG = 2560, 2816, 3072, 3584
O_MZ, O_XBC, O_DT = 4096, 4608, 5632

FM_KIND = (["copy"] * 4 + ["silu"] * 4 + ["f"] * 4 + ["silu"] * 4 +
           ["ropeA", "ropeA", "ropeB", "ropeB", "ropeA", "ropeA", "ropeB", "ropeB"] +
           ["silu"] * 4 + ["conv"] * 8)
NFM = len(FM_KIND)


def _fm_cols():
    cols = []
    cols += list(range(O_U, O_U + 512))
    cols += list(range(O_HQ, O_HQ + 512))
    cols += list(range(O_HF, O_HF + 512))
    cols += list(range(O_HG, O_HG + 512))

    def swapped(base):
        out = []
        for h in range(4):
            out += list(range(base + h * 64 + 32, base + h * 64 + 64))
            out += list(range(base + h * 64, base + h * 64 + 32))
        return out
    cols += list(range(O_RQ, O_RQ + 256)) + swapped(O_RQ)
    cols += list(range(O_RK, O_RK + 256)) + swapped(O_RK)
    cols += list(range(O_RG, O_RG + 512))
    cols += list(range(O_XBC, O_XBC + 1024))
    return np.array(cols, dtype=np.int64)


def _tm_cols():
    cols = list(range(O_HI, O_HI + 512)) + list(range(O_RV, O_RV + 512)) + list(range(O_MZ, O_MZ + 512))
    cols += list(range(O_DT, O_DT + 8))
    return np.array(cols, dtype=np.int64)


def _const_tables():
    c = {}
    c["ident"] = np.eye(128, dtype=np.float32)
    j = np.arange(128) % 64
    inv_freq = 10000.0 ** (-(j % 32).astype(np.float64) / 32.0)
    rope = np.zeros((128, 2), np.float32)
    rope[:, 0] = (inv_freq / TWO_PI).astype(np.float32)
    rope[:, 1] = np.where(j < 32, -1.0, 1.0)
    c["rope_c"] = rope
    t = np.arange(512)
    c["reset64"] = np.tile((t % 64 != 0).astype(np.float32)[None, :], (128, 1))
    sidx = np.arange(64)[:, None]
    tidx = (t % 64)[None, :]
    c["cmask64"] = (sidx <= tidx).astype(np.float32)
    c["hm64"] = np.tile(((t % 64) < 32).astype(np.float32)[None, :], (128, 1))
    gam = 1.0 - 2.0 ** (-5.0 - np.arange(4))
    r = (t % 64).astype(np.float64)
    c["ret_gq"] = np.stack([np.tile((g ** (r + 1))[None, :], (64, 1)) for g in gam]).astype(np.float32)
    dlt = (tidx - sidx).astype(np.float64)
    c["ret_mask"] = np.stack([np.where(dlt >= 0, g ** np.maximum(dlt, 0) * 0.125, 0.0) for g in gam]).astype(np.float32)
    c["iota1"] = np.tile((np.arange(512, dtype=np.float32) + 1.0)[None, :], (128, 1))
    cm = np.zeros((4, 128, 128), np.float32)
    for q in range(4):
        for g2 in range(2):
            g8 = 2 * q + g2
            cm[q, g2 * 64:(g2 + 1) * 64, g8 * 16:(g8 + 1) * 16] = 1.0
    c["s5_cmask"] = cm
    sel = np.zeros((32, 32, 128), np.float32)
    for e in range(32):
        sel[e, e, :] = 1.0
    c["moe_sel"] = sel
    r128 = (t % 128).astype(np.float64)
    c["ret_gq128"] = np.stack([np.tile((g ** (r128 + 1))[None, :], (64, 1)) for g in gam]).astype(np.float32)
    s1 = np.arange(128)[:, None]
    dl = ((t % 128)[None, :] - s1).astype(np.float64)
    c["ret_mask128"] = np.stack([np.where(dl >= 0, g ** np.maximum(dl, 0) * 0.125, 0.0) for g in gam]).astype(np.float32)
    c["ret_gk128"] = np.stack([g ** (127.0 - np.arange(128)) * 0.125 for g in gam], axis=1).astype(np.float32)
    c["negm64"] = ((c["cmask64"] - 1.0) * 30000.0).astype(np.float32)
    s128 = np.arange(128)[:, None]
    t128 = (np.arange(1024) % 128)[None, :]
    c["cmask128"] = (s128 <= t128).astype(np.float32)
    c["negm128"] = ((c["cmask128"] - 1.0) * 30000.0).astype(np.float32)
    c["ret_gk"] = np.stack([g ** (63.0 - np.arange(64)) * 0.125 for g in gam], axis=1).astype(np.float32)
    return c


class Prog:
    def __init__(self, debug=None, upto="all"):
        self.debug = debug or []
        self.upto = upto
        self.nc = bass.Bass("TRN2", target_bir_lowering=False)
        self.dram = {}
        self.dbuf = {}

    def din(self, name, shape, dtype=F32):
        self.dram[name] = self.nc.dram_tensor(name, list(shape), dtype, kind="ExternalInput").ap()
        self.dbuf[name] = Buf(name)
        return self.dram[name]

    def dscr(self, name, shape, dtype):
        kind = "ExternalOutput" if name in self.debug else "Internal"
        self.dram[name] = self.nc.dram_tensor(name, list(shape), dtype, kind=kind).ap()
        self.dbuf[name] = Buf(name)
        return self.dram[name]

    def dout(self, name, shape, dtype=F32):
        self.dram[name] = self.nc.dram_tensor(name, list(shape), dtype, kind="ExternalOutput").ap()
        self.dbuf[name] = Buf(name)
        return self.dram[name]

    def declare(self):
        L = DEPTH
        self.din("xT", [D, SEQ])
        self.din("c_pk", [128, 8])
        self.din("pos", [1, SEQ], I32)
        self.din("ada_w", [L, D, 6 * D])
        self.din("ada_b_pk", [L, 128, 48])
        self.din("w_fm", [L, D, NFM * 128])
        self.din("w_tm", [L, D, 1544])
        self.din("hg_lb_pk", [128, L, 4])
        self.din("conv_w_pk", [L, 128, 8, 4])
        self.din("conv_b_pk", [L, 128, 8])
        self.din("dt_bias", [L, 1, 8])
        self.din("ident", [128, 128])
        self.din("rope_c", [128, 2])
        self.din("reset64", [128, 512])
        self.din("cmask64", [64, 512])
        self.din("hm64", [128, 512])
        self.din("hg_norm_pk", [128, DEPTH])
        self.din("ret_gq", [4, 64, 512])
        self.din("ret_mask", [4, 64, 512])
        self.din("ret_gk", [64, 4])
        self.din("ret_gq128", [4, 64, 512])
        self.din("ret_mask128", [4, 128, 512])
        self.din("ret_gk128", [128, 4])
        self.din("negm64", [64, 512])
        self.din("cmask128", [128, 1024])
        self.din("negm128", [128, 1024])
        self.din("iota1", [128, 512])
        self.din("s5_cmask", [4, 128, 128])
        self.din("s5_lr_pk", [DEPTH, 128, 16])
        self.din("s5_li_pk", [DEPTH, 128, 16])
        self.din("s5_ldt_pk", [DEPTH, 128, 16])
        self.din("s5_bre_pk", [DEPTH, 128, 16, 16])
        self.din("s5_bim_pk", [DEPTH, 128, 16, 16])
        self.din("s5_cre_pk", [DEPTH, 4, 128, 64])
        self.din("s5_cim_pk", [DEPTH, 4, 128, 64])
        self.din("s5_d_pk", [DEPTH, 128, 4])
        self.din("s5_wglu", [DEPTH, 512, 512])
        self.din("w_gate", [DEPTH, D, 4 * D])
        self.din("b_gate_pk", [DEPTH, 128, 32])
        self.din("w_branch", [DEPTH, 4 * 512, D])
        self.din("w_out", [DEPTH, D, D])
        self.din("moe_wr", [DEPTH, D, 36])
        self.din("moe_br", [DEPTH, 1, 36])
        self.din("moe_w13_pk", [DEPTH, 32, 128, 8 * 512])
        self.din("moe_w2_pk", [DEPTH, 4, 128, 16 * D])
        self.dscr("MW13", [DEPTH, 32, 128, 8 * 512], BF16)
        self.dscr("MW2", [DEPTH, 4, 128, 16 * D], BF16)
        self.dscr("CMB", [2, 32, 1024], F32)
        self.din("moe_sel", [32, 32, 128])
        self.din("fnorm_pk", [128, 8])
        self.din("m2_alog", [DEPTH, 1, 8])
        self.din("m2_d", [DEPTH, 1, 8])
        self.din("m2_normw", [DEPTH, 1, 512])
        self.dout("outT", [D, SEQ])
        self.dscr("UT", [512, SEQ], BF16)
        self.dscr("HGQ", [512, SEQ], BF16)
        self.dscr("HGLF", [512, SEQ], F32)
        self.dscr("HGK", [512, SEQ], BF16)
        self.dscr("HGG", [512, SEQ], BF16)
        self.dscr("RQ", [256, SEQ], BF16)
        self.dscr("RK", [256, SEQ], BF16)
        self.dscr("RG", [512, SEQ], BF16)
        self.dscr("XBC", [1024, SEQ], BF16)
        self.dscr("HGI", [SEQ, 512], BF16)
        self.dscr("RV", [SEQ, 512], BF16)
        self.dscr("MZ", [SEQ, 512], BF16)
        self.dscr("DT", [SEQ, 8], F32)
        self.dscr("MOD", [128, 2 * 48], F32)
        self.dscr("YB", [4 * 512, SEQ], BF16)
        self.dscr("XS", [D, SEQ], F32)

    def build(self):
        nc = self.nc
        self.declare()
        with ExitStack() as es:
            kb = self.kb = KB(nc, es)
            self.es = es
            self.consts()
            self.prologue()
            for layer in range(DEPTH):
                def ph(name, fn):
                    with self.nc.named_scope("%s_L%d" % (name, layer)):
                        fn(layer)
                ph("A", self.phase_a)
                if self.upto == "S5":
                    ph("S5", self.phase_s5)
                    break
                if self.upto == "SSD":
                    ph("SSD", self.phase_ssd)
                    break
                if self.upto == "RET":
                    ph("RET", self.phase_ret)
                    break
                if self.upto == "HG":
                    ph("HG", self.phase_hg)
                    break
                ph("HG", self.phase_hg)
                ph("RET", self.phase_ret)
                ph("SSD", self.phase_ssd)
                ph("S5", self.phase_s5)
                ph("C", self.phase_c)
                ph("MOE", self.phase_moe)
            if self.upto == "all":
                with self.nc.named_scope("FINAL"):
                    self.phase_final()
            self.finish()
        return nc

    def finish(self):
        kb = self.kb
        kb.barrier()
        kb._wait("sp", [(kb.bg_sems[i], 16 * kb.bg_cnt[i]) for i in range(len(kb.bg_sems)) if kb.bg_cnt[i]])
        print("[kernel] instruction counts", kb.ninst, flush=True)

    def consts(self):
        kb = self.kb
        dr, db = self.dram, self.dbuf
        self.identf = kb.sb("identf", [128, 128], F32)
        self.identb = kb.sb("identb", [128, 128], BF16)
        kb.dma("sp", self.identf[:], dr["ident"], reads=[db["ident"]], writes=[self.identf])
        kb.copy("dve", self.identb[:], self.identf[:], [self.identf], [self.identb])
        self.ones_d = kb.sb("ones_d", [128, 128], BF16)
        kb.memset("dve", self.ones_d[:], 1.0 / 1024.0, [self.ones_d])
        self.ones_df = kb.sb("ones_df", [128, 128], F32)
        kb.memset("dve", self.ones_df[:], 1.0 / 1024.0, [self.ones_df])
        self.ones_h = kb.sb("ones_h", [128, 128], BF16)
        kb.memset("dve", self.ones_h[:], 1.0 / 128.0, [self.ones_h])
        self.ropec = kb.sb("ropec", [128, 2], F32)
        kb.dma("sp", self.ropec[:], dr["rope_c"], reads=[db["rope_c"]], writes=[self.ropec])
        self.reset64 = kb.sb("reset64", [128, 512], F32)
        kb.dma("sp", self.reset64[:], dr["reset64"], reads=[db["reset64"]], writes=[self.reset64])
        self.hm64 = kb.sb("hm64", [128, 512], F32)
        kb.dma("sp", self.hm64[:], dr["hm64"], reads=[db["hm64"]], writes=[self.hm64])
        self.cmask64 = kb.sb("cmask64", [64, 512], F32)
        kb.dma("sp", self.cmask64[:], dr["cmask64"], reads=[db["cmask64"]], writes=[self.cmask64])
        self.hgnorm = kb.sb("hgnorm", [128, DEPTH], F32)
        kb.dma("sp", self.hgnorm[:], dr["hg_norm_pk"], reads=[db["hg_norm_pk"]], writes=[self.hgnorm])
        self.mod = kb.sb("mod", [128, 2, 48], F32)
        self.ops = kb.sb("ops", [128, 2, 16], F32)
        self.psum = [kb.ps("ps%d" % i, [128, 512], F32) for i in range(8)]
        self.ps_i = 0

    def bank(self):
        p = self.psum[self.ps_i]
        self.ps_i = (self.ps_i + 1) % 8
        return p

    def prologue(self):
        kb = self.kb
        dr, db = self.dram, self.dbuf
        with ExitStack() as es:
            c_f = kb.sb("c_f", [128, 8], F32, es)
            cond = kb.sb("cond", [128, 8], BF16, es)
            adab = kb.sb("adab", [128, 2, 48], F32, es)
            wbuf = [kb.sb("adaw%d" % i, [128, 8, 512], BF16, es) for i in range(2)]
            kb.dma("sp", c_f[:], dr["c_pk"], reads=[db["c_pk"]], writes=[c_f])
            kb.dma("sp", adab[:], dr["ada_b_pk"].rearrange("l p j -> p l j"), reads=[db["ada_b_pk"]], writes=[adab])
            kb.act(cond[:], c_f[:], AF.Silu, [c_f], [cond])
            for l in range(DEPTH):
                ps = self.bank()
                for g in range(12):
                    w = wbuf[g % 2]
                    kb.dma("pool", w[:], dr["ada_w"][l, :, g * 512:(g + 1) * 512].rearrange("(k p) n -> p k n", p=128),
                           reads=[db["ada_w"]], writes=[w])
                    for bi in range(4):
                        col = g * 4 + bi
                        for k in range(8):
                            kb.mm(ps[:, col:col + 1], w[:, k, bi * 128:(bi + 1) * 128], cond[:, k:k + 1],
                                  start=(k == 0), stop=(k == 7), reads=[w, cond], writes=[ps],
                                  sig=(k == 7 and bi == 3))
                kb.tt("dve", self.mod[:, l, :], ps[:, 0:48], adab[:, l, :], ALU.add, [ps, adab], [self.mod])
                kb.ts("dve", self.ops[:, l, 0:8], self.mod[:, l, 8:16], 1.0, None, ALU.add, None, [self.mod], [self.ops])
                kb.ts("dve", self.ops[:, l, 8:16], self.mod[:, l, 32:40], 1.0, None, ALU.add, None, [self.mod], [self.ops])
            if "MOD" in self.debug:
                kb.dma("sp", dr["MOD"], self.mod[:].rearrange("p l j -> p (l j)"), reads=[self.mod], writes=[db["MOD"]])
            kb.barrier()

    def h_stats(self, x_t, sq, rstd, W=512, fp32_sq=False):
        kb = self.kb
        ps = self.bank()
        kb.act(sq[:], x_t[:], AF.Square, [x_t], [sq])
        ones = self.ones_df if fp32_sq else self.ones_d
        for k in range(8):
            kb.mm(ps[:, 0:W], ones[:], sq[:, k, :], start=(k == 0), stop=(k == 7),
                  reads=[ones, sq], writes=[ps])
        kb.act(rstd[:], ps[:, 0:W], AF.Ln, [ps], [rstd], bias=EPS)
        kb.act(rstd[:], rstd[:], AF.Exp, [rstd], [rstd], scale=-0.5)

    def h_apply(self, x_t, l, which, rstd, tmp):
        kb = self.kb
        so = 0 if which == 0 else 8
        for k in range(8):
            kb.stt("dve", tmp[:, k, :], x_t[:, k, :], self.ops[:, l, so + k:so + k + 1], rstd[:],
                   ALU.mult, ALU.mult, [x_t, self.ops, rstd], [tmp])

    def h_tile(self, x_t, h_out_ap_fn, l, which, sq, rstd, tmp, W=512, fp32_sq=False):
        self.h_stats(x_t, sq, rstd, W=W, fp32_sq=fp32_sq)
        self.h_apply(x_t, l, which, rstd, tmp)

    def phase_a(self, l):
        kb = self.kb
        dr, db = self.dram, self.dbuf
        xsrc = "xT" if l == 0 else "XS"
        with ExitStack() as es:
            hT = kb.sb("hT", [128, 8, SEQ], BF16, es)
            hTt = []
            for t in range(NT):
                v = T(hT.t[:, :, t * 512:(t + 1) * 512], "hT%d" % t)
                hTt.append(v)
            WA = 256
            xt = [kb.sb("xt%d" % i, [128, 8, WA], F32, es) for i in range(2)]
            sqs = [kb.sb("sq%d" % i, [128, 8, WA], BF16, es) for i in range(2)]
            o32 = kb.sb("o32", [128, SEQ + 4], F32, es)
            tmps = []
            for i in range(2):
                v = T(o32.t[:, 4 + i * 2048:4 + (i + 1) * 2048].rearrange("p (k n) -> p k n", n=WA), "tmpv%d" % i)
                tmps.append(v)
            rstds = [kb.sb("rstd%d" % i, [128, WA], F32, es) for i in range(2)]
            NA = SEQ // WA

            def a_stats(t):
                kb.dma("sp", xt[t % 2][:], dr[xsrc][:, t * WA:(t + 1) * WA].rearrange("(k p) n -> p k n", p=128),
                       reads=[db[xsrc]], writes=[xt[t % 2]])
                self.h_stats(xt[t % 2], sqs[t % 2], rstds[t % 2], W=WA)
            a_stats(0)
            for t in range(NA):
                if t + 1 < NA:
                    a_stats(t + 1)
                x_t = xt[t % 2]
                tmp = tmps[t % 2]
                self.h_apply(x_t, l, 0, rstds[t % 2], tmp)
                hv = hTt[t // 2]
                for k in range(8):
                    kb.act(hT[:, k, t * WA:(t + 1) * WA], tmp[:, k, :], AF.Identity, [tmp, self.mod], [hv],
                           bias=self.mod[:, l, k:k + 1])
            for v in tmps:
                o32.b.r.extend(v.b.r)
                if v.b.w is not None:
                    o32.b.r.append(v.b.w)
            cosT = kb.sb("cosT", [128, SEQ], F32, es)
            sinT = kb.sb("sinT", [128, SEQ], F32, es)
            lbc = kb.sb("lbc", [128, 4], F32, es)
            oml = kb.sb("oml", [128, 4], F32, es)
            convw = kb.sb("convw", [128, 8, 4], F32, es)
            convb = kb.sb("convb", [128, 8], F32, es)
            dtb = kb.sb("dtb", [128, 8], F32, es)
            CH = 512
            posi = kb.sb("posi", [128, CH], I32, es)
            ang = kb.sb("ang", [128, CH], F32, es)
            ki = kb.sb("ki", [128, CH], I32, es)
            kf = kb.sb("kf", [128, CH], F32, es)
            lbl = kb.sb("lbl", [128, 2, 4], F32, es)
            for c4 in range(SEQ // CH):
                csl = slice(c4 * CH, (c4 + 1) * CH)
                kb.dma("sp", posi[:], dr["pos"][:, csl].partition_broadcast(128), reads=[db["pos"]], writes=[posi])
                kb.copy("dve", kf[:], posi[:], [posi], [kf])
                kb.ts("dve", ang[:], kf[:], self.ropec[:, 0:1], None, ALU.mult, None, [kf, self.ropec], [ang])
                kb.copy("dve", ki[:], ang[:], [ang], [ki])
                kb.copy("dve", kf[:], ki[:], [ki], [kf])
                kb.tt("dve", kf[:], ang[:], kf[:], ALU.subtract, [ang, kf], [kf])
                kb.act(sinT[:, csl], kf[:], AF.Sin, [kf], [sinT], scale=SIN_SCALE)
                kb.ts("dve", sinT[:, csl], sinT[:, csl], self.ropec[:, 1:2], None, ALU.mult, None, [sinT, self.ropec], [sinT])
                kb.ts("dve", ang[:], ang[:], 0.25, None, ALU.add, None, [ang], [ang])
                kb.copy("dve", ki[:], ang[:], [ang], [ki])
                kb.copy("dve", kf[:], ki[:], [ki], [kf])
                kb.tt("dve", kf[:], ang[:], kf[:], ALU.subtract, [ang, kf], [kf])
                kb.act(cosT[:, csl], kf[:], AF.Sin, [kf], [cosT], scale=SIN_SCALE)
            kb.dma("sp", lbl[:], dr["hg_lb_pk"], reads=[db["hg_lb_pk"]], writes=[lbl])
            if l == 0:
                kb.memset("dve", lbc[:], 0.0, [lbc])
            else:
                kb.tt("dve", lbc[:], lbl[:, 1, :], lbl[:, 0, :], ALU.subtract, [lbl], [lbc])
                kb.act(lbc[:], lbc[:], AF.Sigmoid, [lbc], [lbc])
            kb.ts("dve", oml[:], lbc[:], -1.0, 1.0, ALU.mult, ALU.add, [lbc], [oml])
            kb.dma("sp", convw[:], dr["conv_w_pk"][l], reads=[db["conv_w_pk"]], writes=[convw])
            kb.dma("sp", convb[:], dr["conv_b_pk"][l], reads=[db["conv_b_pk"]], writes=[convb])
            kb.dma("sp", dtb[:], dr["dt_bias"][l].partition_broadcast(128), reads=[db["dt_bias"]], writes=[dtb])
            wb = [kb.sb("wfm%d" % i, [128, 8, 512], BF16, es) for i in range(2)]
            o16 = [kb.sb("o16_%d" % i, [128, SEQ], BF16, es) for i in range(2)]
            k16 = kb.sb("k16", [128, SEQ], BF16, es)
            t1 = [kb.sb("t1_%d" % i, [128, 512], F32, es) for i in range(2)]
            t2 = [kb.sb("t2_%d" % i, [128, 512], F32, es) for i in range(2)]
            kb.memset("dve", o32[:, 0:4], 0.0, [o32])
            dests = (["UT"] * 4 + ["HGQ"] * 4 + ["HGLF"] * 4 + ["HGG"] * 4 +
                     ["RQ", "RQ", None, None, "RK", "RK", None, None] + ["RG"] * 4 + ["XBC"] * 8)
            drow = ([0, 1, 2, 3] * 4 + [0, 1, 0, 0, 0, 1, 0, 0] + [0, 1, 2, 3] + list(range(8)))
            oi = 0
            ti = 0
            for g in range(NFM // 4):
                w = wb[g % 2]
                kb.dma("pool", w[:], dr["w_fm"][l, :, g * 512:(g + 1) * 512].rearrange("(k p) n -> p k n", p=128),
                       reads=[db["w_fm"]], writes=[w])

                def mm_block(bi, t):
                    ps = self.bank()
                    for k in range(8):
                        kb.mm(ps[:], w[:, k, bi * 128:(bi + 1) * 128], hT[:, k, t * 512:(t + 1) * 512],
                              start=(k == 0), stop=(k == 7), reads=[w, hTt[t]], writes=[ps])
                    return ps
                kinds = FM_KIND[g * 4:(g + 1) * 4]
                if kinds[0] == "ropeA":
                    for bi in range(2):
                        blk = g * 4 + bi
                        o = o16[oi % 2]
                        oi += 1
                        for t in range(NT):
                            sl = slice(t * 512, (t + 1) * 512)
                            pa = mm_block(bi, t)
                            pb = mm_block(bi + 2, t)
                            a1 = t1[ti % 2]
                            a2 = t2[ti % 2]
                            ti += 1
                            kb.tt("dve", a1[:], pa[:], cosT[:, sl], ALU.mult, [pa, cosT], [a1])
                            kb.tt("dve", a2[:], pb[:], sinT[:, sl], ALU.mult, [pb, sinT], [a2])
                            kb.tt("pool", o[:, sl], a1[:], a2[:], ALU.add, [a1, a2], [o])
                        dn = dests[blk]
                        kb.dma("sp", dr[dn][drow[blk] * 128:(drow[blk] + 1) * 128, :], o[:], reads=[o], writes=[db[dn]])
                    continue
                for bi in range(4):
                    blk = g * 4 + bi
                    kind = FM_KIND[blk]
                    dn = dests[blk]
                    rows = slice(drow[blk] * 128, (drow[blk] + 1) * 128)
                    o = o16[oi % 2]
                    oi += 1
                    for t in range(NT):
                        sl = slice(t * 512, (t + 1) * 512)
                        ps = mm_block(bi, t)
                        if kind == "copy":
                            kb.act(o[:, sl], ps[:], AF.Copy, [ps], [o])
                        elif kind == "silu":
                            kb.act(o[:, sl], ps[:], AF.Silu, [ps], [o])
                        elif kind == "f":
                            a1 = t1[ti % 2]
                            ti += 1
                            kb.act(a1[:], ps[:], AF.Sigmoid, [ps], [a1])
                            kb.ts("dve", a1[:], a1[:], oml[:, bi:bi + 1], lbc[:, bi:bi + 1], ALU.mult, ALU.add,
                                  [a1, oml, lbc], [a1])
                            kb.act(o32[:, 4 + t * 512:4 + (t + 1) * 512], a1[:], AF.Ln, [a1], [o32])
                            kb.ts("dve", k16[:, sl], a1[:], -1.0, 1.0, ALU.mult, ALU.add, [a1], [k16])
                        elif kind == "conv":
                            kb.act(o32[:, 4 + t * 512:4 + (t + 1) * 512], ps[:], AF.Copy, [ps], [o32])
                    if kind == "f":
                        kb.dma("sp", dr["HGLF"][rows, :], o32[:, 4:4 + SEQ], reads=[o32], writes=[db["HGLF"]])
                        kb.dma("sp", dr["HGK"][rows, :], k16[:], reads=[k16], writes=[db["HGK"]])
                    elif kind == "conv":
                        cb = blk - 28
                        acc = hTacc = None
                        for t in range(NT):
                            sl = slice(t * 512, (t + 1) * 512)
                            a1 = t1[ti % 2]
                            ti += 1
                            base = 1 + t * 512
                            kb.ts("dve", a1[:], o32[:, base:base + 512], convw[:, cb, 0:1], None, ALU.mult, None,
                                  [o32, convw], [a1])
                            for i in range(1, 4):
                                kb.stt("dve", a1[:], o32[:, base + i:base + i + 512], convw[:, cb, i:i + 1], a1[:],
                                       ALU.mult, ALU.add, [o32, convw, a1], [a1])
                            kb.act(o[:, sl], a1[:], AF.Silu, [a1, convb], [o], bias=convb[:, cb:cb + 1])
                        kb.dma("sp", dr[dn][rows, :], o[:], reads=[o], writes=[db[dn]])
                    else:
                        kb.dma("sp", dr[dn][rows, :], o[:], reads=[o], writes=[db[dn]])
            tm_dest = ["HGI", "RV", "MZ"]
            st = []
            for i in range(2):
                v = T(o16[i].t[:, 0:2048].rearrange("p (j n) -> p j n", n=512), "stv%d" % i)
                v.b = o16[i].b
                st.append(v)
            si = 0
            for g in range(3):
                w = wb[(NFM // 4 + g) % 2]
                kb.dma("pool", w[:], dr["w_tm"][l, :, g * 512:(g + 1) * 512].rearrange("(k p) n -> p k n", p=128),
                       reads=[db["w_tm"]], writes=[w])
                for q4 in range(8):
                    s = st[si % 2]
                    si += 1
                    for j in range(4):
                        tt_ = q4 * 4 + j
                        ps = self.bank()
                        for k in range(8):
                            kb.mm(ps[:], hT[:, k, tt_ * 128:(tt_ + 1) * 128], w[:, k, :], start=(k == 0), stop=(k == 7),
                                  reads=[hTt[tt_ // 4], w], writes=[ps])
                        if g == 2:
                            kb.act(s[:, j, :], ps[:], AF.Silu, [ps], [s])
                        else:
                            kb.copy("dve", s[:, j, :], ps[:], [ps], [s])
                    kb.dma("sp", dr[tm_dest[g]][q4 * 512:(q4 + 1) * 512, :].rearrange("(j p) n -> p j n", p=128), s[:],
                           reads=[s], writes=[db[tm_dest[g]]])
            wdt = kb.sb("wdt", [128, 8, 8], BF16, es)
            dts = kb.sb("dts", [128, 32, 8], F32, es)
            kb.dma("pool", wdt[:], dr["w_tm"][l, :, 1536:1544].rearrange("(k p) n -> p k n", p=128),
                   reads=[db["w_tm"]], writes=[wdt])
            ps = self.bank()
            for tt_ in range(32):
                for k in range(8):
                    kb.mm(ps[:, tt_ * 8:(tt_ + 1) * 8], hT[:, k, tt_ * 128:(tt_ + 1) * 128], wdt[:, k, :],
                          start=(k == 0), stop=(k == 7), reads=[hTt[tt_ // 4], wdt], writes=[ps], sig=(k == 7 and tt_ == 31))
            kb.tt("dve", dts[:], ps[:, 0:256].rearrange("p (t h) -> p t h", h=8),
                  dtb[:].unsqueeze(1).to_broadcast([128, 32, 8]), ALU.add, [ps, dtb], [dts])
            kb.act(dts[:], dts[:], AF.Exp, [dts], [dts])
            kb.act(dts[:], dts[:], AF.Ln, [dts], [dts], bias=1.0)
            kb.dma("sp", dr["DT"].rearrange("(t p) h -> p t h", p=128), dts[:], reads=[dts], writes=[db["DT"]])
            for e8 in range(4):
                kb.dma("pool", dr["MW13"][l, e8 * 8:(e8 + 1) * 8].rearrange("e p f -> p e f"),
                       dr["moe_w13_pk"][l, e8 * 8:(e8 + 1) * 8].rearrange("e p f -> p e f"), reads=[db["moe_w13_pk"]], writes=[db["MW13"]], bg=True)
            kb.dma("pool", dr["MW2"][l].rearrange("g p f -> p g f"), dr["moe_w2_pk"][l].rearrange("g p f -> p g f"),
                   reads=[db["moe_w2_pk"]], writes=[db["MW2"]], bg=True)
            kb.barrier()


    def head_rstd(self, src, osq, rs, ones, psn):
        kb = self.kb
        kb.act(osq[:], src[:], AF.Square, [src], [osq])
        kb.mm(psn[:], ones[:], osq[:], start=True, stop=True, reads=[ones, osq], writes=[psn])
        kb.act(rs[:], psn[:], AF.Ln, [psn], [rs], bias=EPS)
        kb.act(rs[:], rs[:], AF.Exp, [rs], [rs], scale=-0.5)

    def phase_hg(self, l):
        kb = self.kb
        dr, db = self.dram, self.dbuf
        NH = 4
        with ExitStack() as es:
            P = self.psum
            psO = [P[h] for h in range(NH)]
            psKV = []
            for h in range(NH):
                bk = P[4 + h // 2]
                v = T(bk.t[:, (h % 2) * 128:(h % 2 + 1) * 128], "kv%d" % h)
                v.b = bk.b
                psKV.append(v)
            rot = [P[6], P[7]]
            ri = [0]

            def rbank():
                b = rot[ri[0] % 2]
                ri[0] += 1
                return b
            S = [kb.sb("S%d" % h, [128, 128], F32, es) for h in range(NH)]
            for h in range(NH):
                kb.memset("dve", S[h][:], 0.0, [S[h]])

            def mk(name, shape, dt, n=NH):
                return [kb.sb("%s%d" % (name, i), shape, dt, es) for i in range(n)]
            qt = mk("qt", [128, 512], BF16, 2 * NH)
            kt = mk("kt", [128, 512], BF16, 2 * NH)
            gt = mk("gt", [128, 512], BF16, 2 * NH)
            lf = mk("lf", [128, 512], F32, 2 * NH)
            vt = mk("vt", [64, 8, 128], BF16, 2 * NH)
            bb = mk("bb", [128, 512], F32)
            bm = mk("bm", [128, 512], F32)
            eq = mk("eq", [128, 512], F32)
            ek = mk("ek", [128, 512], F32)
            qs = mk("qs", [128, 512], BF16)
            ks = mk("ks", [128, 512], BF16)
            ksz = mk("ksz", [128, 512], BF16)
            kT = mk("kT", [64, 1024], BF16)
            sc = mk("sc", [64, 512], BF16)
            for h in range(NH):
                kb.memset("dve", sc[h][:], 0.0, [sc[h]])
            em = mk("em", [128, 8], F32)
            cd = mk("cd", [128, 8], F32)
            Sb = mk("Sb", [128, 128], BF16, 2 * NH)
            tS = mk("tS", [128, 128], F32)
            osq = mk("osq", [128, 512], BF16)
            rs = mk("rs", [128, 512], F32)
            y1 = mk("y1", [128, 512], F32)
            yo = mk("yo", [128, 512], BF16, 2 * NH)
            import os
            stop = os.environ.get("HG_STOP", "")
            for st in range(NT if not stop else 1):
                tsl = slice(st * 512, (st + 1) * 512)
                par = (st % 2) * NH
                for h in range(NH):
                    rows = slice(h * 128, (h + 1) * 128)
                    i = par + h
                    kb.dma("sp", qt[i][:], dr["HGQ"][rows, tsl], reads=[db["HGQ"]], writes=[qt[i]])
                    kb.dma("sp", kt[i][:], dr["HGK"][rows, tsl], reads=[db["HGK"]], writes=[kt[i]])
                    kb.dma("sp", lf[i][:], dr["HGLF"][rows, tsl], reads=[db["HGLF"]], writes=[lf[i]])
                    kb.dma("sp", gt[i][:], dr["HGG"][rows, tsl], reads=[db["HGG"]], writes=[gt[i]])
                    kb.dma("sp", vt[i][:], dr["HGI"][tsl, rows].rearrange("(c p) d -> p c d", p=64),
                           reads=[db["HGI"]], writes=[vt[i]])
                for h in range(NH):
                    i = par + h
                    kb.op("dve", lambda e: e.tensor_tensor_scan(bb[h][:], self.reset64[:], lf[i][:], 0.0, ALU.mult, ALU.add),
                          [self.reset64, lf[i]], [bb[h]])
                    b3 = bb[h][:].rearrange("p (c t) -> p c t", t=64)
                    bm3 = bm[h][:].rearrange("p (c t) -> p c t", t=64)
                    kb.tt("dve", bm3, b3, b3[:, :, 31:32].to_broadcast([128, 8, 64]), ALU.subtract, [bb[h]], [bm[h]])
                    kb.act(eq[h][:], bm[h][:], AF.Exp, [bm[h]], [eq[h]])
                    kb.act(ek[h][:], bm[h][:], AF.Exp, [bm[h]], [ek[h]], scale=-1.0)
                    kb.act(em[h][:], b3[:, :, 31], AF.Exp, [bb[h]], [em[h]])
                    kb.act(cd[h][:], bm3[:, :, 63], AF.Exp, [bm[h]], [cd[h]])
                    kb.tt("dve", qs[h][:], qt[i][:], eq[h][:], ALU.mult, [qt[i], eq[h]], [qs[h]])
                    kb.tt("dve", ks[h][:], kt[i][:], ek[h][:], ALU.mult, [kt[i], ek[h]], [ks[h]])
                    pT = rbank()
                    pTb = pT.t[:].bitcast(BF16)
                    for c in range(8):
                        kb.tr(pTb[0:64, c * 128:(c + 1) * 128], ks[h][:, c * 64:(c + 1) * 64], self.identb[:],
                              [ks[h], self.identb], [pT], sig=(c == 7))
                    kb.copy("act", kT[h][:], pTb[0:64, :], [pT], [kT[h]])
                    kb.tt("dve", ksz[h][:], ks[h][:], self.hm64[:], ALU.mult, [ks[h], self.hm64], [ksz[h]])
                    pS = rbank()
                    for c in range(8):
                        cs = slice(c * 64, (c + 1) * 64)
                        c1 = slice(c * 64, c * 64 + 32)
                        c2 = slice(c * 64 + 32, (c + 1) * 64)
                        kb.mm(pS[0:64, c1], ksz[h][:, cs], qs[h][:, c1], start=True, stop=True,
                              reads=[ksz[h], qs[h]], writes=[pS], sig=False)
                        kb.mm(pS[0:64, c2], ks[h][:, cs], qs[h][:, c2], start=True, stop=True,
                              reads=[ks[h], qs[h]], writes=[pS], sig=(c == 7))
                    kb.op("dve", lambda e: e.copy_predicated(sc[h][:], self.cmask64[:].bitcast(mybir.dt.uint32), pS[0:64, :]),
                          [pS, self.cmask64], [sc[h]])
                if stop == "prep":
                    break
                for c in range(8):
                    cs = slice(c * 64, (c + 1) * 64)
                    for h in range(NH):
                        sb_ = Sb[(c % 2) * NH + h]
                        kb.act(sb_[:], S[h][:], AF.Identity, [S[h], em[h]], [sb_], scale=em[h][:, c:c + 1])
                    for h in range(NH):
                        i = par + h
                        sb_ = Sb[(c % 2) * NH + h]
                        kb.mm(psO[h][:, cs], vt[i][:, c, :], sc[h][:, cs], start=True, stop=False,
                              reads=[vt[i], sc[h]], writes=[psO[h]], sig=False)
                        kb.mm(psO[h][:, cs], sb_[:], qs[h][:, cs], start=False, stop=True,
                              reads=[sb_, qs[h]], writes=[psO[h]], sig=True)
                        kb.mm(psKV[h][:], kT[h][:, c * 128:(c + 1) * 128], vt[i][:, c, :], start=True, stop=True,
                              reads=[kT[h], vt[i]], writes=[psKV[h]])
                    for h in range(NH):
                        kb.stt("dve", tS[h][:], S[h][:], em[h][:, c:c + 1], psKV[h][:], ALU.mult, ALU.add,
                               [S[h], em[h], psKV[h]], [tS[h]])
                        kb.ts("dve", S[h][:], tS[h][:], cd[h][:, c:c + 1], None, ALU.mult, None, [tS[h], cd[h]], [S[h]])
                if stop == "chunk":
                    break
                for h in range(NH):
                    i = par + h
                    psn = rbank()
                    self.head_rstd(psO[h], osq[h], rs[h], self.ones_h, psn)
                    kb.stt("dve", y1[h][:], psO[h][:], self.hgnorm[:, l:l + 1], rs[h][:], ALU.mult, ALU.mult,
                           [psO[h], self.hgnorm, rs[h]], [y1[h]])
                    kb.tt("dve", yo[i][:], y1[h][:], gt[i][:], ALU.mult, [y1[h], gt[i]], [yo[i]])
                    kb.dma("sp", dr["YB"][512 + h * 128:512 + (h + 1) * 128, tsl], yo[i][:], reads=[yo[i]], writes=[db["YB"]])
            kb.barrier()

    def phase_ret(self, l):
        kb = self.kb
        dr, db = self.dram, self.dbuf
        NH = 4
        GAM = [1.0 - 2.0 ** (-5.0 - h) for h in range(4)]
        with ExitStack() as es:
            P = self.psum
            psO = [P[h] for h in range(NH)]
            psKV = []
            for h in range(NH):
                bk = P[4 + h // 2]
                v = T(bk.t[0:64, (h % 2) * 128:(h % 2 + 1) * 128], "rkv%d" % h)
                v.b = bk.b
                psKV.append(v)
            rot = [P[6], P[7]]
            ri = [0]

            def rbank():
                b = rot[ri[0] % 2]
                ri[0] += 1
                return b

            def mk(name, shape, dt, n=NH):
                return [kb.sb("%s%d" % (name, i), shape, dt, es) for i in range(n)]
            gq = mk("gq", [64, 512], F32)
            mret = mk("mret", [128, 512], F32)
            gk = kb.sb("gk", [128, 4], F32, es)
            kb.dma("sp", gk[:], dr["ret_gk128"], reads=[db["ret_gk128"]], writes=[gk])
            for h in range(NH):
                kb.dma("sp", gq[h][:], dr["ret_gq128"][h], reads=[db["ret_gq128"]], writes=[gq[h]])
                kb.dma("sp", mret[h][:], dr["ret_mask128"][h], reads=[db["ret_mask128"]], writes=[mret[h]])
            S = mk("rS", [64, 128], F32)
            for h in range(NH):
                kb.memset("dve", S[h][:], 0.0, [S[h]])
            qt = mk("rqt", [64, 512], BF16, 2 * NH)
            kt = mk("rkt", [64, 512], BF16, 2 * NH)
            gt = mk("rgt", [128, 512], BF16, 2 * NH)
            vt = mk("rvt", [128, 4, 128], BF16, 2 * NH)
            qd = mk("rqd", [64, 512], BF16)
            kT = mk("rkT", [128, 256], BF16)
            sc = mk("rsc", [128, 512], BF16)
            Sb = mk("rSb", [64, 128], BF16, 2 * NH)
            osq = mk("rosq", [128, 512], BF16)
            rs = mk("rrs", [128, 512], F32)
            y1 = mk("ry1", [128, 512], F32)
            yo = mk("ryo", [128, 512], BF16, 2 * NH)
            for st in range(NT):
                tsl = slice(st * 512, (st + 1) * 512)
                par = (st % 2) * NH
                for h in range(NH):
                    i = par + h
                    kb.dma("sp", qt[i][:], dr["RQ"][h * 64:(h + 1) * 64, tsl], reads=[db["RQ"]], writes=[qt[i]])
                    kb.dma("sp", kt[i][:], dr["RK"][h * 64:(h + 1) * 64, tsl], reads=[db["RK"]], writes=[kt[i]])
                    kb.dma("sp", gt[i][:], dr["RG"][h * 128:(h + 1) * 128, tsl], reads=[db["RG"]], writes=[gt[i]])
                    kb.dma("sp", vt[i][:], dr["RV"][tsl, h * 128:(h + 1) * 128].rearrange("(c p) d -> p c d", p=128),
                           reads=[db["RV"]], writes=[vt[i]])
                for h in range(NH):
                    i = par + h
                    kb.tt("dve", qd[h][:], qt[i][:], gq[h][:], ALU.mult, [qt[i], gq[h]], [qd[h]])
                    pT = rbank()
                    pTb = pT.t[:].bitcast(BF16)
                    for c in range(4):
                        kb.tr(pTb[:, c * 64:(c + 1) * 64], kt[i][:, c * 128:(c + 1) * 128], self.identb[0:64, 0:64],
                              [kt[i], self.identb], [pT], sig=(c == 3))
                    kb.act(kT[h][:], pTb[:, 0:256], AF.Identity, [pT, gk], [kT[h]], scale=gk[:, h:h + 1])
                    pS = rbank()
                    for c in range(4):
                        cs = slice(c * 128, (c + 1) * 128)
                        kb.mm(pS[:, cs], kt[i][:, cs], qt[i][:, cs], start=True, stop=True,
                              reads=[kt[i], qt[i]], writes=[pS], sig=(c == 3))
                    kb.tt("dve", sc[h][:], pS[:, :], mret[h][:], ALU.mult, [pS, mret[h]], [sc[h]])
                for c in range(4):
                    cs = slice(c * 128, (c + 1) * 128)
                    for h in range(NH):
                        sb_ = Sb[(c % 2) * NH + h]
                        kb.copy("act", sb_[:], S[h][:], [S[h]], [sb_])
                    for h in range(NH):
                        i = par + h
                        sb_ = Sb[(c % 2) * NH + h]
                        kb.mm(psO[h][:, cs], vt[i][:, c, :], sc[h][:, cs], start=True, stop=False,
                              reads=[vt[i], sc[h]], writes=[psO[h]], sig=False)
                        kb.mm(psO[h][:, cs], sb_[:], qd[h][:, cs], start=False, stop=True,
                              reads=[sb_, qd[h]], writes=[psO[h]], sig=True)
                        kb.mm(psKV[h][:], kT[h][:, c * 64:(c + 1) * 64], vt[i][:, c, :], start=True, stop=True,
                              reads=[kT[h], vt[i]], writes=[psKV[h]])
                    for h in range(NH):
                        kb.stt("dve", S[h][:], S[h][:], float(GAM[h] ** 128), psKV[h][:], ALU.mult, ALU.add,
                               [S[h], psKV[h]], [S[h]])
                for h in range(NH):
                    i = par + h
                    psn = rbank()
                    self.head_rstd(psO[h], osq[h], rs[h], self.ones_h, psn)
                    kb.tt("dve", y1[h][:], psO[h][:], rs[h][:], ALU.mult, [psO[h], rs[h]], [y1[h]])
                    kb.tt("dve", yo[i][:], y1[h][:], gt[i][:], ALU.mult, [y1[h], gt[i]], [yo[i]])
                    kb.dma("sp", dr["YB"][1024 + h * 128:1024 + (h + 1) * 128, tsl], yo[i][:], reads=[yo[i]], writes=[db["YB"]])
            kb.barrier()

    def phase_ssd(self, l):
        kb = self.kb
        dr, db = self.dram, self.dbuf
        with ExitStack() as es:
            P = self.psum
            pD = [P[0], P[1]]
            BG, Bt, pY1, pY2, pKV = P[2], P[3], P[4], P[5], P[6]

            def sbt(name, shape, dt):
                return kb.sb(name, shape, dt, es)
            cmk = sbt("cmk128", [128, 1024], F32)
            negm = sbt("negm128", [128, 1024], F32)
            kb.dma("sp", cmk[:], dr["cmask128"], reads=[db["cmask128"]], writes=[cmk])
            kb.dma("sp", negm[:], dr["negm128"], reads=[db["negm128"]], writes=[negm])
            ones = sbt("ones128", [128, 1024], F32)
            kb.memset("dve", ones[:], 1.0, [ones])
            arow = sbt("arow", [128, 8], F32)
            drow = sbt("drow", [128, 8], F32)
            nrow = sbt("nrow", [128, 512], F32)
            kb.dma("sp", arow[:], dr["m2_alog"][l].partition_broadcast(128), reads=[db["m2_alog"]], writes=[arow])
            kb.dma("sp", drow[:], dr["m2_d"][l].partition_broadcast(128), reads=[db["m2_d"]], writes=[drow])
            kb.dma("sp", nrow[:], dr["m2_normw"][l].partition_broadcast(128), reads=[db["m2_normw"]], writes=[nrow])
            kb.act(arow[:], arow[:], AF.Exp, [arow], [arow])
            S = sbt("mS", [128, 512], F32)
            Sb = sbt("mSb", [128, 512], BF16)
            kb.memset("dve", S[:], 0.0, [S])
            kb.memset("dve", Sb[:], 0.0, [Sb])
            xsT = [sbt("xsT%d" % i, [128, 4, 512], BF16) for i in range(2)]
            BT = [sbt("BT%d" % i, [128, 2, 512], BF16) for i in range(2)]
            CT = [sbt("CT%d" % i, [128, 2, 512], BF16) for i in range(2)]
            zt = [sbt("zt%d" % i, [128, 4, 512], BF16) for i in range(2)]
            dtt = [sbt("dtt%d" % i, [128, 4, 8], F32) for i in range(2)]
            yo = [sbt("myo%d" % i, [128, 4, 512], BF16) for i in range(2)]

            def two(name, shape, dt):
                return [sbt("%s%d" % (name, i), shape, dt) for i in range(2)]
            dA, dAn = two("dA", [128, 8], F32), two("dAn", [128, 8], F32)
            X, Y = two("X", [128, 1024], F32), two("Y", [128, 1024], F32)
            Lm = two("Lm", [128, 1024], F32)
            scL = two("scL", [128, 1024], BF16)
            xB = two("xB", [128, 768], BF16)
            vv, vh = two("vv", [128, 512], BF16), two("vh", [128, 512], BF16)
            bsb, eb, dec, eBT = two("bsb", [128, 8], F32), two("eb", [128, 8], F32), two("dec", [128, 8], F32), two("eBT", [128, 8], F32)
            ta, tb, tc_, yz, ysq = (two("ta", [128, 512], F32), two("tb", [128, 512], F32), two("tc", [128, 512], F32),
                                    two("yz", [128, 512], F32), two("ysq", [128, 512], F32))
            ssq, rst = two("ssq", [128, 1], F32), two("rst", [128, 1], F32)
            yn = two("yn", [128, 512], BF16)
            tS = sbt("tSm", [128, 512], F32)
            cm3 = cmk[:].rearrange("p (h t) -> p h t", t=128)
            on3 = ones[:].rearrange("p (h t) -> p h t", t=128)
            ci = 0
            for st in range(NT):
                tsl = slice(st * 512, (st + 1) * 512)
                i = st % 2
                kb.dma("sp", xsT[i][:], dr["XBC"][0:512, tsl].rearrange("(k p) n -> p k n", p=128), reads=[db["XBC"]], writes=[xsT[i]])
                kb.dma("sp", BT[i][:], dr["XBC"][512:768, tsl].rearrange("(k p) n -> p k n", p=128), reads=[db["XBC"]], writes=[BT[i]])
                kb.dma("sp", CT[i][:], dr["XBC"][768:1024, tsl].rearrange("(k p) n -> p k n", p=128), reads=[db["XBC"]], writes=[CT[i]])
                kb.dma("sp", zt[i][:], dr["MZ"][tsl, :].rearrange("(c p) d -> p c d", p=128), reads=[db["MZ"]], writes=[zt[i]])
                kb.dma("sp", dtt[i][:], dr["DT"][tsl, :].rearrange("(c p) d -> p c d", p=128), reads=[db["DT"]], writes=[dtt[i]])
                for c in range(4):
                    cs = slice(c * 128, (c + 1) * 128)
                    j = ci % 2
                    ci += 1
                    BGb = BG.t[:].bitcast(BF16)
                    Btb = Bt.t[:].bitcast(BF16)
                    kb.tt("dve", dAn[j][:], dtt[i][:, c, :], arow[:], ALU.mult, [dtt[i], arow], [dAn[j]])
                    kb.ts("dve", dA[j][:], dAn[j][:], -1.0, None, ALU.mult, None, [dAn[j]], [dA[j]])
                    kb.tt("dve", X[j][:].rearrange("p (h t) -> p h t", t=128), cm3,
                          dA[j][:].unsqueeze(2).to_broadcast([128, 8, 128]), ALU.mult, [cmk, dA[j]], [X[j]])
                    kb.tt("dve", Y[j][:].rearrange("p (h t) -> p h t", t=128), on3,
                          dAn[j][:].unsqueeze(2).to_broadcast([128, 8, 128]), ALU.mult, [ones, dAn[j]], [Y[j]])
                    for hb in range(2):
                        hsl = slice(hb * 512, (hb + 1) * 512)
                        kb.mm(pD[hb][:], self.identf[:], negm[:, hsl], start=True, stop=False, reads=[self.identf, negm], writes=[pD[hb]], sig=False)
                        kb.mm(pD[hb][:], cmk[:, 0:128], Y[j][:, hsl], start=False, stop=False, reads=[cmk, Y[j]], writes=[pD[hb]], sig=False)
                        kb.mm(pD[hb][:], ones[:, 0:128], X[j][:, hsl], start=False, stop=True, reads=[ones, X[j]], writes=[pD[hb]], sig=True)
                        kb.act(Lm[j][:, hsl], pD[hb][:], AF.Exp, [pD[hb]], [Lm[j]])
                    for g in range(2):
                        kb.mm(BG[:, g * 128:(g + 1) * 128], BT[i][:, g, cs], CT[i][:, g, cs], start=True, stop=True,
                              reads=[BT[i], CT[i]], writes=[BG], sig=(g == 1))
                    kb.mm(Bt[:, 384:392], cmk[:, 0:128], dA[j][:], start=True, stop=True, reads=[cmk, dA[j]], writes=[Bt], sig=False)
                    kb.mm(Bt[:, 392:400], ones[:, 0:128], dA[j][:], start=True, stop=True, reads=[ones, dA[j]], writes=[Bt], sig=False)
                    for k in range(4):
                        kb.tr(Btb[:, k * 128:(k + 1) * 128], xsT[i][:, k, cs], self.identb[:], [xsT[i], self.identb], [Bt], sig=False)
                    for g in range(2):
                        kb.tr(Btb[:, 512 + g * 128:512 + (g + 1) * 128], BT[i][:, g, cs], self.identb[:],
                              [BT[i], self.identb], [Bt], sig=(g == 1))
                    for g in range(2):
                        gs = slice(g * 512, (g + 1) * 512)
                        kb.tt("dve", scL[j][:, gs].rearrange("p (h t) -> p h t", t=128), Lm[j][:, gs].rearrange("p (h t) -> p h t", t=128),
                              BG[:, g * 128:(g + 1) * 128].unsqueeze(1).to_broadcast([128, 4, 128]), ALU.mult, [Lm[j], BG], [scL[j]])
                    kb.copy("act", xB[j][:], Btb[:, 0:768], [Bt], [xB[j]])
                    kb.copy("act", bsb[j][:], Bt[:, 384:392], [Bt], [bsb[j]])
                    kb.act(eb[j][:], Bt[:, 384:392], AF.Exp, [Bt], [eb[j]])
                    kb.act(eBT[j][:], Bt[:, 392:400], AF.Exp, [Bt], [eBT[j]])
                    kb.tt("dve", dec[j][:], Bt[:, 392:400], bsb[j][:], ALU.subtract, [Bt, bsb[j]], [dec[j]])
                    kb.act(dec[j][:], dec[j][:], AF.Exp, [dec[j]], [dec[j]])
                    kb.tt("dve", vv[j][:].rearrange("p (h t) -> p h t", t=64), xB[j][:, 0:512].rearrange("p (h t) -> p h t", t=64),
                          dtt[i][:, c, :].unsqueeze(2).to_broadcast([128, 8, 64]), ALU.mult, [xB[j], dtt[i]], [vv[j]])
                    for h in range(8):
                        kb.mm(pY1[:, h * 64:(h + 1) * 64], scL[j][:, h * 128:(h + 1) * 128], vv[j][:, h * 64:(h + 1) * 64],
                              start=True, stop=True, reads=[scL[j], vv[j]], writes=[pY1], sig=(h == 7))
                    for g in range(2):
                        gs = slice(g * 256, (g + 1) * 256)
                        kb.mm(pY2[:, gs], CT[i][:, g, cs], Sb[:, gs], start=True, stop=True, reads=[CT[i], Sb], writes=[pY2], sig=(g == 1))
                    kb.tt("dve", tb[j][:].rearrange("p (h t) -> p h t", t=64), xB[j][:, 0:512].rearrange("p (h t) -> p h t", t=64),
                          drow[:].unsqueeze(2).to_broadcast([128, 8, 64]), ALU.mult, [xB[j], drow], [tb[j]])
                    kb.tt("dve", vh[j][:].rearrange("p (h t) -> p h t", t=64), vv[j][:].rearrange("p (h t) -> p h t", t=64),
                          dec[j][:].unsqueeze(2).to_broadcast([128, 8, 64]), ALU.mult, [vv[j], dec[j]], [vh[j]])
                    for g in range(2):
                        gs = slice(g * 256, (g + 1) * 256)
                        kb.mm(pKV[:, gs], xB[j][:, 512 + g * 128:512 + (g + 1) * 128], vh[j][:, gs], start=True, stop=True,
                              reads=[xB[j], vh[j]], writes=[pKV], sig=(g == 1))
                    kb.tt("dve", ta[j][:].rearrange("p (h t) -> p h t", t=64), pY2[:].rearrange("p (h t) -> p h t", t=64),
                          eb[j][:].unsqueeze(2).to_broadcast([128, 8, 64]), ALU.mult, [pY2, eb[j]], [ta[j]])
                    kb.tt("dve", tS[:].rearrange("p (h t) -> p h t", t=64), S[:].rearrange("p (h t) -> p h t", t=64),
                          eBT[j][:].unsqueeze(2).to_broadcast([128, 8, 64]), ALU.mult, [S, eBT[j]], [tS])
                    kb.tt("dve", S[:], tS[:], pKV[:], ALU.add, [tS, pKV], [S])
                    kb.copy("act", Sb[:], S[:], [S], [Sb])
                    kb.tt("dve", tc_[j][:], pY1[:], ta[j][:], ALU.add, [pY1, ta[j]], [tc_[j]])
                    kb.tt("dve", tc_[j][:], tc_[j][:], tb[j][:], ALU.add, [tc_[j], tb[j]], [tc_[j]])
                    kb.tt("dve", yz[j][:], tc_[j][:], zt[i][:, c, :], ALU.mult, [tc_[j], zt[i]], [yz[j]])
                    kb.memset("dve", ssq[j][:], 0.0, [ssq[j]])
                    kb.act(ysq[j][:], yz[j][:], AF.Square, [yz[j]], [ysq[j], ssq[j]], accum_out=ssq[j][:])
                    kb.act(rst[j][:], ssq[j][:], AF.Ln, [ssq[j]], [rst[j]], bias=EPS, scale=1.0 / 512.0)
                    kb.act(rst[j][:], rst[j][:], AF.Exp, [rst[j]], [rst[j]], scale=-0.5)
                    kb.stt("dve", yn[j][:], yz[j][:], rst[j][:, 0:1], nrow[:], ALU.mult, ALU.mult, [yz[j], rst[j], nrow], [yn[j]])
                    for k in range(4):
                        kb.tr(BGb[:, 512 + k * 128:512 + (k + 1) * 128], yn[j][:, k * 128:(k + 1) * 128],
                              self.identb[:], [yn[j], self.identb], [BG], sig=(k == 3))
                    kb.copy("act", yo[i][:, :, cs], BGb[:, 512:1024].rearrange("p (k t) -> p k t", t=128), [BG], [yo[i]])
                kb.dma("sp", dr["YB"][1536:2048, tsl].rearrange("(k p) n -> p k n", p=128), yo[i][:], reads=[yo[i]], writes=[db["YB"]])
            kb.barrier()


    def phase_ssd64(self, l):
        kb = self.kb
        dr, db = self.dram, self.dbuf
        with ExitStack() as es:
            P = self.psum
            pD, pGB, pT, pY1, pY2, pKV = P[0], P[1], P[2], P[3], P[4], P[5]
            pYT = [P[6], P[7]]

            def sbt(name, shape, dt):
                return kb.sb(name, shape, dt, es)
            negm = sbt("negm", [64, 512], F32)
            kb.dma("sp", negm[:], dr["negm64"], reads=[db["negm64"]], writes=[negm])
            ones = sbt("ones64", [64, 512], F32)
            kb.memset("dve", ones[:], 1.0, [ones])
            arow = sbt("arow", [64, 8], F32)
            drow = sbt("drow", [64, 8], F32)
            nrow = sbt("nrow", [64, 512], F32)
            kb.dma("sp", arow[:], dr["m2_alog"][l].partition_broadcast(64), reads=[db["m2_alog"]], writes=[arow])
            kb.dma("sp", drow[:], dr["m2_d"][l].partition_broadcast(64), reads=[db["m2_d"]], writes=[drow])
            kb.dma("sp", nrow[:], dr["m2_normw"][l].partition_broadcast(64), reads=[db["m2_normw"]], writes=[nrow])
            kb.act(arow[:], arow[:], AF.Exp, [arow], [arow])
            S = sbt("mS", [128, 512], F32)
            Sb = sbt("mSb", [128, 512], BF16)
            kb.memset("dve", S[:], 0.0, [S])
            kb.memset("dve", Sb[:], 0.0, [Sb])
            xsT = [sbt("xsT%d" % i, [128, 4, 512], BF16) for i in range(2)]
            BT = [sbt("BT%d" % i, [128, 2, 512], BF16) for i in range(2)]
            CT = [sbt("CT%d" % i, [128, 2, 512], BF16) for i in range(2)]
            zt = [sbt("zt%d" % i, [64, 8, 512], BF16) for i in range(2)]
            dtt = [sbt("dtt%d" % i, [64, 8, 8], F32) for i in range(2)]
            dA = sbt("dA", [64, 8], F32)
            dAn = sbt("dAn", [64, 8], F32)
            X = sbt("X", [64, 512], F32)
            Y = sbt("Y", [64, 512], F32)
            Lm = sbt("Lm", [64, 512], F32)
            scL = sbt("scL", [64, 512], BF16)
            xB = sbt("xB", [64, 768], BF16)
            vv = sbt("vv", [64, 512], BF16)
            vh = sbt("vh", [64, 512], BF16)
            bsb = sbt("bsb", [64, 8], F32)
            eb = sbt("eb", [64, 8], F32)
            dec = sbt("dec", [64, 8], F32)
            eBT = sbt("eBT", [128, 8], F32)
            ta = sbt("ta", [64, 512], F32)
            tb = sbt("tb", [64, 512], F32)
            tc_ = sbt("tc", [64, 512], F32)
            yz = sbt("yz", [64, 512], F32)
            ysq = sbt("ysq", [64, 512], F32)
            ssq = sbt("ssq", [64, 1], F32)
            rst = sbt("rst", [64, 1], F32)
            yn = sbt("yn", [64, 512], BF16)
            tS = sbt("tSm", [128, 512], F32)
            yo = [sbt("myo%d" % i, [128, 4, 512], BF16) for i in range(2)]
            cm3 = self.cmask64[:].rearrange("p (h t) -> p h t", t=64)
            for st in range(NT):
                tsl = slice(st * 512, (st + 1) * 512)
                i = st % 2
                kb.dma("sp", xsT[i][:], dr["XBC"][0:512, tsl].rearrange("(k p) n -> p k n", p=128), reads=[db["XBC"]], writes=[xsT[i]])
                kb.dma("sp", BT[i][:], dr["XBC"][512:768, tsl].rearrange("(k p) n -> p k n", p=128), reads=[db["XBC"]], writes=[BT[i]])
                kb.dma("sp", CT[i][:], dr["XBC"][768:1024, tsl].rearrange("(k p) n -> p k n", p=128), reads=[db["XBC"]], writes=[CT[i]])
                kb.dma("sp", zt[i][:], dr["MZ"][tsl, :].rearrange("(c p) d -> p c d", p=64), reads=[db["MZ"]], writes=[zt[i]])
                kb.dma("sp", dtt[i][:], dr["DT"][tsl, :].rearrange("(c p) d -> p c d", p=64), reads=[db["DT"]], writes=[dtt[i]])
                pYTb = [b.t[:].bitcast(BF16) for b in pYT]
                for c in range(8):
                    cs = slice(c * 64, (c + 1) * 64)
                    kb.tt("dve", dAn[:], dtt[i][:, c, :], arow[:], ALU.mult, [dtt[i], arow], [dAn])
                    kb.ts("dve", dA[:], dAn[:], -1.0, None, ALU.mult, None, [dAn], [dA])
                    kb.tt("dve", X[:].rearrange("p (h t) -> p h t", t=64), cm3,
                          dA[:].unsqueeze(2).to_broadcast([64, 8, 64]), ALU.mult, [self.cmask64, dA], [X])
                    kb.tt("dve", Y[:].rearrange("p (h t) -> p h t", t=64), ones[:].rearrange("p (h t) -> p h t", t=64),
                          dAn[:].unsqueeze(2).to_broadcast([64, 8, 64]), ALU.mult, [ones, dAn], [Y])
                    kb.mm(pD[0:64, :], self.identf[0:64, 0:64], negm[:], start=True, stop=False,
                          reads=[self.identf, negm], writes=[pD], sig=False)
                    kb.mm(pD[0:64, :], self.cmask64[:, 0:64], Y[:], start=False, stop=False,
                          reads=[self.cmask64, Y], writes=[pD], sig=False)
                    kb.mm(pD[0:64, :], ones[:, 0:64], X[:], start=False, stop=True,
                          reads=[ones, X], writes=[pD], sig=True)
                    kb.act(Lm[:], pD[0:64, :], AF.Exp, [pD], [Lm])
                    for g in range(2):
                        kb.mm(pGB[0:64, g * 64:(g + 1) * 64], BT[i][:, g, cs], CT[i][:, g, cs], start=True, stop=True,
                              reads=[BT[i], CT[i]], writes=[pGB], sig=False)
                    kb.mm(pGB[0:64, 128:136], self.cmask64[:, 0:64], dA[:], start=True, stop=True,
                          reads=[self.cmask64, dA], writes=[pGB], sig=False)
                    kb.mm(pGB[0:64, 136:144], ones[:, 0:64], dA[:], start=True, stop=True,
                          reads=[ones, dA], writes=[pGB], sig=False)
                    kb.mm(pGB[:, 144:152], ones[:, 0:128], dA[:], start=True, stop=True,
                          reads=[ones, dA], writes=[pGB], sig=True)
                    for g in range(2):
                        kb.tt("dve", scL[:, g * 256:(g + 1) * 256].rearrange("p (h t) -> p h t", t=64),
                              Lm[:, g * 256:(g + 1) * 256].rearrange("p (h t) -> p h t", t=64),
                              pGB[0:64, g * 64:(g + 1) * 64].unsqueeze(1).to_broadcast([64, 4, 64]), ALU.mult,
                              [Lm, pGB], [scL])
                    kb.copy("act", bsb[:], pGB[0:64, 128:136], [pGB], [bsb])
                    kb.act(eb[:], pGB[0:64, 128:136], AF.Exp, [pGB], [eb])
                    kb.tt("dve", dec[:], pGB[0:64, 136:144], bsb[:], ALU.subtract, [pGB, bsb], [dec])
                    kb.act(dec[:], dec[:], AF.Exp, [dec], [dec])
                    kb.act(eBT[:], pGB[:, 144:152], AF.Exp, [pGB], [eBT])
                    pTb = pT.t[:].bitcast(BF16)
                    for k in range(4):
                        kb.tr(pTb[0:64, k * 128:(k + 1) * 128], xsT[i][:, k, cs], self.identb[:], [xsT[i], self.identb], [pT], sig=False)
                    for g in range(2):
                        kb.tr(pTb[0:64, 512 + g * 128:512 + (g + 1) * 128], BT[i][:, g, cs], self.identb[:],
                              [BT[i], self.identb], [pT], sig=(g == 1))
                    kb.copy("act", xB[:], pTb[0:64, 0:768], [pT], [xB])
                    kb.tt("dve", vv[:].rearrange("p (h t) -> p h t", t=64), xB[:, 0:512].rearrange("p (h t) -> p h t", t=64),
                          dtt[i][:, c, :].unsqueeze(2).to_broadcast([64, 8, 64]), ALU.mult, [xB, dtt[i]], [vv])
                    for h in range(8):
                        hs = slice(h * 64, (h + 1) * 64)
                        kb.mm(pY1[0:64, hs], scL[:, hs], vv[:, hs], start=True, stop=True, reads=[scL, vv], writes=[pY1], sig=(h == 7))
                    for g in range(2):
                        gs = slice(g * 256, (g + 1) * 256)
                        kb.mm(pY2[0:64, gs], CT[i][:, g, cs], Sb[:, gs], start=True, stop=True, reads=[CT[i], Sb], writes=[pY2], sig=(g == 1))
                    kb.tt("dve", ta[:].rearrange("p (h t) -> p h t", t=64), pY2[0:64, :].rearrange("p (h t) -> p h t", t=64),
                          eb[:].unsqueeze(2).to_broadcast([64, 8, 64]), ALU.mult, [pY2, eb], [ta])
                    kb.tt("dve", tb[:].rearrange("p (h t) -> p h t", t=64), xB[:, 0:512].rearrange("p (h t) -> p h t", t=64),
                          drow[:].unsqueeze(2).to_broadcast([64, 8, 64]), ALU.mult, [xB, drow], [tb])
                    kb.tt("dve", tc_[:], pY1[0:64, :], ta[:], ALU.add, [pY1, ta], [tc_])
                    kb.tt("dve", tc_[:], tc_[:], tb[:], ALU.add, [tc_, tb], [tc_])
                    kb.tt("dve", yz[:], tc_[:], zt[i][:, c, :], ALU.mult, [tc_, zt[i]], [yz])
                    kb.memset("dve", ssq[:], 0.0, [ssq])
                    kb.act(ysq[:], yz[:], AF.Square, [yz], [ysq, ssq], accum_out=ssq[:])
                    kb.act(rst[:], ssq[:], AF.Ln, [ssq], [rst], bias=EPS, scale=1.0 / 512.0)
                    kb.act(rst[:], rst[:], AF.Exp, [rst], [rst], scale=-0.5)
                    kb.stt("dve", yn[:], yz[:], rst[:, 0:1], nrow[:], ALU.mult, ALU.mult, [yz, rst, nrow], [yn])
                    for k in range(4):
                        bk = pYT[k // 2]
                        kb.tr(pYTb[k // 2][:, (k % 2) * 512 + c * 64:(k % 2) * 512 + (c + 1) * 64], yn[:, k * 128:(k + 1) * 128],
                              self.identb[0:64, 0:64], [yn, self.identb], [bk], sig=(k % 2 == 1))
                    kb.tt("dve", vh[:].rearrange("p (h t) -> p h t", t=64), vv[:].rearrange("p (h t) -> p h t", t=64),
                          dec[:].unsqueeze(2).to_broadcast([64, 8, 64]), ALU.mult, [vv, dec], [vh])
                    for g in range(2):
                        gs = slice(g * 256, (g + 1) * 256)
                        kb.mm(pKV[:, gs], xB[:, 512 + g * 128:512 + (g + 1) * 128], vh[:, gs], start=True, stop=True,
                              reads=[xB, vh], writes=[pKV], sig=(g == 1))
                    kb.tt("dve", tS[:].rearrange("p (h t) -> p h t", t=64), S[:].rearrange("p (h t) -> p h t", t=64),
                          eBT[:].unsqueeze(2).to_broadcast([128, 8, 64]), ALU.mult, [S, eBT], [tS])
                    kb.tt("dve", S[:], tS[:], pKV[:], ALU.add, [tS, pKV], [S])
                    kb.copy("act", Sb[:], S[:], [S], [Sb])
                for k in range(4):
                    kb.copy("act" if k % 2 else "dve", yo[i][:, k, :], pYTb[k // 2][:, (k % 2) * 512:(k % 2 + 1) * 512],
                            [pYT[k // 2]], [yo[i]])
                kb.dma("sp", dr["YB"][1536:2048, tsl].rearrange("(k p) n -> p k n", p=128), yo[i][:], reads=[yo[i]], writes=[db["YB"]])
            kb.barrier()

    def frac(self, dst, src, ti, tf, bufs_r, dst_t, ti_t, tf_t):
        kb = self.kb
        kb.copy("dve", ti, src, bufs_r, [ti_t])
        kb.copy("dve", tf, ti, [ti_t], [tf_t])
        kb.tt("dve", dst, src, tf, ALU.subtract, bufs_r + [tf_t], [dst_t])

    def phase_s5(self, l):
        kb = self.kb
        dr, db = self.dram, self.dbuf
        J = 4
        NBK = 512 // J
        with ExitStack() as es:
            P = self.psum

            def sbt(name, shape, dt, e=None):
                return kb.sb(name, shape, dt, e or es)
            rho4 = sbt("rho4", [128, 16], F32)
            BTre = [sbt("BTre%d" % j, [128, 16, 128], BF16) for j in range(J)]
            BTim = [sbt("BTim%d" % j, [128, 16, 128], BF16) for j in range(J)]
            W1 = [sbt("W1_%d" % r, [128, 16, 128], BF16) for r in range(J)]
            W2 = [sbt("W2_%d" % r, [128, 16, 128], BF16) for r in range(J)]
            Kt = sbt("Kt", [128, 4, J, 128], BF16)
            cosT = sbt("s5cos", [128, 16, NBK], F32)
            sinT = sbt("s5sin", [128, 16, NBK], F32)
            dsk = sbt("dsk", [128, 4], F32)
            wglu = sbt("wglu", [128, 4, 512], BF16)
            kb.dma("sp", dsk[:], dr["s5_d_pk"][l], reads=[db["s5_d_pk"]], writes=[dsk])
            kb.dma("pool", wglu[:], dr["s5_wglu"][l].rearrange("(k p) n -> p k n", p=128), reads=[db["s5_wglu"]], writes=[wglu])
            with ExitStack() as e1:
                def s1(name, shape, dt=F32):
                    return sbt(name, shape, dt, e1)

                def v16(name):
                    return s1(name, [128, 16])
                lr, li, dt_, tfv, t_f, fr, fr2, fr4 = [v16(n) for n in ("lr", "li", "dtv", "tfv", "t_f", "fr", "fr2", "fr4")]
                t_i = s1("t_i", [128, 16], I32)
                sn, cs_, den, cr, ci, ta, tb, xr, rho = [v16(n) for n in ("sn", "cs", "den", "cr", "ci", "ta", "tb", "xr", "rho")]
                Lr = [v16("Lr%d" % j) for j in range(J + 1)]
                Li = [v16("Li%d" % j) for j in range(J + 1)]
                kb.dma("sp", lr[:], dr["s5_lr_pk"][l], reads=[db["s5_lr_pk"]], writes=[lr])
                kb.dma("sp", li[:], dr["s5_li_pk"][l], reads=[db["s5_li_pk"]], writes=[li])
                kb.dma("sp", dt_[:], dr["s5_ldt_pk"][l], reads=[db["s5_ldt_pk"]], writes=[dt_])
                kb.ts("dve", lr[:], lr[:], -1e-4, None, ALU.min, None, [lr], [lr])
                kb.act(dt_[:], dt_[:], AF.Exp, [dt_], [dt_])
                kb.tt("dve", rho[:], lr[:], dt_[:], ALU.mult, [lr, dt_], [rho])
                kb.act(rho[:], rho[:], AF.Exp, [rho], [rho])
                kb.tt("dve", tfv[:], li[:], dt_[:], ALU.mult, [li, dt_], [tfv])
                kb.ts("dve", tfv[:], tfv[:], 1.0 / TWO_PI, None, ALU.mult, None, [tfv], [tfv])
                self.frac(fr[:], tfv[:], t_i[:], t_f[:], [tfv], fr, t_i, t_f)
                kb.act(sn[:], fr[:], AF.Sin, [fr], [sn], scale=SIN_SCALE)
                kb.ts("dve", tfv[:], fr[:], 0.25, None, ALU.add, None, [fr], [tfv])
                self.frac(fr2[:], tfv[:], t_i[:], t_f[:], [tfv], fr2, t_i, t_f)
                kb.act(cs_[:], fr2[:], AF.Sin, [fr2], [cs_], scale=SIN_SCALE)
                kb.tt("dve", Lr[1][:], rho[:], cs_[:], ALU.mult, [rho, cs_], [Lr[1]])
                kb.tt("dve", Li[1][:], rho[:], sn[:], ALU.mult, [rho, sn], [Li[1]])

                def cmul(orr, oi, ar, ai, br_, bi_):
                    kb.tt("dve", ta[:], ar[:], br_[:], ALU.mult, [ar, br_], [ta])
                    kb.tt("dve", tb[:], ai[:], bi_[:], ALU.mult, [ai, bi_], [tb])
                    kb.tt("dve", orr[:], ta[:], tb[:], ALU.subtract, [ta, tb], [orr])
                    kb.tt("dve", ta[:], ar[:], bi_[:], ALU.mult, [ar, bi_], [ta])
                    kb.tt("dve", tb[:], ai[:], br_[:], ALU.mult, [ai, br_], [tb])
                    kb.tt("dve", oi[:], ta[:], tb[:], ALU.add, [ta, tb], [oi])
                cmul(Lr[2], Li[2], Lr[1], Li[1], Lr[1], Li[1])
                cmul(Lr[3], Li[3], Lr[2], Li[2], Lr[1], Li[1])
                cmul(Lr[4], Li[4], Lr[2], Li[2], Lr[2], Li[2])
                kb.tt("dve", rho4[:], rho[:], rho[:], ALU.mult, [rho], [rho4])
                kb.tt("dve", rho4[:], rho4[:], rho4[:], ALU.mult, [rho4], [rho4])
                kb.ts("dve", tfv[:], fr[:], float(J), None, ALU.mult, None, [fr], [tfv])
                self.frac(fr4[:], tfv[:], t_i[:], t_f[:], [tfv], fr4, t_i, t_f)
                kb.ts("dve", xr[:], Lr[1][:], -1.0, None, ALU.add, None, [Lr[1]], [xr])
                kb.tt("dve", den[:], lr[:], lr[:], ALU.mult, [lr], [den])
                kb.tt("dve", ta[:], li[:], li[:], ALU.mult, [li], [ta])
                kb.tt("dve", den[:], den[:], ta[:], ALU.add, [den, ta], [den])
                kb.op("dve", lambda e: e.reciprocal(den[:], den[:]), [den], [den])
                kb.tt("dve", ta[:], xr[:], lr[:], ALU.mult, [xr, lr], [ta])
                kb.tt("dve", tb[:], Li[1][:], li[:], ALU.mult, [Li[1], li], [tb])
                kb.tt("dve", ta[:], ta[:], tb[:], ALU.add, [ta, tb], [ta])
                kb.tt("dve", cr[:], ta[:], den[:], ALU.mult, [ta, den], [cr])
                kb.tt("dve", ta[:], Li[1][:], lr[:], ALU.mult, [Li[1], lr], [ta])
                kb.tt("dve", tb[:], xr[:], li[:], ALU.mult, [xr, li], [tb])
                kb.tt("dve", ta[:], ta[:], tb[:], ALU.subtract, [ta, tb], [ta])
                kb.tt("dve", ci[:], ta[:], den[:], ALU.mult, [ta, den], [ci])
                bre = s1("bre", [128, 16, 16])
                bim = s1("bim", [128, 16, 16])
                bbr = s1("bbr", [128, 16, 16])
                bbi = s1("bbi", [128, 16, 16])
                tq = s1("tq", [128, 16, 16])
                Zre = s1("Zre", [128, 16, 128])
                Zim = s1("Zim", [128, 16, 128])
                Zjr = s1("Zjr", [128, 16, 128])
                Zji = s1("Zji", [128, 16, 128])
                Zt = s1("Zt", [128, 16, 128])
                kb.dma("sp", bre[:], dr["s5_bre_pk"][l], reads=[db["s5_bre_pk"]], writes=[bre])
                kb.dma("sp", bim[:], dr["s5_bim_pk"][l], reads=[db["s5_bim_pk"]], writes=[bim])
                crb = cr[:].unsqueeze(2).to_broadcast([128, 16, 16])
                cib = ci[:].unsqueeze(2).to_broadcast([128, 16, 16])
                kb.tt("dve", bbr[:], bre[:], crb, ALU.mult, [bre, cr], [bbr])
                kb.tt("dve", tq[:], bim[:], cib, ALU.mult, [bim, ci], [tq])
                kb.tt("dve", bbr[:], bbr[:], tq[:], ALU.subtract, [bbr, tq], [bbr])
                kb.tt("dve", bbi[:], bim[:], crb, ALU.mult, [bim, cr], [bbi])
                kb.tt("dve", tq[:], bre[:], cib, ALU.mult, [bre, ci], [tq])
                kb.tt("dve", bbi[:], bbi[:], tq[:], ALU.add, [bbi, tq], [bbi])
                kb.memset("dve", Zre[:], 0.0, [Zre])
                kb.memset("dve", Zim[:], 0.0, [Zim])
                for g2 in range(2):
                    rows = slice(g2 * 64, (g2 + 1) * 64)
                    for q in range(4):
                        col = (2 * q + g2) * 16
                        kb.copy("dve", Zre[rows, q::4, col:col + 16], bbr[rows, q::4, :], [bbr], [Zre])
                        kb.copy("dve", Zim[rows, q::4, col:col + 16], bbi[rows, q::4, :], [bbi], [Zim])
                Cre_f = s1("Cre_f", [128, 16, 128])
                Cimn_f = s1("Cimn_f", [128, 16, 128])
                cmk = s1("cmk", [128, 4, 128])
                kb.dma("sp", cmk[:], dr["s5_cmask"].rearrange("q p m -> p q m"), reads=[db["s5_cmask"]], writes=[cmk])
                c2 = [s1("c2_%d" % i, [128, 128]) for i in range(2)]
                ctr = [s1("ctr_%d" % i, [128, 128]) for i in range(2)]
                bi_ = 0
                for (nm, Cd, sgn) in (("s5_cre_pk", Cre_f, 1.0), ("s5_cim_pk", Cimn_f, -1.0)):
                    for ct in range(4):
                        cc = c2[ct % 2]
                        ctt = ctr[ct % 2]
                        kb.dma("sp", cc[:, 0:64], dr[nm][l, ct], reads=[db[nm]], writes=[cc])
                        kb.dma("sp", cc[:, 64:128], dr[nm][l, ct], reads=[db[nm]], writes=[cc])
                        ps = P[bi_ % 8]
                        bi_ += 1
                        kb.tr(ps[:, 0:128], cc[:], self.identf[:], [cc, self.identf], [ps])
                        kb.ts("dve", ctt[:], ps[:, 0:128], sgn, None, ALU.mult, None, [ps], [ctt])
                        for q in range(4):
                            kb.tt("dve", Cd[:, ct * 4 + q, :], ctt[:], cmk[:, q, :], ALU.mult, [ctt, cmk], [Cd])
                for j in range(J):
                    if j == 0:
                        zr_, zi_ = Zre, Zim
                    else:
                        ab = Lr[j][:].unsqueeze(2).to_broadcast([128, 16, 128])
                        bb_ = Li[j][:].unsqueeze(2).to_broadcast([128, 16, 128])
                        kb.tt("dve", Zjr[:], Zre[:], ab, ALU.mult, [Zre, Lr[j]], [Zjr])
                        kb.tt("dve", Zt[:], Zim[:], bb_, ALU.mult, [Zim, Li[j]], [Zt])
                        kb.tt("dve", Zjr[:], Zjr[:], Zt[:], ALU.subtract, [Zjr, Zt], [Zjr])
                        kb.tt("dve", Zji[:], Zim[:], ab, ALU.mult, [Zim, Lr[j]], [Zji])
                        kb.tt("dve", Zt[:], Zre[:], bb_, ALU.mult, [Zre, Li[j]], [Zt])
                        kb.tt("dve", Zji[:], Zji[:], Zt[:], ALU.add, [Zji, Zt], [Zji])
                        zr_, zi_ = Zjr, Zji
                    for (Z, BTd) in ((zr_, BTre[j]), (zi_, BTim[j])):
                        for g4 in range(4):
                            ps = P[bi_ % 8]
                            bi_ += 1
                            for jj in range(4):
                                gp = g4 * 4 + jj
                                kb.tr(ps[:, jj * 128:(jj + 1) * 128], Z[:, gp, :], self.identf[:], [Z, self.identf], [ps], sig=(jj == 3))
                            kb.copy("act", BTd[:, g4 * 4:(g4 + 1) * 4, :], ps[:].rearrange("p (j m) -> p j m", m=128), [ps], [BTd])
                    ps = P[bi_ % 8]
                    bi_ += 1
                    for ct in range(4):
                        for q in range(4):
                            gp = ct * 4 + q
                            kb.mm(ps[:, ct * 128:(ct + 1) * 128], zr_[:, gp, :], Cre_f[:, gp, :], start=(q == 0), stop=False,
                                  reads=[zr_, Cre_f], writes=[ps], sig=False)
                            kb.mm(ps[:, ct * 128:(ct + 1) * 128], zi_[:, gp, :], Cimn_f[:, gp, :], start=False, stop=(q == 3),
                                  reads=[zi_, Cimn_f], writes=[ps], sig=(q == 3))
                    kb.copy("act", Kt[:, :, j, :], ps[:].rearrange("p (c m) -> p c m", m=128), [ps], [Kt])
                for r in range(J):
                    ab = Lr[r + 1][:].unsqueeze(2).to_broadcast([128, 16, 128])
                    bb_ = Li[r + 1][:].unsqueeze(2).to_broadcast([128, 16, 128])
                    kb.tt("dve", Zjr[:], Cre_f[:], ab, ALU.mult, [Cre_f, Lr[r + 1]], [Zjr])
                    kb.tt("dve", Zt[:], Cimn_f[:], bb_, ALU.mult, [Cimn_f, Li[r + 1]], [Zt])
                    kb.tt("dve", W1[r][:], Zjr[:], Zt[:], ALU.add, [Zjr, Zt], [W1[r]])
                    kb.tt("dve", Zji[:], Cimn_f[:], ab, ALU.mult, [Cimn_f, Lr[r + 1]], [Zji])
                    kb.tt("dve", Zt[:], Cre_f[:], bb_, ALU.mult, [Cre_f, Li[r + 1]], [Zt])
                    kb.tt("dve", W2[r][:], Zji[:], Zt[:], ALU.subtract, [Zji, Zt], [W2[r]])
                io = s1("iota", [128, 512])
                kb.dma("sp", io[:], dr["iota1"], reads=[db["iota1"]], writes=[io])
                an = s1("an", [128, NBK])
                a_i = s1("a_i", [128, NBK], I32)
                a_f = s1("a_f", [128, NBK])
                a_r = s1("a_r", [128, NBK])
                for gp in range(16):
                    kb.ts("dve", an[:], io[:, 0:NBK], fr4[:, gp:gp + 1], None, ALU.mult, None, [io, fr4], [an])
                    self.frac(a_r[:], an[:], a_i[:], a_f[:], [an], a_r, a_i, a_f)
                    kb.act(sinT[:, gp, :], a_r[:], AF.Sin, [a_r], [sinT], scale=SIN_SCALE)
                    kb.ts("dve", an[:], a_r[:], 0.25, None, ALU.add, None, [a_r], [an])
                    self.frac(a_r[:], an[:], a_i[:], a_f[:], [an], a_r, a_i, a_f)
                    kb.act(cosT[:, gp, :], a_r[:], AF.Sin, [a_r], [cosT], scale=SIN_SCALE)
                kb.barrier()
            import os
            if os.environ.get("S5_STOP") == "setup":
                return
            sre_p = sbt("sre_p", [128, 16], F32)
            sim_p = sbt("sim_p", [128, 16], F32)
            kb.memset("dve", sre_p[:], 0.0, [sre_p])
            kb.memset("dve", sim_p[:], 0.0, [sim_p])
            SPr = [sbt("SPr%d" % i, [128, 16, NBK + 4], BF16) for i in range(2)]
            SPi = [sbt("SPi%d" % i, [128, 16, NBK + 4], BF16) for i in range(2)]
            for i in range(2):
                kb.memset("dve", SPr[i][:], 0.0, [SPr[i]])
                kb.memset("dve", SPi[i][:], 0.0, [SPi[i]])
            uT = [sbt("s5u%d" % i, [128, 4, 512], BF16) for i in range(2)]
            u4 = [sbt("s5u4_%d" % i, [128, 4, J, NBK], BF16) for i in range(2)]
            NB = 2

            def mk(name, dt=F32, n=NB, w=2 * NBK):
                return [sbt("%s%d" % (name, i), [128, w], dt) for i in range(n)]
            t1, t2, t3, t4 = mk("s5t1"), mk("s5t2"), mk("s5t3"), mk("s5t4")
            zir, zii, zr, zi = mk("zir"), mk("zii"), mk("zr"), mk("zi")
            u1, u2, u3, u4_ = mk("u1"), mk("u2"), mk("u3"), mk("u4")
            yg = [sbt("yg%d" % i, [128, 4, 512], BF16) for i in range(2)]
            gx = mk("gx", w=512)
            gsq = mk("gsq", w=512)
            gin = mk("gin", w=512)
            gsg = mk("gsg", w=512)
            yo = [sbt("s5yo%d" % i, [128, 4, 512], BF16) for i in range(2)]
            psA = [P[0], P[1]]
            psB = [P[2], P[3]]
            psY = [P[4], P[5]]
            psG = [P[6], P[7]]
            for st in range(NT):
                tsl = slice(st * 512, (st + 1) * 512)
                ui = uT[st % 2]
                ud = u4[st % 2]
                spr, spi = SPr[st % 2], SPi[st % 2]
                spr_o, spi_o = SPr[(st + 1) % 2], SPi[(st + 1) % 2]
                kb.dma("sp", ui[:], dr["UT"][:, tsl].rearrange("(k p) n -> p k n", p=128), reads=[db["UT"]], writes=[ui])
                kb.copy("act", ud[:], ui[:].rearrange("p k (c r) -> p k r c", r=J), [ui], [ud])
                if st > 0:
                    kb.copy("dve", spr[:, :, 0:1], spr_o[:, :, NBK:NBK + 1], [spr_o], [spr])
                    kb.copy("dve", spi[:, :, 0:1], spi_o[:, :, NBK:NBK + 1], [spi_o], [spi])
                ygt = yg[st % 2]
                for ct in range(4):
                    pY = psY[ct % 2]

                    def body(pq, j):
                        gp0 = ct * 4 + 2 * pq
                        pa, pb = psA[j], psB[j]
                        W2_ = 2 * NBK
                        cg = cosT[:, gp0:gp0 + 2, :]
                        sg = sinT[:, gp0:gp0 + 2, :]

                        def v3(t_):
                            return t_[:, 0:W2_].rearrange("p (g c) -> p g c", g=2)
                        for gi_ in range(2):
                            for tp in range(J):
                                kb.mm(pa[:, gi_ * NBK:(gi_ + 1) * NBK], BTre[tp][:, gp0 + gi_, :], ud[:, ct, J - 1 - tp, :],
                                      start=(tp == 0), stop=(tp == J - 1), reads=[BTre[tp], ud], writes=[pa], sig=(tp == J - 1 and gi_ == 1))
                        for gi_ in range(2):
                            for tp in range(J):
                                kb.mm(pb[:, gi_ * NBK:(gi_ + 1) * NBK], BTim[tp][:, gp0 + gi_, :], ud[:, ct, J - 1 - tp, :],
                                      start=(tp == 0), stop=(tp == J - 1), reads=[BTim[tp], ud], writes=[pb], sig=(tp == J - 1 and gi_ == 1))
                        yield
                        kb.tt("dve", v3(t1[j]), v3(pa), cg, ALU.mult, [pa, cosT], [t1[j]])
                        yield
                        kb.tt("dve", v3(t2[j]), v3(pb), sg, ALU.mult, [pb, sinT], [t2[j]])
                        yield
                        kb.tt("dve", v3(t3[j]), v3(pb), cg, ALU.mult, [pb, cosT], [t3[j]])
                        yield
                        kb.tt("dve", v3(t4[j]), v3(pa), sg, ALU.mult, [pa, sinT], [t4[j]])
                        yield
                        kb.tt("dve", zir[j][:], t1[j][:], t2[j][:], ALU.add, [t1[j], t2[j]], [zir[j]])
                        yield
                        kb.tt("dve", zii[j][:], t3[j][:], t4[j][:], ALU.subtract, [t3[j], t4[j]], [zii[j]])
                        yield
                        for gi_ in range(2):
                            gp = gp0 + gi_
                            gsl = slice(gi_ * NBK, (gi_ + 1) * NBK)
                            rb = rho4[:, gp:gp + 1].to_broadcast([128, NBK])
                            kb.op("dve", lambda e: e.tensor_tensor_scan(zr[j][:, gsl], rb, zir[j][:, gsl], sre_p[:, gp:gp + 1], ALU.mult, ALU.add),
                                  [rho4, zir[j], sre_p], [zr[j]])
                            yield
                            kb.op("dve", lambda e: e.tensor_tensor_scan(zi[j][:, gsl], rb, zii[j][:, gsl], sim_p[:, gp:gp + 1], ALU.mult, ALU.add),
                                  [rho4, zii[j], sim_p], [zi[j]])
                            yield
                        kb.tt("dve", v3(u1[j]), v3(zr[j]), cg, ALU.mult, [zr[j], cosT], [u1[j]])
                        yield
                        kb.tt("dve", v3(u2[j]), v3(zi[j]), sg, ALU.mult, [zi[j], sinT], [u2[j]])
                        yield
                        kb.tt("dve", v3(u3[j]), v3(zr[j]), sg, ALU.mult, [zr[j], sinT], [u3[j]])
                        yield
                        kb.tt("dve", v3(u4_[j]), v3(zi[j]), cg, ALU.mult, [zi[j], cosT], [u4_[j]])
                        yield
                        kb.tt("dve", spr[:, gp0:gp0 + 2, 1:NBK + 1], v3(u1[j]), v3(u2[j]), ALU.subtract, [u1[j], u2[j]], [spr])
                        yield
                        kb.tt("dve", spi[:, gp0:gp0 + 2, 1:NBK + 1], v3(u3[j]), v3(u4_[j]), ALU.add, [u3[j], u4_[j]], [spi])
                        yield
                        kb.tt("dve", sre_p[:, gp0:gp0 + 2], v3(u1[j])[:, :, NBK - 1], v3(u2[j])[:, :, NBK - 1], ALU.subtract, [u1[j], u2[j]], [sre_p])
                        kb.tt("dve", sim_p[:, gp0:gp0 + 2], v3(u3[j])[:, :, NBK - 1], v3(u4_[j])[:, :, NBK - 1], ALU.add, [u3[j], u4_[j]], [sim_p])
                        yield

                    gens = [body(0, 0), body(1, 1)]
                    while gens:
                        for g_ in list(gens):
                            try:
                                next(g_)
                            except StopIteration:
                                gens.remove(g_)
                    for r in range(J):
                        osl = slice(r * NBK, (r + 1) * NBK)
                        n_mm = 8 + r + 1
                        i_mm = 0
                        for q in range(4):
                            gp = ct * 4 + q
                            kb.mm(pY[:, osl], W1[r][:, gp, :], spr[:, gp, 0:NBK], start=(i_mm == 0), stop=False,
                                  reads=[W1[r], spr], writes=[pY], sig=False)
                            i_mm += 1
                            kb.mm(pY[:, osl], W2[r][:, gp, :], spi[:, gp, 0:NBK], start=False, stop=False,
                                  reads=[W2[r], spi], writes=[pY], sig=False)
                            i_mm += 1
                        for tp in range(r + 1):
                            i_mm += 1
                            kb.mm(pY[:, osl], Kt[:, ct, tp, :], ud[:, ct, r - tp, :], start=False, stop=(i_mm == n_mm),
                                  reads=[Kt, ud], writes=[pY], sig=(i_mm == n_mm))
                    jj = ct % 2
                    kb.stt("dve", gx[jj][:].rearrange("p (c r) -> p c r", r=J), ui[:, ct, :].rearrange("p (c r) -> p c r", r=J),
                           dsk[:, ct:ct + 1], pY[:].rearrange("p (r c) -> p c r", r=J), ALU.mult, ALU.add, [ui, dsk, pY], [gx[jj]])
                    kb.act(gsq[jj][:], gx[jj][:], AF.Square, [gx[jj]], [gsq[jj]])
                    kb.ts("dve", gsq[jj][:], gsq[jj][:], 0.044715, 1.0, ALU.mult, ALU.add, [gsq[jj]], [gsq[jj]])
                    kb.tt("dve", gin[jj][:], gsq[jj][:], gx[jj][:], ALU.mult, [gsq[jj], gx[jj]], [gin[jj]])
                    kb.act(gsg[jj][:], gin[jj][:], AF.Sigmoid, [gin[jj]], [gsg[jj]], scale=1.5957691216057308)
                    kb.tt("dve", ygt[:, ct, :], gx[jj][:], gsg[jj][:], ALU.mult, [gx[jj], gsg[jj]], [ygt])
                if os.environ.get("S5_STOP") == "up":
                    continue
                yot = yo[st % 2]
                for ob in range(4):
                    pG = psG[ob % 2]
                    for k in range(4):
                        kb.mm(pG[:], wglu[:, k, ob * 128:(ob + 1) * 128], ygt[:, k, :], start=(k == 0), stop=(k == 3),
                              reads=[wglu, ygt], writes=[pG])
                    jj = ob % 2
                    kb.act(gsg[jj][:], pG[:], AF.Sigmoid, [pG], [gsg[jj]])
                    kb.tt("dve", yot[:, ob, :], ygt[:, ob, :], gsg[jj][:], ALU.mult, [ygt, gsg[jj]], [yot])
                kb.dma("sp", dr["YB"][0:512, tsl].rearrange("(k p) n -> p k n", p=128), yot[:], reads=[yot], writes=[db["YB"]])
            kb.barrier()

    def phase_c(self, l):
        kb = self.kb
        dr, db = self.dram, self.dbuf
        xsrc = "xT" if l == 0 else "XS"
        W = 512
        with ExitStack() as es:
            def sbt(name, shape, dt):
                return kb.sb(name, shape, dt, es)
            wg = sbt("wg", [128, 8, 4 * D], BF16)
            wbr = sbt("wbr", [128, 16, D], BF16)
            wo = sbt("wo", [128, 8, D], BF16)
            bg = sbt("bg", [128, 32], F32)
            kb.dma("sp", bg[:], dr["b_gate_pk"][l], reads=[db["b_gate_pk"]], writes=[bg])
            for j in range(8):
                kb.dma("pool", wg[:, :, j * 512:(j + 1) * 512], dr["w_gate"][l, :, j * 512:(j + 1) * 512].rearrange("(k p) n -> p k n", p=128),
                       reads=[db["w_gate"]], writes=[wg])
            for j in range(2):
                kb.dma("pool", wbr[:, :, j * 512:(j + 1) * 512], dr["w_branch"][l, :, j * 512:(j + 1) * 512].rearrange("(k p) n -> p k n", p=128),
                       reads=[db["w_branch"]], writes=[wbr])
                kb.dma("pool", wo[:, :, j * 512:(j + 1) * 512], dr["w_out"][l, :, j * 512:(j + 1) * 512].rearrange("(k p) n -> p k n", p=128),
                       reads=[db["w_out"]], writes=[wo])
            xt = [sbt("cx%d" % i, [128, 8, W], F32) for i in range(1)] * 2
            yt = [sbt("cy%d" % i, [128, 16, W], BF16) for i in range(1)] * 2
            sq = sbt("csq", [128, 8, W], BF16)
            tmp = sbt("ctmp", [128, 8, W], F32)
            rstd = sbt("crstd", [128, W], F32)
            hb = sbt("chb", [128, 8, W], BF16)
            mt = sbt("cm", [128, 8, W], BF16)
            gs = [sbt("cg%d" % i, [128, W], F32) for i in range(2)]
            pr = [sbt("cp%d" % i, [128, W], F32) for i in range(2)]
            macc = [sbt("cma%d" % i, [128, W], F32) for i in range(2)]
            gi = 0
            hbs = [hb, hb]
            NTC = SEQ // W

            def prep(t):
                tsl_ = slice(t * W, (t + 1) * W)
                x_t_ = xt[t % 2]
                y_t_ = yt[t % 2]
                kb.dma("sp", x_t_[:], dr[xsrc][:, tsl_].rearrange("(k p) n -> p k n", p=128), reads=[db[xsrc]], writes=[x_t_])
                kb.dma("sp", y_t_[:], dr["YB"][:, tsl_].rearrange("(k p) n -> p k n", p=128), reads=[db["YB"]], writes=[y_t_])
                self.h_tile(x_t_, None, l, 0, sq, rstd, tmp, W=W)
                for k in range(8):
                    kb.act(hbs[t % 2][:, k, :], tmp[:, k, :], AF.Identity, [tmp, self.mod], [hbs[t % 2]], bias=self.mod[:, l, k:k + 1])
            prep(0)
            for t in range(NTC):
                tsl = slice(t * W, (t + 1) * W)
                x_t = xt[t % 2]
                y_t = yt[t % 2]
                hb = hbs[t % 2]
                if t > 0:
                    prep(t)
                for d in range(8):
                    ma = macc[d % 2]
                    for n in range(4):
                        pg = self.bank()
                        for k in range(8):
                            kb.mm(pg[:, 0:W], wg[:, k, n * D + d * 128:n * D + (d + 1) * 128], hb[:, k, :], start=(k == 0), stop=(k == 7),
                                  reads=[wg, hb], writes=[pg])
                        pb = self.bank()
                        for k in range(4):
                            kb.mm(pb[:, 0:W], wbr[:, n * 4 + k, d * 128:(d + 1) * 128], y_t[:, n * 4 + k, :], start=(k == 0), stop=(k == 3),
                                  reads=[wbr, y_t], writes=[pb])
                        g = gs[gi % 2]
                        p_ = pr[gi % 2]
                        gi += 1
                        kb.act(g[:], pg[:, 0:W], AF.Sigmoid, [pg, bg], [g], bias=bg[:, n * 8 + d:n * 8 + d + 1])
                        if n == 0:
                            kb.tt("dve", ma[:], g[:], pb[:, 0:W], ALU.mult, [g, pb], [ma])
                        else:
                            kb.tt("dve", p_[:], g[:], pb[:, 0:W], ALU.mult, [g, pb], [p_])
                            if n < 3:
                                kb.tt("dve", ma[:], ma[:], p_[:], ALU.add, [ma, p_], [ma])
                            else:
                                kb.tt("dve", mt[:, d, :], ma[:], p_[:], ALU.add, [ma, p_], [mt])
                for ob in range(8):
                    po = self.bank()
                    for d in range(8):
                        kb.mm(po[:, 0:W], wo[:, d, ob * 128:(ob + 1) * 128], mt[:, d, :], start=(d == 0), stop=(d == 7),
                              reads=[wo, mt], writes=[po])
                    kb.stt("dve", x_t[:, ob, :], po[:, 0:W], self.mod[:, l, 16 + ob:16 + ob + 1], x_t[:, ob, :], ALU.mult, ALU.add,
                           [po, self.mod, x_t], [x_t])
                kb.dma("sp", dr["XS"][:, tsl].rearrange("(k p) n -> p k n", p=128), x_t[:], reads=[x_t], writes=[db["XS"]])
            kb.barrier()

    def phase_moe(self, l):
        kb = self.kb
        dr, db = self.dram, self.dbuf
        MT = 1024
        with ExitStack() as es:
            def sbt(name, shape, dt):
                return kb.sb(name, shape, dt, es)
            acc = sbt("acc", [128, 8, MT], F32)
            h2 = sbt("h2", [128, 8, MT], BF16)
            actb = sbt("actb", [128, 16, MT], BF16)
            w2g = sbt("w2g", [128, 16, D], BF16)
            w13 = [sbt("w13_%d" % i, [128, 8, 512], BF16) for i in range(2)]
            combT = sbt("combT", [32, MT], F32)
            cbs = [sbt("cbs%d" % i, [128, 512], F32) for i in range(4)]
            cbi = 0
            wr = sbt("wr", [128, 8, 36], F32)
            br = sbt("br", [128, 36], F32)
            kb.dma("sp", wr[:], dr["moe_wr"][l].rearrange("(k p) n -> p k n", p=128), reads=[db["moe_wr"]], writes=[wr])
            kb.dma("sp", br[:], dr["moe_br"][l].partition_broadcast(128), reads=[db["moe_br"]], writes=[br])
            sq = sbt("msq", [128, 8, 512], F32)
            tmp = sbt("mtmp", [128, 8, 512], F32)
            rstd = sbt("mrstd", [128, 512], F32)
            sl_ = [sbt("msl%d" % i, [128, 512], F32) for i in range(2)]
            tl_ = [sbt("mtl%d" % i, [128, 512], F32) for i in range(2)]
            RS = []
            for q_ in range(4):
                R = {}
                for nm, shp in (("lg", [128, 36]), ("gmax", [128, 1]), ("ngm", [128, 1]), ("goh", [128, 4]), ("gex", [128, 4]),
                                ("gsum", [128, 1]), ("gw", [128, 1]), ("e3", [128, 4, 8]), ("esel", [128, 8]), ("m1", [128, 1]),
                                ("m2", [128, 1]), ("oh1", [128, 8]), ("oh2", [128, 8]), ("e2", [128, 8]), ("dd", [128, 1]),
                                ("ed", [128, 1]), ("den", [128, 1]), ("w1", [128, 1]), ("w2", [128, 1]), ("c1", [128, 8]),
                                ("ce", [128, 8]), ("comb", [128, 32])):
                    R[nm] = sbt("r%d_%s" % (q_, nm), shp, F32)
                RS.append(R)
            si = 0
            wi = 0
            for mt_ in range(SEQ // MT):
                msl = slice(mt_ * MT, (mt_ + 1) * MT)
                kb.dma("sp", acc[:], dr["XS"][:, msl].rearrange("(k p) n -> p k n", p=128), reads=[db["XS"]], writes=[acc])
                def route(sub_id, hf, sub, R):
                    ss = slice(sub * 128, (sub + 1) * 128)
                    ps = self.bank()
                    for k in range(8):
                        kb.mm(ps[:, 0:36], tmp[:, k, ss], wr[:, k, :], start=(k == 0), stop=(k == 7), reads=[tmp, wr], writes=[ps])
                    yield
                    kb.tt("dve", R["lg"][:], ps[:, 0:36], br[:], ALU.add, [ps, br], [R["lg"]])
                    yield
                    kb.op("dve", lambda e: e.tensor_reduce(R["gmax"][:], R["lg"][:, 0:4], AX.X, ALU.max), [R["lg"]], [R["gmax"]])
                    yield
                    kb.ts("dve", R["goh"][:], R["lg"][:, 0:4], R["gmax"][:, 0:1], None, ALU.is_equal, None, [R["lg"], R["gmax"]], [R["goh"]])
                    kb.ts("dve", R["ngm"][:], R["gmax"][:], -1.0, None, ALU.mult, None, [R["gmax"]], [R["ngm"]])
                    kb.memset("dve", R["gsum"][:], 0.0, [R["gsum"]])
                    yield
                    kb.act(R["gex"][:], R["lg"][:, 0:4], AF.Exp, [R["lg"], R["ngm"]], [R["gex"], R["gsum"]], bias=R["ngm"][:, 0:1], accum_out=R["gsum"][:])
                    kb.tt("dve", R["e3"][:], R["lg"][:, 4:36].rearrange("p (g e) -> p g e", e=8), R["goh"][:].unsqueeze(2).to_broadcast([128, 4, 8]),
                          ALU.mult, [R["lg"], R["goh"]], [R["e3"]])
                    yield
                    kb.op("dve", lambda e: e.tensor_reduce(R["esel"][:], R["e3"][:].rearrange("p g e -> p e g"), AX.X, ALU.add), [R["e3"]], [R["esel"]])
                    yield
                    kb.op("dve", lambda e: e.tensor_reduce(R["m1"][:], R["esel"][:], AX.X, ALU.max), [R["esel"]], [R["m1"]])
                    yield
                    kb.ts("dve", R["oh1"][:], R["esel"][:], R["m1"][:, 0:1], None, ALU.is_equal, None, [R["esel"], R["m1"]], [R["oh1"]])
                    yield
                    kb.stt("dve", R["e2"][:], R["oh1"][:], -1e30, R["esel"][:], ALU.mult, ALU.add, [R["oh1"], R["esel"]], [R["e2"]])
                    yield
                    kb.op("dve", lambda e: e.tensor_reduce(R["m2"][:], R["e2"][:], AX.X, ALU.max), [R["e2"]], [R["m2"]])
                    yield
                    kb.ts("dve", R["oh2"][:], R["e2"][:], R["m2"][:, 0:1], None, ALU.is_equal, None, [R["e2"], R["m2"]], [R["oh2"]])
                    kb.tt("dve", R["dd"][:], R["m2"][:], R["m1"][:], ALU.subtract, [R["m2"], R["m1"]], [R["dd"]])
                    yield
                    kb.act(R["ed"][:], R["dd"][:], AF.Exp, [R["dd"]], [R["ed"]])
                    kb.op("dve", lambda e: e.reciprocal(R["gw"][:], R["gsum"][:]), [R["gsum"]], [R["gw"]])
                    yield
                    kb.ts("dve", R["den"][:], R["ed"][:], 1.0, None, ALU.add, None, [R["ed"]], [R["den"]])
                    yield
                    kb.op("dve", lambda e: e.reciprocal(R["den"][:], R["den"][:]), [R["den"]], [R["den"]])
                    yield
                    kb.tt("dve", R["w1"][:], R["den"][:], R["gw"][:], ALU.mult, [R["den"], R["gw"]], [R["w1"]])
                    yield
                    kb.tt("dve", R["w2"][:], R["w1"][:], R["ed"][:], ALU.mult, [R["w1"], R["ed"]], [R["w2"]])
                    kb.ts("dve", R["c1"][:], R["oh1"][:], R["w1"][:, 0:1], None, ALU.mult, None, [R["oh1"], R["w1"]], [R["c1"]])
                    yield
                    kb.stt("dve", R["ce"][:], R["oh2"][:], R["w2"][:, 0:1], R["c1"][:], ALU.mult, ALU.add, [R["oh2"], R["w2"], R["c1"]], [R["ce"]])
                    yield
                    kb.tt("dve", R["comb"][:].rearrange("p (g e) -> p g e", e=8), R["goh"][:].unsqueeze(2).to_broadcast([128, 4, 8]),
                          R["ce"][:].unsqueeze(1).to_broadcast([128, 4, 8]), ALU.mult, [R["goh"], R["ce"]], [R["comb"]])
                    yield
                    pt = self.bank()
                    kb.tr(pt[0:32, 0:128], R["comb"][:], self.identf[:], [R["comb"], self.identf], [pt])
                    kb.copy("act", combT[:, hf * 512 + sub * 128:hf * 512 + (sub + 1) * 128], pt[0:32, 0:128], [pt], [combT])
                    yield

                for hf in range(MT // 512):
                    hs = slice(hf * 512, (hf + 1) * 512)
                    xv = T(acc.t[:, :, hs], "accv")
                    xv.b = acc.b
                    self.h_tile(xv, None, l, 1, sq, rstd, tmp, W=512, fp32_sq=True)
                    for k in range(8):
                        kb.ts("dve", tmp[:, k, :], tmp[:, k, :], self.mod[:, l, 24 + k:24 + k + 1], None, ALU.add, None,
                              [tmp, self.mod], [tmp])
                        kb.copy("act", h2[:, k, hs], tmp[:, k, :], [tmp], [h2])
                    gens = [route(hf * 4 + sub, hf, sub, RS[sub]) for sub in range(4)]
                    while gens:
                        for g_ in list(gens):
                            try:
                                next(g_)
                            except StopIteration:
                                gens.remove(g_)
                cmb_d = dr["CMB"][mt_ % 2]
                kb.dma("sp", cmb_d, combT[:], reads=[combT], writes=[db["CMB"]])
                for grp in range(4):
                    kb.dma("sp", w2g[:].rearrange("p k n -> p (k n)"), dr["MW2"][l, grp], reads=[db["MW2"]], writes=[w2g])
                    for e in range(8):
                        ge = grp * 8 + e
                        w = w13[wi % 2]
                        wi += 1
                        kb.dma("sp", w[:].rearrange("p k n -> p (k n)"), dr["MW13"][l, ge], reads=[db["MW13"]], writes=[w])
                        for ts_ in range(MT // 512):
                            tsl = slice(ts_ * 512, (ts_ + 1) * 512)
                            pcb = cbs[cbi % 4]
                            cbi += 1
                            kb.dma("sp", pcb[:], cmb_d[ge:ge + 1, tsl].partition_broadcast(128), reads=[db["CMB"]], writes=[pcb])
                            for fb in range(2):
                                p1 = self.bank()
                                for k in range(8):
                                    kb.mm(p1[:], w[:, k, fb * 128:(fb + 1) * 128], h2[:, k, tsl], start=(k == 0), stop=(k == 7), reads=[w, h2], writes=[p1])
                                p3 = self.bank()
                                for k in range(8):
                                    kb.mm(p3[:], w[:, k, 256 + fb * 128:256 + (fb + 1) * 128], h2[:, k, tsl], start=(k == 0), stop=(k == 7), reads=[w, h2], writes=[p3])
                                sl = sl_[si % 2]
                                tl = tl_[si % 2]
                                si += 1
                                kb.act(sl[:], p1[:], AF.Silu, [p1], [sl])
                                kb.tt("dve", tl[:], sl[:], p3[:], ALU.mult, [sl, p3], [tl])
                                kb.tt("dve", actb[:, e * 2 + fb, tsl], tl[:], pcb[:], ALU.mult, [tl, pcb], [actb])
                    for ob in range(8):
                        for ts_ in range(MT // 512):
                            tsl = slice(ts_ * 512, (ts_ + 1) * 512)
                            po = self.bank()
                            for kk in range(16):
                                kb.mm(po[:], w2g[:, kk, ob * 128:(ob + 1) * 128], actb[:, kk, tsl], start=(kk == 0), stop=(kk == 15),
                                      reads=[w2g, actb], writes=[po])
                            kb.stt("dve", acc[:, ob, tsl], po[:], self.mod[:, l, 40 + ob:40 + ob + 1], acc[:, ob, tsl], ALU.mult, ALU.add,
                                   [po, self.mod, acc], [acc])
                kb.dma("sp", dr["XS"][:, msl].rearrange("(k p) n -> p k n", p=128), acc[:], reads=[acc], writes=[db["XS"]])
            kb.barrier()

    def phase_final(self):
        kb = self.kb
        dr, db = self.dram, self.dbuf
        with ExitStack() as es:
            fw = kb.sb("fw", [128, 8], F32, es)
            kb.dma("sp", fw[:], dr["fnorm_pk"], reads=[db["fnorm_pk"]], writes=[fw])
            xt = [kb.sb("fx%d" % i, [128, 8, 512], F32, es) for i in range(2)]
            ot = [kb.sb("fo%d" % i, [128, 8, 512], F32, es) for i in range(2)]
            sq = kb.sb("fsq", [128, 8, 512], F32, es)
            rstd = kb.sb("frs", [128, 512], F32, es)
            for t in range(NT):
                tsl = slice(t * 512, (t + 1) * 512)
                x_t = xt[t % 2]
                o_t = ot[t % 2]
                kb.dma("sp", x_t[:], dr["XS"][:, tsl].rearrange("(k p) n -> p k n", p=128), reads=[db["XS"]], writes=[x_t])
                ps = self.bank()
                kb.act(sq[:], x_t[:], AF.Square, [x_t], [sq])
                for k in range(8):
                    kb.mm(ps[:], self.ones_df[:], sq[:, k, :], start=(k == 0), stop=(k == 7), reads=[self.ones_df, sq], writes=[ps])
                kb.act(rstd[:], ps[:], AF.Ln, [ps], [rstd], bias=EPS)
                kb.act(rstd[:], rstd[:], AF.Exp, [rstd], [rstd], scale=-0.5)
                for k in range(8):
                    kb.stt("dve", o_t[:, k, :], x_t[:, k, :], fw[:, k:k + 1], rstd[:], ALU.mult, ALU.mult, [x_t, fw, rstd], [o_t])
                kb.dma("sp", dr["outT"][:, tsl].rearrange("(k p) n -> p k n", p=128), o_t[:], reads=[o_t], writes=[db["outT"]])
            kb.barrier()


def _prep_inputs(inputs, b):
    f = lambda a: np.ascontiguousarray(a, dtype=np.float32)
    m = {}
    m["xT"] = f(inputs["x"][b].T)
    m["c_pk"] = f(inputs["c"][b].reshape(8, 128).T)
    m["pos"] = np.ascontiguousarray(inputs["positions"][b][None, :].astype(np.int32))
    m["ada_w"] = f(inputs["ada_w"])
    m["ada_b_pk"] = f(inputs["ada_b"].reshape(DEPTH, 6, 8, 128).transpose(0, 3, 1, 2).reshape(DEPTH, 128, 48))
    m["w_fm"] = f(inputs["w_in"][:, :, _fm_cols()])
    m["w_tm"] = f(inputs["w_in"][:, :, _tm_cols()])
    m["hg_lb_pk"] = f(inputs["hg_lb_logits"].reshape(DEPTH, 4, 128).transpose(2, 0, 1))
    m["conv_w_pk"] = f(inputs["m2_conv_w"].reshape(DEPTH, 4, 8, 128).transpose(0, 3, 2, 1))
    m["conv_b_pk"] = f(inputs["m2_conv_b"].reshape(DEPTH, 8, 128).transpose(0, 2, 1))
    m["dt_bias"] = f(inputs["m2_dt_bias"].reshape(DEPTH, 1, 8))
    m["hg_norm_pk"] = f(inputs["hg_norm_w"].T)
    pk = lambda a: a.reshape(DEPTH, 16, 2, 64).transpose(0, 2, 3, 1).reshape(DEPTH, 128, 16)
    m["s5_lr_pk"] = f(pk(inputs["s5_lam_re"]))
    m["s5_li_pk"] = f(pk(inputs["s5_lam_im"]))
    m["s5_ldt_pk"] = f(np.broadcast_to(inputs["s5_log_dt"].reshape(DEPTH, 16, 2).transpose(0, 2, 1)[:, :, None, :],
                                       (DEPTH, 2, 64, 16)).reshape(DEPTH, 128, 16))
    pkb = lambda a: a.reshape(DEPTH, 16, 2, 64, 16).transpose(0, 2, 3, 1, 4).reshape(DEPTH, 128, 16, 16)
    m["s5_bre_pk"] = f(pkb(inputs["s5_b_re"]))
    m["s5_bim_pk"] = f(pkb(inputs["s5_b_im"]))
    m["s5_cre_pk"] = f(inputs["s5_c_re"].reshape(DEPTH, 4, 128, 64))
    m["s5_cim_pk"] = f(inputs["s5_c_im"].reshape(DEPTH, 4, 128, 64))
    m["s5_d_pk"] = f(inputs["s5_d"].reshape(DEPTH, 4, 128).transpose(0, 2, 1))
    m["s5_wglu"] = f(inputs["s5_w_glu"])
    m["w_gate"] = f(inputs["w_gate"])
    m["b_gate_pk"] = f(inputs["b_gate"].reshape(DEPTH, 32, 128).transpose(0, 2, 1))
    m["w_branch"] = f(inputs["w_branch"].reshape(DEPTH, 4 * 512, D))
    m["w_out"] = f(inputs["w_out"])
    m["moe_wr"] = f(np.concatenate([inputs["moe_w_group"], inputs["moe_w_expert"]], axis=2))
    m["moe_br"] = f(np.concatenate([inputs["moe_b_group"], inputs["moe_b_expert"]], axis=1).reshape(DEPTH, 1, 36))
    w13 = np.concatenate([inputs["moe_w1"].reshape(DEPTH, 32, 8, 128, 256), inputs["moe_w3"].reshape(DEPTH, 32, 8, 128, 256)], axis=4)
    m["moe_w13_pk"] = f(w13.transpose(0, 1, 3, 2, 4).reshape(DEPTH, 32, 128, 8 * 512))
    m["moe_w2_pk"] = f(inputs["moe_w2"].reshape(DEPTH, 4, 16, 128, D).transpose(0, 1, 3, 2, 4).reshape(DEPTH, 4, 128, 16 * D))
    m["fnorm_pk"] = f(inputs["final_norm_w"].reshape(8, 128).T)
    m["m2_alog"] = f(inputs["m2_a_log"].reshape(DEPTH, 1, 8))
    m["m2_d"] = f(inputs["m2_d"].reshape(DEPTH, 1, 8))
    m["m2_normw"] = f(inputs["m2_norm_w"].reshape(DEPTH, 1, 512))
    m.update(_const_tables())
    return m


def run(inputs, batches=(0, 1, 2, 3), core_ids=None, debug=None, upto="all", trace=False):
    prog = Prog(debug=debug, upto=upto)
    nc = prog.build()
    in_maps = [_prep_inputs(inputs, b) for b in batches]
    if core_ids is None:
        core_ids = list(range(len(batches)))
    if trace:
        return run_bass_kernel_spmd(nc, in_maps, core_ids=core_ids, trace=True)
    res = run_bass_kernel_spmd(nc, in_maps, core_ids=core_ids)
    return res.results


def kernel(**inputs):
    inputs = {k: np.asarray(v) for k, v in inputs.items()}
    results = run(inputs, batches=(0, 1, 2, 3), core_ids=[0, 2, 4, 6])
    out = np.stack([np.ascontiguousarray(r["outT"].T) for r in results], axis=0)
    return out.astype(np.float32)
```
